# Optimizing a Trainium2 kernel written in Bass

```python
import jax
import jax.numpy as jnp
from jax import lax
import numpy as np

D_MODEL = 2048
BATCH = 4
SEQ = 4096
DEPTH = 4

N_MEM = 256
NORM_EPS = 1e-6

RWKV_HEADS = 16
RWKV_HEAD_DIM = 64
RWKV_WIDTH = RWKV_HEADS * RWKV_HEAD_DIM
DECAY_LORA = 96
ICLR_LORA = 96
VRES_LORA = 64
GATE_LORA = 256
RWKV_GN_EPS = 64e-5

MLA_HEADS = 8
QK_NOPE_DIM = 128
QK_ROPE_DIM = 64
V_HEAD_DIM = 128
Q_LORA_RANK = 512
KV_LORA_RANK = 256
MLA_WIDTH = MLA_HEADS * V_HEAD_DIM
ROPE_THETA = 10000.0
Q_BLOCK = 128

MIX_WIDTH = RWKV_WIDTH + MLA_WIDTH
RWKV_IN = 3 * RWKV_WIDTH + DECAY_LORA + ICLR_LORA + GATE_LORA
MLA_IN = Q_LORA_RANK + KV_LORA_RANK + QK_ROPE_DIM
IN_WIDTH = RWKV_IN + MLA_IN

MEM_HEADS = 4
MEM_HEAD_DIM = 128
MEM_WIDTH = MEM_HEADS * MEM_HEAD_DIM

D_FF = 4 * D_MODEL

kernel_name = 'hybrid_rwkv7_mla_memory_block'


def rms_norm(x, g):
    x32 = x.astype(jnp.float32)
    y = x32 * lax.rsqrt(jnp.mean(jnp.square(x32), axis=-1, keepdims=True) + NORM_EPS)
    return (y * g.astype(jnp.float32)).astype(x.dtype)


def token_shift(y, mu):
    y_prev = jnp.pad(y[:, :-1], ((0, 0), (1, 0), (0, 0)))
    return y + (y_prev - y) * mu


def rope_tables(seq_len):
    pos = jnp.arange(seq_len, dtype=jnp.float32)
    inv_freq = ROPE_THETA ** (-jnp.arange(0, QK_ROPE_DIM, 2, dtype=jnp.float32) / QK_ROPE_DIM)
    ang = pos[:, None] * inv_freq[None, :]
    return jnp.cos(ang), jnp.sin(ang)


def apply_rope(x, cos, sin):
    half = x.shape[-1] // 2
    x1 = x[..., :half].astype(jnp.float32)
    x2 = x[..., half:].astype(jnp.float32)
    return jnp.concatenate([x1 * cos - x2 * sin, x1 * sin + x2 * cos], axis=-1).astype(x.dtype)


def rwkv7_scan(r, decay, k, v, kk, a):
    B, S, H, N = r.shape

    def step(state, inp):
        r_t, w_t, k_t, v_t, kk_t, b_t = inp
        sa = jnp.einsum('bhvk,bhk->bhv', state, -kk_t)
        state = (state * w_t[:, :, None, :]
                 + sa[..., :, None] * b_t[:, :, None, :]
                 + v_t[..., :, None] * k_t[:, :, None, :])
        return state, jnp.einsum('bhvk,bhk->bhv', state, r_t)

    xs = tuple(jnp.moveaxis(t, 1, 0) for t in (r, decay, k, v, kk, kk * a))
    _, out = lax.scan(step, jnp.zeros((B, H, N, N), jnp.float32), xs)
    return jnp.moveaxis(out, 0, 1)


def rwkv7_time_mix(cols, mu, w0, w_up, a0, a_up, g_up, k_k, k_a, r_k, lnx_g, lnx_b,
                   v_first, vres_cols, mu_vres, v0, v_up):
    B, S, _ = cols.shape
    H, N, C = RWKV_HEADS, RWKV_HEAD_DIM, RWKV_WIDTH
    f32 = jnp.float32
    c = token_shift(cols, mu)
    r, k, v, wl, al, gl = jnp.split(
        c, [C, 2 * C, 3 * C, 3 * C + DECAY_LORA, 3 * C + DECAY_LORA + ICLR_LORA], axis=-1)
    log_w = -jax.nn.softplus(-(w0 + jnp.tanh(wl) @ w_up).astype(f32)) - 0.5
    a = jax.nn.sigmoid((a0 + al @ a_up).astype(f32))
    g = jax.nn.sigmoid(gl) @ g_up
    if v_first is None:
        v_first = v
    else:
        vr = token_shift(vres_cols, mu_vres)
        v = v + (v_first - v) * jax.nn.sigmoid(v0 + vr @ v_up)

    def heads(t):
        return t.reshape(B, S, H, N).astype(f32)

    r, k, v, a, log_w = heads(r), heads(k), heads(v), heads(a), heads(log_w)
    kk = k * k_k.reshape(H, N).astype(f32)
    kk = kk * lax.rsqrt(jnp.maximum(jnp.sum(kk * kk, axis=-1, keepdims=True), 1e-24))
    k = k * (1.0 + (a - 1.0) * k_a.reshape(H, N).astype(f32))
    decay = jnp.exp(-jnp.exp(log_w))
    o = rwkv7_scan(r, decay, k, v, kk, a)
    mean = jnp.mean(o, axis=-1, keepdims=True)
    var = jnp.mean(jnp.square(o - mean), axis=-1, keepdims=True)
    o = ((o - mean) * lax.rsqrt(var + RWKV_GN_EPS)).reshape(B, S, C) * lnx_g + lnx_b
    bonus = jnp.sum(r * k * r_k.astype(f32), axis=-1, keepdims=True) * v
    o = o + bonus.reshape(B, S, C)
    return (o * g).astype(cols.dtype), v_first


def causal_block_attention(qn, qr, kn, kr, v):
    S = qn.shape[1]
    scale = (QK_NOPE_DIM + QK_ROPE_DIM) ** -0.5
    outs = []
    for i in range(S // Q_BLOCK):
        q0, q1 = i * Q_BLOCK, (i + 1) * Q_BLOCK
        s = (jnp.einsum('bqhd,bkhd->bhqk', qn[:, q0:q1], kn[:, :q1])
             + jnp.einsum('bqhr,bkr->bhqk', qr[:, q0:q1], kr[:, :q1])).astype(jnp.float32) * scale
        mask = (q0 + jnp.arange(Q_BLOCK))[:, None] >= jnp.arange(q1)[None, :]
        s = jnp.where(mask, s, jnp.finfo(jnp.float32).min)
        p = jax.nn.softmax(s, axis=-1).astype(v.dtype)
        outs.append(jnp.einsum('bhqk,bkhd->bqhd', p, v[:, :q1]))
    return jnp.concatenate(outs, axis=1)


def mla_heads(cols, q_norm_g, w_uq, kv_norm_g, w_ukv, cos, sin):
    B, S, _ = cols.shape
    cq, ckv, kr = jnp.split(cols, [Q_LORA_RANK, Q_LORA_RANK + KV_LORA_RANK], axis=-1)
    q = (rms_norm(cq, q_norm_g) @ w_uq).reshape(B, S, MLA_HEADS, QK_NOPE_DIM + QK_ROPE_DIM)
    kv = (rms_norm(ckv, kv_norm_g) @ w_ukv).reshape(B, S, MLA_HEADS, QK_NOPE_DIM + V_HEAD_DIM)
    qn, qr = q[..., :QK_NOPE_DIM], q[..., QK_NOPE_DIM:]
    kn, v = kv[..., :QK_NOPE_DIM], kv[..., QK_NOPE_DIM:]
    qr = apply_rope(qr, cos[:, None, :], sin[:, None, :])
    kr = apply_rope(kr, cos, sin)
    o = causal_block_attention(qn, qr, kn, kr, v)
    return o.reshape(B, S, MLA_WIDTH)


def memory_cross_attention(u, mem_n, wq, wk, wv, wo):
    B, S, _ = u.shape
    M = mem_n.shape[1]
    q = (u @ wq).reshape(B, S, MEM_HEADS, MEM_HEAD_DIM)
    k = (mem_n @ wk).reshape(B, M, MEM_HEADS, MEM_HEAD_DIM)
    v = (mem_n @ wv).reshape(B, M, MEM_HEADS, MEM_HEAD_DIM)
    s = jnp.einsum('bqhd,bmhd->bhqm', q, k).astype(jnp.float32) * MEM_HEAD_DIM ** -0.5
    p = jax.nn.softmax(s, axis=-1).astype(v.dtype)
    o = jnp.einsum('bhqm,bmhd->bqhd', p, v).reshape(B, S, MEM_WIDTH)
    return o @ wo


def setup_inputs(seed: int = 0) -> dict:
    key = jax.random.key(seed)
    keys = iter(jax.random.split(key, 48))
    f32 = jnp.float32

    def normal(shape, scale):
        return scale * jax.random.normal(next(keys), shape, f32)

    def gain(shape):
        return 1.0 + 0.05 * jax.random.normal(next(keys), shape, f32)

    def uniform(shape, lo, hi):
        return jax.random.uniform(next(keys), shape, f32, lo, hi)

    L, L1, D = DEPTH, DEPTH - 1, D_MODEL
    return {
        'x': normal((BATCH, SEQ, D), 1.0),
        'mem': normal((BATCH, N_MEM, D), 1.0),
        'mem_norm_g': gain((D,)),
        'mix_pre_g': gain((L, D)),
        'w_in': normal((L, D, IN_WIDTH), D ** -0.5),
        'w_in_vres': normal((L1, D, VRES_LORA), D ** -0.5),
        'mu_rwkv': uniform((L, RWKV_IN), 0.0, 1.0),
        'mu_vres': uniform((L1, VRES_LORA), 0.0, 1.0),
        'w0': uniform((L, RWKV_WIDTH), -6.0, -1.0),
        'w_up': normal((L, DECAY_LORA, RWKV_WIDTH), 0.5 * DECAY_LORA ** -0.5),
        'a0': normal((L, RWKV_WIDTH), 0.1),
        'a_up': normal((L, ICLR_LORA, RWKV_WIDTH), ICLR_LORA ** -0.5),
        'v0': 1.0 + normal((L1, RWKV_WIDTH), 0.1),
        'v_up': normal((L1, VRES_LORA, RWKV_WIDTH), VRES_LORA ** -0.5),
        'g_up': normal((L, GATE_LORA, RWKV_WIDTH), GATE_LORA ** -0.5),
        'k_k': 0.85 + normal((L, RWKV_WIDTH), 0.05),
        'k_a': gain((L, RWKV_WIDTH)),
        'r_k': normal((L, RWKV_HEADS, RWKV_HEAD_DIM), 0.1),
        'lnx_g': gain((L, RWKV_WIDTH)),
        'lnx_b': normal((L, RWKV_WIDTH), 0.01),
        'q_norm_g': gain((L, Q_LORA_RANK)),
        'w_uq': normal((L, Q_LORA_RANK, MLA_HEADS * (QK_NOPE_DIM + QK_ROPE_DIM)), Q_LORA_RANK ** -0.5),
        'kv_norm_g': gain((L, KV_LORA_RANK)),
        'w_ukv': normal((L, KV_LORA_RANK, MLA_HEADS * (QK_NOPE_DIM + V_HEAD_DIM)), KV_LORA_RANK ** -0.5),
        'w_out': normal((L, MIX_WIDTH, D), MIX_WIDTH ** -0.5),
        'mix_post_g': gain((L, D)),
        'mem_pre_g': gain((L, D)),
        'wq_mem': normal((L, D, MEM_WIDTH), D ** -0.5),
        'wk_mem': normal((L, D, MEM_WIDTH), D ** -0.5),
        'wv_mem': normal((L, D, MEM_WIDTH), D ** -0.5),
        'wo_mem': normal((L, MEM_WIDTH, D), MEM_WIDTH ** -0.5),
        'mem_post_g': gain((L, D)),
        'ffn_pre_g': gain((L, D)),
        'w_ff1': normal((L, D, D_FF), D ** -0.5),
        'w_ff2': normal((L, D_FF, D), D_FF ** -0.5),
        'ffn_post_g': gain((L, D)),
    }


def reference(x, mem, mem_norm_g, mix_pre_g, w_in, w_in_vres, mu_rwkv, mu_vres, w0, w_up, a0, a_up,
              v0, v_up, g_up, k_k, k_a, r_k, lnx_g, lnx_b, q_norm_g, w_uq, kv_norm_g, w_ukv, w_out,
              mix_post_g, mem_pre_g, wq_mem, wk_mem, wv_mem, wo_mem, mem_post_g, ffn_pre_g, w_ff1,
              w_ff2, ffn_post_g):
    cos, sin = rope_tables(x.shape[1])
    mem_n = rms_norm(mem, mem_norm_g)
    h = x
    v_first = None
    for l in range(DEPTH):
        u = rms_norm(h, mix_pre_g[l])
        if l == 0:
            proj = u @ w_in[0]
            vres_cols, mu_v, v0_l, v_up_l = None, None, None, None
        else:
            proj = u @ jnp.concatenate([w_in[l], w_in_vres[l - 1]], axis=1)
            vres_cols, mu_v, v0_l, v_up_l = proj[..., IN_WIDTH:], mu_vres[l - 1], v0[l - 1], v_up[l - 1]
        y_rwkv, v_first = rwkv7_time_mix(
            proj[..., :RWKV_IN], mu_rwkv[l], w0[l], w_up[l], a0[l], a_up[l], g_up[l], k_k[l], k_a[l],
            r_k[l], lnx_g[l], lnx_b[l], v_first, vres_cols, mu_v, v0_l, v_up_l)
        y_mla = mla_heads(proj[..., RWKV_IN:IN_WIDTH], q_norm_g[l], w_uq[l], kv_norm_g[l], w_ukv[l], cos, sin)
        y = jnp.concatenate([y_rwkv, y_mla], axis=-1) @ w_out[l]
        h = h + rms_norm(y, mix_post_g[l])
        u = rms_norm(h, mem_pre_g[l])
        y = memory_cross_attention(u, mem_n, wq_mem[l], wk_mem[l], wv_mem[l], wo_mem[l])
        h = h + rms_norm(y, mem_post_g[l])
        u = rms_norm(h, ffn_pre_g[l])
        y = jnp.square(jax.nn.relu(u @ w_ff1[l])) @ w_ff2[l]
        h = h + rms_norm(y, ffn_post_g[l])
    return h
```

```python
import contextlib
import numpy as np
import concourse.bass as bass
import concourse.mybir as mybir
from concourse.bass_utils import run_bass_kernel_spmd

F32 = mybir.dt.float32
BF16 = mybir.dt.bfloat16
AF = mybir.ActivationFunctionType
ALU = mybir.AluOpType

D = 2048
TT = 512
NMEM = 256
RW = 1024
IN_W = 4352
DFF = 8192
GN_EPS = 64e-5
EPS = 1e-6
DECAY_C = -0.6065306597126334


class Sched:
    CE = ('pe', 'dve', 'act', 'pool')
    ENG = ('pe', 'dve', 'act', 'pool', 'sp')

    def __init__(self, nc, stack):
        self.nc = nc
        self.stack = stack
        self.q = {e: [] for e in self.ENG}
        self.semh = {}
        self.cnt = {}
        for e in self.CE:
            self._mksem('s_' + e)
        self.lastw = {}
        self.readers = {}
        self.seen = {e: {} for e in self.ENG}
        self.n_ops = 0

    def _mksem(self, name):
        self.semh[name] = self.stack.enter_context(self.nc.semaphore(name))
        self.cnt[name] = 0

    def _waits(self, eng, reads, writes):
        need = {}

        def add(d):
            if d is not None and need.get(d[0], 0) < d[1]:
                need[d[0]] = d[1]
        for k in reads:
            add(self.lastw.get(k))
        for k in writes:
            add(self.lastw.get(k))
            r = self.readers.get(k)
            if r:
                for s, v in r.items():
                    add((s, v))
        waits = []
        for s, v in need.items():
            if eng == 'pe' and s == 's_pe':
                continue
            if self.seen[eng].get(s, 0) < v:
                self.seen[eng][s] = v
                waits.append((s, v))
        return waits

    def _commit(self, ident, reads, writes):
        for k in writes:
            self.lastw[k] = ident
            self.readers[k] = {}
        for k in reads:
            r = self.readers.setdefault(k, {})
            if r.get(ident[0], 0) < ident[1]:
                r[ident[0]] = ident[1]

    def op(self, eng, fn, reads=(), writes=(), inc=True):
        waits = self._waits(eng, reads, writes)
        s = 's_' + eng
        if inc:
            self.cnt[s] += 1
            ident = (s, self.cnt[s])
        else:
            ident = (s, self.cnt[s] + 1)
        self._commit(ident, reads, writes)
        self.q[eng].append((waits, fn, (s, 1) if inc else None))
        self.n_ops += 1

    def dma(self, eng, out, in_, reads=(), writes=(), semkey=None, slow=False):
        assert semkey is not None
        s = 'd_' + semkey
        if s not in self.semh:
            self._mksem(s)
        waits = self._waits(eng, reads, writes)
        self.cnt[s] += 16
        ident = (s, self.cnt[s])
        self._commit(ident, reads, writes)
        if slow:
            fn = lambda e: e.dma_start(out=out, in_=in_, allow_slow_non_contiguous=True)
        else:
            fn = lambda e: e.dma_start(out=out, in_=in_)
        self.q[eng].append((waits, fn, (s, 16)))
        self.n_ops += 1

    def barrier(self, engines=None):
        for e in (engines or self.ENG):
            waits = []
            for s, c in self.cnt.items():
                if c > 0 and self.seen[e].get(s, 0) < c:
                    if e == 'pe' and s == 's_pe':
                        continue
                    self.seen[e][s] = c
                    waits.append((s, c))
            if waits:
                self.q[e].append((waits, None, None))

    def emit(self):
        engobj = {'pe': 'tensor', 'dve': 'vector', 'act': 'scalar', 'pool': 'gpsimd', 'sp': 'sync'}
        with self.nc.Block() as block:
            for e in self.ENG:
                items = self.q[e]
                if not items:
                    continue

                def body(eo, items=items):
                    for waits, fn, inc in items:
                        for s, v in waits:
                            eo.wait_ge(self.semh[s], v)
                        if fn is not None:
                            ins = fn(eo)
                            if inc is not None:
                                ins.then_inc(self.semh[inc[0]], inc[1])
                getattr(block, engobj[e])(body)


PCOL = {}
_off = 0
for _n, _w in [('mix_pre_g', 16), ('mix_post_g', 16), ('mem_pre_g', 16), ('mem_post_g', 16), ('ffn_pre_g', 16),
               ('ffn_post_g', 16), ('mu_r', 8), ('mu_k', 8), ('mu_v', 8), ('mu_wl', 1), ('mu_al', 1), ('mu_gl', 2),
               ('mu_vr', 1), ('w0', 8), ('a0', 8), ('v0', 8), ('k_k', 8), ('k_a', 8), ('r_k', 8), ('lnx_g', 8),
               ('lnx_b', 8), ('qn_g', 4), ('kvn_g', 2)]:
    PCOL[_n] = (_off, _w)
    _off += _w
NP_RAW = _off
MU_COLS = PCOL['mu_r'][0]
N_MU = 29
PCOL['om'] = (NP_RAW, N_MU)
PCOL['omka'] = (NP_RAW + N_MU, 8)
NPAR = NP_RAW + N_MU + 8


def _fm(v, ncol):
    buf = np.zeros(ncol * 128, np.float32)
    v = np.asarray(v, np.float32).reshape(-1)
    buf[:v.size] = v
    return buf.reshape(ncol, 128).T


def pack_params(inp, l):
    P = np.zeros((128, NPAR), np.float32)

    def put(name, arr):
        o, w = PCOL[name]
        P[:, o:o + w] = arr
    for n in ['mix_pre_g', 'mix_post_g', 'mem_pre_g', 'mem_post_g', 'ffn_pre_g', 'ffn_post_g']:
        put(n, _fm(inp[n][l], 16))
    mu = inp['mu_rwkv'][l]
    put('mu_r', _fm(mu[0:1024], 8))
    put('mu_k', _fm(mu[1024:2048], 8))
    put('mu_v', _fm(mu[2048:3072], 8))
    put('mu_wl', _fm(mu[3072:3168], 1))
    put('mu_al', _fm(mu[3168:3264], 1))
    put('mu_gl', _fm(mu[3264:3520], 2))
    if l > 0:
        put('mu_vr', _fm(inp['mu_vres'][l - 1], 1))
        put('v0', _fm(inp['v0'][l - 1], 8))
    for n in ['w0', 'a0', 'k_k', 'k_a', 'lnx_g', 'lnx_b']:
        put(n, _fm(inp[n][l], 8))
    put('r_k', _fm(inp['r_k'][l].reshape(-1), 8))
    put('qn_g', _fm(inp['q_norm_g'][l], 4))
    put('kvn_g', _fm(inp['kv_norm_g'][l], 2))
    return P


def make_consts(T):
    c = {}
    c['identF'] = np.eye(128, dtype=np.float32)
    bo = np.zeros((128, 128), np.float32)
    bo[:64, :64] = 1.0
    bo[64:, 64:] = 1.0
    c['blockones'] = bo
    r = np.arange(128)
    mk1 = np.zeros((128, 192), np.float32)
    mk1[:, :128] = (r[:, None] < r[None, :]).astype(np.float32)
    mk1[:, 128:] = ((r[:, None] % 64) <= np.arange(64)[None, :]).astype(np.float32)
    c['mk1'] = mk1
    c['mk2'] = (r[:, None] > r[None, :]).astype(np.float32)
    c['cmask'] = np.where(r[:, None] >= r[None, :], 0.0, -1e9).astype(np.float32)
    rs = np.ones((128, TT), np.float32)
    rs[:, ::64] = 0.0
    c['reset'] = rs
    pos = np.arange(T, dtype=np.float32)
    inv_freq = (10000.0 ** (-np.arange(0, 64, 2, dtype=np.float32) / 64)).astype(np.float32)
    ang = (pos[:, None] * inv_freq[None, :]).astype(np.float32)
    cs, sn = np.cos(ang).T.astype(np.float32), np.sin(ang).T.astype(np.float32)
    c['rope'] = np.stack([np.concatenate([cs, cs], 0), np.concatenate([-sn, sn], 0)], 0).astype(np.float32)
    return c


def build(T, L, taps=()):
    NT = T // TT
    nc = bass.Bass("TRN2", target_bir_lowering=False)
    din = lambda name, shape, dt=F32: nc.dram_tensor(name, list(shape), dt, kind="ExternalInput").ap()
    x_d = din("x", [T, D])
    mem_d = din("mem", [NMEM, D])
    memg_d = din("memg", [128, 16])
    par_d = din("params", [L, 128, NPAR])
    w_in_d = din("w_in", [L, D, IN_W])
    w_vres_d = din("w_in_vres", [max(L - 1, 1), D, 64])
    w_up_d = din("w_up", [L, 96, RW])
    a_up_d = din("a_up", [L, 96, RW])
    g_up_d = din("g_up", [L, 256, RW])
    v_up_d = din("v_up", [max(L - 1, 1), 64, RW])
    w_uq_d = din("w_uq", [L, 512, 1536])
    w_ukv_d = din("w_ukv", [L, 256, 2048])
    w_out_d = din("w_out", [L, D, D])
    wq_d = din("wq_mem", [L, D, 512])
    wk_d = din("wk_mem", [L, D, 512])
    wv_d = din("wv_mem", [L, D, 512])
    wo_d = din("wo_mem", [L, 512, D])
    ff1_d = din("w_ff1", [L, D, DFF])
    ff2_d = din("w_ff2", [L, DFF, D])
    cn = make_consts(T)
    cd = {k: din("c_" + k, v.shape) for k, v in cn.items()}
    out_d = nc.dram_tensor("out", [T, D], F32, kind="ExternalOutput").ap()
    tap_d = {}
    for name, shape in taps:
        tap_d[name] = nc.dram_tensor("tap_" + name, list(shape), F32, kind="ExternalOutput").ap()
    hT_d = nc.dram_tensor("hT_s", [D, T], F32).ap()
    vf_d = nc.dram_tensor("vf_s", [RW, T], F32).ap()
    kn_d = nc.dram_tensor("kn_s", [8, 128, T], BF16).ap()
    kr_d = nc.dram_tensor("kr_s", [64, T], BF16).ap()
    vt_d = nc.dram_tensor("vt_s", [T, 1024], BF16).ap()
    memT_d = nc.dram_tensor("memT_s", [128, 16 * NMEM], BF16).ap()

    with contextlib.ExitStack() as st:
        S = Sched(nc, st)
        sb = lambda name, shape, dt=F32: st.enter_context(nc.sbuf_tensor(name, list(shape), dt))
        pst = lambda name, shape, dt=F32: st.enter_context(nc.psum_tensor(name, list(shape), dt))

        identF = sb("identF", [128, 128])
        identB = sb("identB", [128, 128], BF16)
        onesB = sb("onesB", [128, 128], BF16)
        blockones = sb("blockones", [128, 128])
        gnones = sb("gnones", [128, 128])
        mk1 = sb("mk1", [128, 192])
        mk2 = sb("mk2", [128, 128])
        cmask = sb("cmask", [128, 128])
        reset = sb("reset", [128, TT])
        hT = sb("hT", [128, 16, TT])
        uT = sb("uT", [128, 16, TT], BF16)
        yT = sb("yT", [128, 16, TT], BF16)
        wb = [sb("wb0", [128, 16 * 512], BF16), sb("wb1", [128, 16 * 512], BF16)]
        par = sb("par", [128, NPAR])
        lw = sb("lw", [128, 5, RW], BF16)
        kmT = sb("kmT", [128, 4, NMEM], BF16)
        vm = sb("vm", [128, 2, 512], BF16)
        Sbd = sb("Sbd", [128, 8, 2, 128])
        carry = sb("carry", [128, N_MU])
        rstd = sb("rstd", [128, TT])
        small = sb("small", [128, 64])
        ARENA_N = 20480
        arena = sb("arena", [128, ARENA_N])

        psF = [pst("psF%d" % i, [128, 512]) for i in range(6)]
        psB = [pst("psB%d" % i, [128, 1024], BF16) for i in range(2)]
        pctr = {'f': 0, 'b': 0}

        def pf():
            i = pctr['f'] % 6
            pctr['f'] += 1
            return psF[i], ('psF', i)

        def pb():
            i = pctr['b'] % 2
            pctr['b'] += 1
            return psB[i], ('psB', i)

        def pc(name):
            o, w = PCOL[name]
            return o

        def mm(out, lhsT, rhs, r, w, start=True, stop=True):
            S.op('pe', lambda e: e.matmul(out, lhsT, rhs, start=start, stop=stop), r, w, inc=stop)

        def tr(out, in_, ident, r, w, inc=True):
            S.op('pe', lambda e: e.transpose(out, in_, ident), r, w, inc=inc)

        def act(out, in_, func, r, w, bias=0.0, scale=1.0, accum=None):
            if accum is None:
                S.op('act', lambda e: e.activation(out, in_, func, bias=bias, scale=scale), r, w)
            else:
                S.op('act', lambda e: e.activation(out, in_, func, bias=bias, scale=scale, accum_out=accum), r, w)

        def cp(eng, out, in_, r, w):
            if eng == 'act':
                S.op('act', lambda e: e.copy(out, in_), r, w)
            else:
                S.op(eng, lambda e: e.tensor_copy(out, in_), r, w)

        def tt(eng, out, a, b, op, r, w):
            S.op(eng, lambda e: e.tensor_tensor(out, a, b, op), r, w)

        def ts(eng, out, a, s1, op0, r, w, s2=None, op1=None):
            if op1 is None:
                S.op(eng, lambda e: e.tensor_scalar(out, a, s1, None, op0), r, w)
            else:
                S.op(eng, lambda e: e.tensor_scalar(out, a, s1, s2, op0, op1), r, w)

        def stt(out, in0, scalar, in1, op0, op1, r, w):
            S.op('dve', lambda e: e.scalar_tensor_tensor(out, in0, scalar, in1, op0, op1), r, w)

        def memset(eng, ap, val, w):
            S.op(eng, lambda e: e.memset(ap, val), (), w)

        def tap(name, src_ap, r, dst=None):
            if name in tap_d:
                S.dma('sp', tap_d[name] if dst is None else dst, src_ap, reads=r, semkey='tap_' + name)

        def AF32(off, n):
            return arena[:, off:off + n]

        def AB16(off, n):
            return arena[:, off:off + n].bitcast(BF16)

        for nm, t in [('identF', identF), ('blockones', blockones), ('mk1', mk1), ('mk2', mk2), ('cmask', cmask),
                      ('reset', reset)]:
            S.dma('sp', t[:], cd[nm], writes=[nm], semkey='c_' + nm)
        cp('dve', identB[:], identF[:], ['identF'], ['identB'])
        memset('dve', onesB[:], 1.0, ['onesB'])
        ts('dve', gnones[:], blockones[:], 1.0 / 64.0, ALU.mult, ['blockones'], ['gnones'])

        wctr = {'i': 0}

        def wload(parts):
            i = wctr['i'] % 2
            wctr['i'] += 1
            key = ('wb', i)
            for (off, kc, ncols, src) in parts:
                dst = wb[i][:, off:off + kc * ncols].rearrange("p (k n) -> p k n", k=kc)
                S.dma('pool', dst, src, writes=[key], semkey='wb%d' % i)
            return wb[i], key

        def wview(buf, off, kc, ncols):
            return buf[:, off:off + kc * ncols].rearrange("p (k n) -> p k n", k=kc)

        def rms_stats(src_fn, KC, dn, eps, rkeys, sq_buf=None, sqkey=None):
            sq = uT if sq_buf is None else sq_buf
            sqk = 'uT' if sqkey is None else sqkey
            for c in range(KC):
                act(sq[:, c, :], src_fn(c), AF.Square, rkeys, [(sqk, c)])
            p, pk = pf()
            for c in range(KC):
                mm(p[:, :], onesB[:], sq[:, c, :], ['onesB', (sqk, c)], [pk], start=(c == 0), stop=(c == KC - 1))
            act(rstd[:], p[:, :], AF.Sqrt, [pk], ['rstd'], bias=eps, scale=1.0 / dn)
            S.op('dve', lambda e: e.reciprocal(rstd[:], rstd[:]), ['rstd'], ['rstd'])

        def pre_norm(gname):
            rms_stats(lambda c: hT[:, c, :], 16, float(D), EPS, ['hT'])
            g0 = pc(gname)
            for c in range(16):
                stt(uT[:, c, :], hT[:, c, :], par[:, g0 + c:g0 + c + 1], rstd[:], ALU.mult, ALU.mult,
                    ['hT', 'par', 'rstd', ('uT', c)], [('uT', c)])

        def post_norm_res(z, zkey, gname):
            rms_stats(lambda c: z[:, c, :], 16, float(D), EPS, [zkey])
            g0 = pc(gname)
            for c in range(16):
                stt(z[:, c, :], z[:, c, :], par[:, g0 + c:g0 + c + 1], rstd[:], ALU.mult, ALU.mult,
                    [zkey, 'par', 'rstd'], [(zkey, 'n', c)])
                tt('pool', hT[:, c, :], hT[:, c, :], z[:, c, :], ALU.add, [(zkey, 'n', c), 'hT'], ['hT'])
            S.op('pool', lambda e: e.memset(small[:, 60:61], 0.0), [(zkey, 'n', c) for c in range(16)], [zkey])

        def prep_mem():
            mt = AF32(0, 2 * D).rearrange("p (b d) -> p b d", b=2)
            S.dma('sp', mt, mem_d.rearrange("(b p) d -> p b d", p=128), writes=['mt'], semkey='mt')
            mg = small[:, 0:16]
            S.dma('sp', mg, memg_d, writes=['mg'], semkey='mg')
            mf = AF32(2 * D, 16 * NMEM).rearrange("p (c t) -> p c t", c=16)
            for b in range(2):
                for c4 in range(4):
                    p, pk = pf()
                    for i in range(4):
                        c = c4 * 4 + i
                        tr(p[:, i * 128:(i + 1) * 128], mt[:, b, c * 128:(c + 1) * 128], identF[:], ['mt', 'identF'], [pk], inc=(i == 3))
                    cp('act', mf[:, c4 * 4:(c4 + 1) * 4, b * 128:(b + 1) * 128],
                       p[:, :].rearrange("p (i t) -> p i t", i=4), [pk], [('mf', b, c4)])
            mfk = [('mf', b, c4) for b in range(2) for c4 in range(4)]
            sq = AB16(2 * D + 16 * NMEM, 16 * NMEM // 2).rearrange("p (c t) -> p c t", c=16)
            for c in range(16):
                act(sq[:, c, :], mf[:, c, :], AF.Square, mfk, [('msq', c)])
            p, pk = pf()
            for c in range(16):
                mm(p[:, 0:NMEM], onesB[:], sq[:, c, :], ['onesB', ('msq', c)], [pk], start=(c == 0), stop=(c == 15))
            act(rstd[:, 0:NMEM], p[:, 0:NMEM], AF.Sqrt, [pk], ['rstd'], bias=EPS, scale=1.0 / D)
            S.op('dve', lambda e: e.reciprocal(rstd[:, 0:NMEM], rstd[:, 0:NMEM]), ['rstd'], ['rstd'])
            memT = AB16(2 * D + 16 * NMEM + 16 * NMEM // 2, 16 * NMEM // 2).rearrange("p (c t) -> p c t", c=16)
            for c in range(16):
                stt(memT[:, c, :], mf[:, c, :], mg[:, c:c + 1], rstd[:, 0:NMEM], ALU.mult, ALU.mult,
                    mfk + ['mg', 'rstd'], ['memT'])
            S.dma('sp', memT_d, AB16(2 * D + 16 * NMEM + 16 * NMEM // 2, 16 * NMEM // 2), reads=['memT'], writes=['memT_d'], semkey='memTs')

        def layer_setup(l):
            S.barrier()
            S.dma('sp', par[:], par_d[l], writes=['par'], semkey='par')
            o, w = PCOL['om']
            ts('dve', par[:, o:o + N_MU], par[:, MU_COLS:MU_COLS + N_MU], -1.0, ALU.mult, ['par'], ['par'], s2=1.0, op1=ALU.add)
            o2, _ = PCOL['omka']
            ka = pc('k_a')
            ts('dve', par[:, o2:o2 + 8], par[:, ka:ka + 8], -1.0, ALU.mult, ['par'], ['par'], s2=1.0, op1=ALU.add)
            S.dma('pool', lw[0:96, 0, :], w_up_d[l], writes=['lw'], semkey='lw')
            S.dma('pool', lw[0:96, 1, :], a_up_d[l], writes=['lw'], semkey='lw')
            S.dma('pool', lw[:, 2:4, :], g_up_d[l].rearrange("(k p) n -> p k n", p=128), writes=['lw'], semkey='lw')
            if l > 0:
                S.dma('pool', lw[0:64, 4, :], v_up_d[l - 1], writes=['lw'], semkey='lw')
            memset('dve', carry[:], 0.0, ['carry'])
            memset('dve', Sbd[:], 0.0, ['Sbd'])
            memT = AB16(0, 16 * NMEM // 2).rearrange("p (c t) -> p c t", c=16)
            S.dma('sp', AB16(0, 16 * NMEM // 2), memT_d, reads=['memT_d'], writes=['memT'], semkey='memTl')
            buf, key = wload([(0, 16, 512, wk_d[l].rearrange("(k p) n -> p k n", p=128))])
            wv = wview(buf, 0, 16, 512)
            for h in range(4):
                p, pk = pf()
                for c in range(16):
                    mm(p[:, 0:NMEM], wv[:, c, h * 128:(h + 1) * 128], memT[:, c, :], [key, 'memT'], [pk], start=(c == 0), stop=(c == 15))
                cp('act', kmT[:, h, :], p[:, 0:NMEM], [pk], ['kmT'])
            buf, key = wload([(0, 16, 512, wv_d[l].rearrange("(k p) n -> p k n", p=128))])
            wv = wview(buf, 0, 16, 512)
            for b in range(2):
                p, pk = pf()
                for c in range(16):
                    mm(p[:, :], memT[:, c, b * 128:(b + 1) * 128], wv[:, c, :], [key, 'memT'], [pk], start=(c == 0), stop=(c == 15))
                cp('act', vm[:, b, :], p[:, :], [pk], ['vm'])

        def load_tile(l, j):
            if l == 0:
                xs = AF32(0, D)
                for blk in range(4):
                    S.dma('sp', xs, x_d[j * TT + blk * 128: j * TT + (blk + 1) * 128, :], writes=['xs'], semkey='xs')
                    for c4 in range(4):
                        p, pk = pf()
                        for i in range(4):
                            c = c4 * 4 + i
                            tr(p[:, i * 128:(i + 1) * 128], xs[:, c * 128:(c + 1) * 128], identF[:], ['xs', 'identF'], [pk], inc=(i == 3))
                        cp('act' if c4 % 2 else 'dve', hT[:, c4 * 4:(c4 + 1) * 4, blk * 128:(blk + 1) * 128],
                           p[:, :].rearrange("p (i t) -> p i t", i=4), [pk], [('hT', 'ld', blk, c4)])
                S.op('pool', lambda e: e.memset(small[:, 61:62], 0.0), [('hT', 'ld', blk, c4) for blk in range(4) for c4 in range(4)], ['hT'])
            else:
                S.dma('sp', hT[:], hT_d[:, j * TT:(j + 1) * TT].rearrange("(c p) t -> p c t", p=128),
                      reads=[('hT_d', j)], writes=['hT'], semkey='hT')

        def store_tile(l, j):
            if l == L - 1:
                os_ = AF32(0, D)
                for blk in range(4):
                    for c4 in range(4):
                        p, pk = pf()
                        for i in range(4):
                            c = c4 * 4 + i
                            tr(p[:, i * 128:(i + 1) * 128], hT[:, c, blk * 128:(blk + 1) * 128], identF[:], ['hT', 'identF'], [pk], inc=(i == 3))
                        cp('act' if c4 % 2 else 'dve', os_[:, c4 * 512:(c4 + 1) * 512], p[:, :], [pk], [('os', c4)])
                    S.dma('sp', out_d[j * TT + blk * 128: j * TT + (blk + 1) * 128, :], os_,
                          reads=[('os', c4) for c4 in range(4)], writes=[('out', j, blk)], semkey='os')
            else:
                S.dma('sp', hT_d[:, j * TT:(j + 1) * TT].rearrange("(c p) t -> p c t", p=128), hT[:],
                      reads=['hT'], writes=[('hT_d', j)], semkey='hT')

        def phase_rwkv(l, j):
            S.barrier()
            o = 0

            def slot(n):
                nonlocal o
                a = (o, n)
                o += n
                return a
            s_ar = slot(8 * 192)
            s_bt = slot(8 * 128)
            s_kt = slot(8 * 128)
            s_bh = slot(8 * 128)
            s_kh = slot(8 * 128)
            s_vh = slot(8 * 128)
            names = ['r', 'k', 'v', 'kk', 'a', 'lw', 'G', 'e1', 'e2', 'g', 'bon', 'k2', 'b', 't1', 't2']
            sl = {n: slot(TT) for n in names}
            s_ymu = [slot(TT + 1), slot(TT + 1)]
            s_tm = [slot(384), slot(384)]
            s_ab = [slot(192), slot(192)]
            s_ak = [slot(192), slot(192)]
            s_p = [slot(128) for _ in range(2)]
            s_ptt = [slot(128) for _ in range(2)]
            s_m = [slot(128) for _ in range(2)]
            s_x = [slot(128) for _ in range(2)]
            s_u = [slot(128) for _ in range(2)]
            tw = AB16(o, TT // 2); o += TT // 2
            alb = AB16(o, TT // 2); o += TT // 2
            sg = AB16(o, TT).rearrange("p (k t) -> p k t", k=2); o += TT
            vrb = AB16(o, TT // 2); o += TT // 2
            assert o <= ARENA_N, o
            AR = AF32(*s_ar).rearrange("p (c n) -> p c n", c=8)
            BT = AF32(*s_bt).rearrange("p (c n) -> p c n", c=8)
            KT = AF32(*s_kt).rearrange("p (c n) -> p c n", c=8)
            BH = AF32(*s_bh).rearrange("p (c n) -> p c n", c=8)
            KH = AF32(*s_kh).rearrange("p (c n) -> p c n", c=8)
            VH = AF32(*s_vh).rearrange("p (c n) -> p c n", c=8)
            V = {n: AF32(*sl[n]) for n in names}
            for nm, t in [('AR', AR), ('BT', BT), ('KT', KT), ('BH', BH), ('KH', KH), ('VH', VH)]:
                memset('pool', t, 0.0, [nm])

            pre_norm('mix_pre_g')
            uk = [('uT', c) for c in range(16)]
            ymu_i = [0]

            def shift_evac(p, pk, M, cid, out_ap, wkeys):
                yb = AF32(*s_ymu[ymu_i[0] % 2])
                yk = ('ymu', ymu_i[0] % 2)
                ymu_i[0] += 1
                cp('pool', yb[0:M, 0:1], carry[0:M, cid:cid + 1], ['carry'], [yk])
                S.op('act', lambda e: e.mul(yb[0:M, 1:TT + 1], p, par[0:M, MU_COLS + cid:MU_COLS + cid + 1]), [pk, 'par', yk], [yk])
                oo = PCOL['om'][0]
                stt(out_ap, p, par[0:M, oo + cid:oo + cid + 1], yb[0:M, 0:TT], ALU.mult, ALU.add, [pk, 'par', yk], wkeys)
                cp('pool', carry[0:M, cid:cid + 1], yb[0:M, TT:TT + 1], [yk], ['carry'])

            parts = [(0, 16, 448, w_in_d[l][:, 3072:3520].rearrange("(k p) n -> p k n", p=128))]
            if l > 0:
                parts.append((16 * 448, 16, 64, w_vres_d[l - 1].rearrange("(k p) n -> p k n", p=128)))
            buf, key = wload(parts)
            wl_ = wview(buf, 0, 16, 448)
            wvr = wview(buf, 16 * 448, 16, 64)
            t1 = V['t1']

            def proj(lhs_fn, M, rkeys):
                p, pk = pf()
                for c in range(16):
                    mm(p[0:M, :], lhs_fn(c), uT[:, c, :], rkeys + [('uT', c)], [pk], start=(c == 0), stop=(c == 15))
                return p[0:M, :], pk
            p, pk = proj(lambda c: wl_[:, c, 0:96], 96, [key])
            shift_evac(p, pk, 96, 24, t1[0:96, :], ['t1'])
            act(tw[0:96, :], t1[0:96, :], AF.Tanh, ['t1'], ['tw'])
            p, pk = proj(lambda c: wl_[:, c, 96:192], 96, [key])
            shift_evac(p, pk, 96, 25, alb[0:96, :], ['alb'])
            for i in range(2):
                p, pk = proj(lambda c, i=i: wl_[:, c, 192 + 128 * i:320 + 128 * i], 128, [key])
                shift_evac(p, pk, 128, 26 + i, t1[:, :], ['t1'])
                act(sg[:, i, :], t1[:, :], AF.Sigmoid, ['t1'], ['sg'])
            if l > 0:
                p, pk = proj(lambda c: wvr[:, c, :], 64, [key])
                shift_evac(p, pk, 64, 28, vrb[0:64, :], ['vrb'])

            for pr in range(8):
                parts = [(i * 16 * 128, 16, 128, w_in_d[l][:, i * 1024 + pr * 128: i * 1024 + (pr + 1) * 128].rearrange("(k p) n -> p k n", p=128))
                         for i in range(3)]
                buf, key = wload(parts)
                for i, nm in enumerate(['r', 'k', 'v']):
                    wv_ = wview(buf, i * 16 * 128, 16, 128)
                    p, pk = proj(lambda c, wv_=wv_: wv_[:, c, :], 128, [key])
                    shift_evac(p, pk, 128, 8 * i + pr, V[nm], [nm])
                rwkv_pair(l, j, pr, V, AR, BT, KT, BH, KH, VH, tw, alb, sg, vrb,
                          s_tm, s_ab, s_ak, s_p, s_ptt, s_m, s_x, s_u)

        def rwkv_pair(l, j, pr, V, AR, BT, KT, BH, KH, VH, tw, alb, sg, vrb, s_tm, s_ab, s_ak, s_p, s_ptt, s_m, s_x, s_u):
            cs = slice(pr * 128, (pr + 1) * 128)
            col = lambda n: par[:, pc(n) + pr:pc(n) + pr + 1]
            r, k, v, kk, a, lwv, G, e1, e2, g, bon, k2, b, t1, t2 = [V[n] for n in
                ['r', 'k', 'v', 'kk', 'a', 'lw', 'G', 'e1', 'e2', 'g', 'bon', 'k2', 'b', 't1', 't2']]
            O = k
            p, pk = pf()
            mm(p[:, :], lw[0:96, 0, cs], tw[0:96, :], ['lw', 'tw'], [pk])
            act(lwv, p[:, :], AF.Sigmoid, [pk, 'par'], ['lw_'], bias=col('w0'))
            ts('pool', lwv, lwv, DECAY_C, ALU.mult, ['lw_'], ['lw_'])
            p, pk = pf()
            mm(p[:, :], lw[0:96, 1, cs], alb[0:96, :], ['lw', 'alb'], [pk])
            act(a, p[:, :], AF.Sigmoid, [pk, 'par'], ['a'], bias=col('a0'))
            p, pk = pf()
            for i in range(2):
                mm(p[:, :], lw[:, 2 + i, cs], sg[:, i, :], ['lw', 'sg'], [pk], start=(i == 0), stop=(i == 1))
            cp('act', g, p[:, :], [pk], ['g'])
            if l > 0:
                p, pk = pf()
                mm(p[:, :], lw[0:64, 4, cs], vrb[0:64, :], ['lw', 'vrb'], [pk])
                act(t1, p[:, :], AF.Sigmoid, [pk, 'par'], ['t1'], bias=col('v0'))
                S.dma('sp', t2, vf_d[pr * 128:(pr + 1) * 128, j * TT:(j + 1) * TT], reads=[('vf_d', pr, j)], writes=['t2'], semkey='vfl')
                tt('dve', t2, t2, v, ALU.subtract, ['t2', 'v'], ['t2'])
                tt('dve', t2, t2, t1, ALU.mult, ['t2', 't1'], ['t2'])
                tt('dve', v, v, t2, ALU.add, ['v', 't2'], ['v'])
            else:
                S.dma('sp', vf_d[pr * 128:(pr + 1) * 128, j * TT:(j + 1) * TT], v, reads=['v'], writes=[('vf_d', pr, j)], semkey='vfs')
            ts('pool', kk, k, col('k_k'), ALU.mult, ['k', 'par'], ['kk'])
            tt('pool', t1, kk, kk, ALU.mult, ['kk'], ['t1'])
            p, pk = pf()
            mm(p[:, :], blockones[:], t1, ['blockones', 't1'], [pk])
            act(t2, p[:, :], AF.Sqrt, [pk], ['t2'])
            ts('dve', t2, t2, 1e-12, ALU.max, ['t2'], ['t2'])
            S.op('dve', lambda e: e.reciprocal(t2, t2), ['t2'], ['t2'])
            tt('dve', kk, kk, t2, ALU.mult, ['kk', 't2'], ['kk'])
            oka = PCOL['omka'][0]
            ts('dve', t1, a, col('k_a'), ALU.mult, ['a', 'par'], ['t1'], s2=par[:, oka + pr:oka + pr + 1], op1=ALU.add)
            tt('dve', k2, k, t1, ALU.mult, ['k', 't1'], ['k2'])
            tt('pool', b, kk, a, ALU.mult, ['kk', 'a'], ['b'])
            stt(t1, r, col('r_k'), k2, ALU.mult, ALU.mult, ['r', 'par', 'k2'], ['t1'])
            p, pk = pf()
            mm(p[:, :], blockones[:], t1, ['blockones', 't1'], [pk])
            tt('dve', bon, p[:, :], v, ALU.mult, [pk, 'v'], ['bon'])
            S.op('dve', lambda e: e.tensor_tensor_scan(G, reset[:], lwv, 0.0, ALU.mult, ALU.add), ['reset', 'lw_'], ['G'])
            c3 = lambda ap: ap.rearrange("p (c t) -> p c t", c=8)
            hs = [(slice(0, 64), slice(0, 64)), (slice(64, 128), slice(64, 128))]
            act(e1, G, AF.Exp, ['G'], ['e1'])
            tt('dve', AR[:, :, 128:192], c3(r), c3(e1), ALU.mult, ['r', 'e1'], ['AR'])
            tt('pool', e2, G, lwv, ALU.subtract, ['G', 'lw_'], ['e2'])
            act(e2, e2, AF.Exp, ['e2'], ['e2'])
            for ps_, fs_ in hs:
                stt(AR[ps_, :, fs_], c3(kk)[ps_], -1.0, c3(e2)[ps_], ALU.mult, ALU.mult, ['kk', 'e2'], ['AR'])
            act(e1, G, AF.Exp, ['G', 'AR'], ['e1'], scale=-1.0)
            for ps_, fs_ in hs:
                tt('dve', BT[ps_, :, fs_], c3(b)[ps_], c3(e1)[ps_], ALU.mult, ['b', 'e1'], ['BT'])
                tt('pool', KT[ps_, :, fs_], c3(k2)[ps_], c3(e1)[ps_], ALU.mult, ['k2', 'e1'], ['KT'])
            for c in range(8):
                act(e2[:, c * 64:(c + 1) * 64], G[:, c * 64:(c + 1) * 64], AF.Exp, ['G', 'e2'], ['e2'],
                    bias=G[:, c * 64 + 63:c * 64 + 64], scale=-1.0)
            for ps_, fs_ in hs:
                tt('dve', BH[ps_, :, fs_], c3(b)[ps_], c3(e2)[ps_], ALU.mult, ['b', 'e2'], ['BH'])
                tt('pool', KH[ps_, :, fs_], c3(k2)[ps_], c3(e2)[ps_], ALU.mult, ['k2', 'e2'], ['KH'])
                cp('pool', VH[ps_, :, fs_], c3(v)[ps_], ['v'], ['VH'])
            WC = small[:, 16:24]
            act(WC, G.rearrange("p (c t) -> p c t", c=8)[:, :, 63], AF.Exp, ['G'], ['WC'])

            for c in range(8):
                ci = c % 2
                TM = AF32(*s_tm[ci]); tmk = ('TM', ci)
                AB = AF32(*s_ab[ci]); abk = ('AB', ci)
                AK = AF32(*s_ak[ci]); akk = ('AK', ci)
                p, pk = pf()
                tr(p[:, 0:128], BH[:, c, :], identF[:], ['BH', 'identF'], [pk], inc=False)
                tr(p[:, 128:256], KH[:, c, :], identF[:], ['KH', 'identF'], [pk], inc=False)
                tr(p[:, 256:384], VH[:, c, :], identF[:], ['VH', 'identF'], [pk], inc=True)
                cp('act', TM, p[:, 0:384], [pk], [tmk])
                p, pk = pf()
                mm(p[:, 0:192], BT[:, c, :], AR[:, c, :], ['BT', 'AR'], [pk])
                tt('dve', AB, p[:, 0:192], mk1[:], ALU.mult, [pk, 'mk1'], [abk])
                p, pk = pf()
                mm(p[:, 0:192], KT[:, c, :], AR[:, c, :], ['KT', 'AR'], [pk])
                tt('dve', AK, p[:, 0:192], mk1[:], ALU.mult, [pk, 'mk1'], [akk])
                p, pk = pf()
                mm(p[:, 0:128], AR[:, c, 0:128], BT[:, c, :], ['BT', 'AR'], [pk])
                Pm = [AF32(*s_p[i]) for i in range(2)]
                Pt = [AF32(*s_ptt[i]) for i in range(2)]
                Mm = [AF32(*s_m[i]) for i in range(2)]
                tt('dve', Pt[0], p[:, 0:128], mk2[:], ALU.mult, [pk, 'mk2'], [('Pt', 0)])
                tt('pool', Mm[0], AB[:, 0:128], identF[:], ALU.add, [abk, 'identF'], [('Mm', 0)])
                Pcur, Pkey = AB[:, 0:128], abk
                for kq in range(1, 6):
                    src, dst = (kq - 1) % 2, kq % 2
                    if kq < 5:
                        p, pk = pf()
                        mm(p[:, 0:128], Pt[src], Pcur, [('Pt', src), Pkey], [pk])
                        cp('act', Pm[dst], p[:, 0:128], [pk], [('Pm', dst)])
                    p, pk = pf()
                    mm(p[:, 0:128], Pcur, Pt[src], [('Pt', src), Pkey], [pk])
                    cp('act', Pt[dst], p[:, 0:128], [pk], [('Pt', dst)])
                    p, pk = pf()
                    mm(p[:, 0:128], Pt[dst], Mm[src], [('Pt', dst), ('Mm', src)], [pk])
                    tt('dve', Mm[dst], p[:, 0:128], Mm[src], ALU.add, [pk, ('Mm', src)], [('Mm', dst)])
                    Pcur, Pkey = Pm[dst], ('Pm', dst)
                Tt, Ttk = Mm[1], ('Mm', 1)
                Scur = Sbd[:, pr, c % 2, :]
                Snxt = Sbd[:, pr, (c + 1) % 2, :]
                sck, snk = ('Sbd', pr, c % 2), ('Sbd', pr, (c + 1) % 2)
                X = AF32(*s_x[ci]); xk = ('X', ci)
                U = AF32(*s_u[ci]); uk_ = ('U', ci)
                p, pk = pf()
                mm(p[:, 0:128], AR[:, c, 0:128], Scur, ['AR', sck, 'Sbd'], [pk], start=True, stop=False)
                mm(p[:, 0:128], AK[:, 0:128], TM[:, 256:384], [akk, tmk], [pk], start=False, stop=True)
                cp('act', X, p[:, 0:128], [pk], [xk])
                p, pk = pf()
                mm(p[:, 0:128], Tt, X, [Ttk, xk], [pk])
                cp('act', U, p[:, 0:128], [pk], [uk_])
                p, pk = pf()
                mm(p[:, 0:128], TM[:, 0:128], U, [tmk, uk_], [pk], start=True, stop=False)
                mm(p[:, 0:128], TM[:, 128:256], TM[:, 256:384], [tmk], [pk], start=False, stop=True)
                stt(Snxt, Scur, WC[:, c:c + 1], p[:, 0:128], ALU.mult, ALU.add, [sck, 'WC', pk, 'Sbd'], [snk])
                p, pk = pf()
                mm(p[:, 0:64], Scur, AR[:, c, 128:192], [sck, 'AR', 'Sbd'], [pk], start=True, stop=False)
                mm(p[:, 0:64], U, AB[:, 128:192], [uk_, abk], [pk], start=False, stop=False)
                mm(p[:, 0:64], TM[:, 256:384], AK[:, 128:192], [tmk, akk], [pk], start=False, stop=True)
                cp('act', O[:, c * 64:(c + 1) * 64], p[:, 0:64], [pk, 'k'], ['k'])
            p, pk = pf()
            mm(p[:, :], gnones[:], O, ['gnones', 'k'], [pk])
            tt('dve', t1, O, p[:, :], ALU.subtract, ['k', pk], ['t1'])
            tt('pool', t2, t1, t1, ALU.mult, ['t1'], ['t2'])
            p, pk = pf()
            mm(p[:, :], gnones[:], t2, ['gnones', 't2'], [pk])
            act(t2, p[:, :], AF.Sqrt, [pk], ['t2'], bias=GN_EPS)
            S.op('dve', lambda e: e.reciprocal(t2, t2), ['t2'], ['t2'])
            tt('dve', t1, t1, t2, ALU.mult, ['t1', 't2'], ['t1'])
            ts('dve', t1, t1, col('lnx_g'), ALU.mult, ['t1', 'par'], ['t1'], s2=col('lnx_b'), op1=ALU.add)
            tt('pool', t1, t1, bon, ALU.add, ['t1', 'bon'], ['t1'])
            tt('dve', yT[:, pr, :], t1, g, ALU.mult, ['t1', 'g'], [('yT', pr)])
            if l == 0 and j == 0 and pr == 0:
                tap('O', O, ['k'])
                tap('v', v, ['v'])
                tap('r', r, ['r'])
                tap('kk', kk, ['kk'])
                tap('G', G, ['G'])
                tap('yr', t1, ['t1'])

        def phase_mla(l, j):
            S.barrier()
            o = 0

            def slot(n):
                nonlocal o
                a_ = (o, n)
                o += n
                return a_
            NK = (j + 1) * TT
            s_S = slot(max(T, 8 * TT))
            s_cq = (s_S[0], 4 * TT)
            s_ckv = (s_S[0] + 4 * TT, 2 * TT)
            s_t1 = (s_S[0] + 6 * TT, TT)
            s_t2 = (s_S[0] + 7 * TT, TT)

            def b16(n_units):
                nonlocal o
                v_ = AB16(o, n_units)
                o += n_units
                return v_
            s_rope = (o, 2 * TT)
            P_ = b16(max(T // 2, 2 * TT))
            Kh = b16(T // 2)
            Vh = b16(T // 2).rearrange("p (b d) -> p b d", d=128)
            krl = b16(T // 2)
            qT = b16(8 * TT // 2).rearrange("p (h t) -> p h t", h=8)
            qrT = b16(8 * TT // 2).rearrange("p (h t) -> p h t", h=8)
            cqn = b16(4 * TT // 2).rearrange("p (c t) -> p c t", c=4)
            ckvn = b16(2 * TT // 2).rearrange("p (c t) -> p c t", c=2)
            knst = b16(TT // 2)
            krst = b16(TT // 2)
            vst = [b16(512), b16(512)]
            PT = [b16(256), b16(256)]
            osb = b16(64)
            rsum = small[:, 24:26]
            mx = small[:, 26:27]
            assert o <= ARENA_N, o
            Ssb = AF32(*s_S)
            cq = AF32(*s_cq).rearrange("p (c t) -> p c t", c=4)
            ckv = AF32(*s_ckv).rearrange("p (c t) -> p c t", c=2)
            tmp1 = AF32(*s_t1)
            tmp2 = AF32(*s_t2)
            rope = AF32(*s_rope).rearrange("p (a t) -> p a t", a=2)
            S.dma('sp', rope[0:64], cd['rope'][:, :, j * TT:(j + 1) * TT].rearrange("a p t -> p a t"), writes=['rope'], semkey='rope')

            def proj(lhs_fn, M, rkeys):
                p, pk = pf()
                for c in range(16):
                    mm(p[0:M, :], lhs_fn(c), uT[:, c, :], rkeys + [('uT', c)], [pk], start=(c == 0), stop=(c == 15))
                return p, pk
            buf, key = wload([(0, 16, 512, w_in_d[l][:, 3520:4032].rearrange("(k p) n -> p k n", p=128))])
            wv_ = wview(buf, 0, 16, 512)
            for c in range(4):
                p, pk = proj(lambda cc, c=c: wv_[:, cc, c * 128:(c + 1) * 128], 128, [key])
                cp('act', cq[:, c, :], p[:, :], [pk], ['cq'])
            buf, key = wload([(0, 16, 320, w_in_d[l][:, 4032:4352].rearrange("(k p) n -> p k n", p=128))])
            wv_ = wview(buf, 0, 16, 320)
            for c in range(2):
                p, pk = proj(lambda cc, c=c: wv_[:, cc, c * 128:(c + 1) * 128], 128, [key])
                cp('act', ckv[:, c, :], p[:, :], [pk], ['ckv'])

            def rope_mm(lhs_fn, nkc, rhs_fn, rkeys):
                p1, pk1 = pf()
                for c in range(nkc):
                    mm(p1[0:64, :], lhs_fn(c, 0, 64), rhs_fn(c), rkeys(c), [pk1], start=(c == 0), stop=(c == nkc - 1))
                p2, pk2 = pf()
                for c in range(nkc):
                    mm(p2[0:32, :], lhs_fn(c, 32, 64), rhs_fn(c), rkeys(c), [pk2], start=(c == 0), stop=(c == nkc - 1))
                for c in range(nkc):
                    mm(p2[32:64, :], lhs_fn(c, 0, 32), rhs_fn(c), rkeys(c), [pk2], start=(c == 0), stop=(c == nkc - 1))
                tt('dve', tmp1[0:64, :], p1[0:64, :], rope[0:64, 0, :], ALU.mult, [pk1, 'rope'], ['tmp1'])
                tt('dve', tmp2[0:64, :], p2[0:64, :], rope[0:64, 1, :], ALU.mult, [pk2, 'rope'], ['tmp2'])
                tt('dve', tmp1[0:64, :], tmp1[0:64, :], tmp2[0:64, :], ALU.add, ['tmp1', 'tmp2'], ['tmp1'])

            kb = 256
            rope_mm(lambda c, lo, hi: wv_[:, c, kb + lo:kb + hi], 16, lambda c: uT[:, c, :], lambda c: [key, ('uT', c)])
            cp('act', krst[0:64, :], tmp1[0:64, :], ['tmp1'], ['krst'])
            S.dma('sp', kr_d[:, j * TT:(j + 1) * TT], krst[0:64, :], reads=['krst'], writes=[('kr_d', j)], semkey='krst')

            rms_stats(lambda c: cq[:, c, :], 4, 512.0, EPS, ['cq'], sq_buf=qT, sqkey='qTs')
            g0 = pc('qn_g')
            for c in range(4):
                stt(cqn[:, c, :], cq[:, c, :], par[:, g0 + c:g0 + c + 1], rstd[:], ALU.mult, ALU.mult, ['cq', 'par', 'rstd'], ['cqn'])
            rms_stats(lambda c: ckv[:, c, :], 2, 256.0, EPS, ['ckv'], sq_buf=qT, sqkey='qTs')
            g0 = pc('kvn_g')
            for c in range(2):
                stt(ckvn[:, c, :], ckv[:, c, :], par[:, g0 + c:g0 + c + 1], rstd[:], ALU.mult, ALU.mult, ['ckv', 'par', 'rstd'], ['ckvn'])

            SC = float((128 + 64) ** -0.5)
            buf, key = wload([(0, 4, 1536, w_uq_d[l].rearrange("(k p) n -> p k n", p=128))])
            wq_ = wview(buf, 0, 4, 1536)
            sqk = [('qTs', c) for c in range(4)]
            for h in range(8):
                p, pk = pf()
                for c in range(4):
                    mm(p[:, :], wq_[:, c, h * 192:h * 192 + 128], cqn[:, c, :], [key, 'cqn'], [pk], start=(c == 0), stop=(c == 3))
                S.op('act', lambda e, p=p, h=h: e.mul(qT[:, h, :], p[:, :], SC), [pk] + sqk, [('qTh', h)] + (sqk if h < 4 else []))
                rb = h * 192 + 128
                rope_mm(lambda c, lo, hi, rb=rb: wq_[:, c, rb + lo:rb + hi], 4, lambda c: cqn[:, c, :], lambda c: [key, 'cqn'])
                S.op('act', lambda e, h=h: e.mul(qrT[0:64, h, :], tmp1[0:64, :], SC), ['tmp1'], [('qrT', h)])
            buf, key = wload([(0, 2, 2048, w_ukv_d[l].rearrange("(k p) n -> p k n", p=128))])
            wkv_ = wview(buf, 0, 2, 2048)
            for h in range(8):
                p, pk = pf()
                for c in range(2):
                    mm(p[:, :], wkv_[:, c, h * 256:h * 256 + 128], ckvn[:, c, :], [key, 'ckvn'], [pk], start=(c == 0), stop=(c == 1))
                cp('act', knst[:, :], p[:, :], [pk], ['knst'])
                S.dma('sp', kn_d[h][:, j * TT:(j + 1) * TT], knst[:, :], reads=['knst'], writes=[('kn_d', h, j)], semkey='knst')
            wkv4 = wkv_.rearrange("p k (h two d) -> p k h two d", h=8, two=2)
            for tb in range(4):
                for hg in range(2):
                    p, pk = pf()
                    for c in range(2):
                        mm(p[:, :].rearrange("p (h d) -> p h d", h=4), ckvn[:, c, tb * 128:(tb + 1) * 128],
                           wkv4[:, c, hg * 4:(hg + 1) * 4, 1, :], [key, 'ckvn'], [pk], start=(c == 0), stop=(c == 1))
                    cp('act', vst[tb % 2][:, hg * 512:(hg + 1) * 512], p[:, :], [pk], [('vst', tb % 2)])
                S.dma('sp', vt_d[j * TT + tb * 128:j * TT + (tb + 1) * 128, :], vst[tb % 2], reads=[('vst', tb % 2)],
                      writes=[('vt_d', j, tb)], semkey='vst%d' % (tb % 2))
            S.barrier()
            S.dma('sp', krl[0:64, 0:NK], kr_d[:, 0:NK], reads=[('kr_d', jj) for jj in range(j + 1)], writes=['krl'], semkey='krl')
            for h in range(8):
                S.dma('sp', Kh[:, 0:NK], kn_d[h][:, 0:NK], reads=[('kn_d', h, jj) for jj in range(j + 1)], writes=['Kh'], semkey='Kh')
                S.dma('sp', Vh[:, 0:NK // 128, :], vt_d[0:NK, h * 128:(h + 1) * 128].rearrange("(b p) d -> p b d", p=128),
                      reads=[('vt_d', jj, tb_) for jj in range(j + 1) for tb_ in range(4)], writes=['Vh'], semkey='Vh')
                for qb in range(4):
                    q0 = j * TT + qb * 128
                    nk = q0 + 128
                    qs = slice(qb * 128, (qb + 1) * 128)
                    ng = (nk + 511) // 512
                    for gi in range(ng):
                        k0 = gi * 512
                        kw = min(512, nk - k0)
                        p, pk = pf()
                        mm(p[:, 0:kw], qT[:, h, qs], Kh[:, k0:k0 + kw], [('qTh', h), 'Kh'], [pk], start=True, stop=False)
                        mm(p[:, 0:kw], qrT[0:64, h, qs], krl[0:64, k0:k0 + kw], [('qrT', h), 'krl'], [pk], start=False, stop=True)
                        if k0 + kw == nk:
                            if kw > 128:
                                cp('act', Ssb[:, k0:k0 + kw - 128], p[:, 0:kw - 128], [pk], ['Ssb'])
                            tt('dve', Ssb[:, nk - 128:nk], p[:, kw - 128:kw], cmask[:], ALU.add, [pk, 'cmask'], ['Ssb'])
                        else:
                            cp('act', Ssb[:, k0:k0 + kw], p[:, 0:kw], [pk], ['Ssb'])
                    S.op('dve', lambda e, nk=nk: e.reduce_max(mx, Ssb[:, 0:nk], mybir.AxisListType.X), ['Ssb'], ['mx'])
                    ts('dve', mx, mx, -1.0, ALU.mult, ['mx'], ['mx'])
                    act(P_[:, 0:nk], Ssb[:, 0:nk], AF.Exp, ['Ssb', 'mx'], ['P_', 'rsum'], bias=mx, accum=rsum[:, 0:1])
                    S.op('dve', lambda e: e.reciprocal(rsum[:, 1:2], rsum[:, 0:1]), ['rsum'], ['rinv'])
                    nb = nk // 128
                    po, pok = pf()
                    for b4 in range(0, nb, 4):
                        nbb = min(4, nb - b4)
                        pt_, ptk = pb()
                        for i in range(nbb):
                            tr(pt_[:, i * 128:(i + 1) * 128], P_[:, (b4 + i) * 128:(b4 + i + 1) * 128], identB[:], ['P_', 'identB'], [ptk], inc=(i == nbb - 1))
                        pti = (b4 // 4) % 2
                        cp('act' if pti else 'dve', PT[pti][:, 0:nbb * 128], pt_[:, 0:nbb * 128], [ptk], [('PT', pti)])
                        for i in range(nbb):
                            bb = b4 + i
                            mm(po[:, 0:128], PT[pti][:, i * 128:(i + 1) * 128], Vh[:, bb, :], [('PT', pti), 'Vh'], [pok],
                               start=(bb == 0), stop=(bb == nb - 1))
                    S.op('act', lambda e, po=po: e.mul(osb[:, :], po[:, 0:128], rsum[:, 1:2]), [pok, 'rinv'], ['osb'])
                    pt_, ptk = pb()
                    tr(pt_[:, 0:128], osb[:, :], identB[:], ['osb', 'identB'], [ptk])
                    cp('dve', yT[:, 8 + h, qs], pt_[:, 0:128], [ptk], [('yT', 8 + h)])

        def phase_ffn(l, j):
            S.barrier()
            z = AF32(0, 16 * TT).rearrange("p (c t) -> p c t", c=16)
            o = 16 * TT
            h1 = [AB16(o, 4 * TT // 2).rearrange("p (c t) -> p c t", c=4), AB16(o + 4 * TT // 2, 4 * TT // 2).rearrange("p (c t) -> p c t", c=4)]
            o += 4 * TT
            qm = AB16(o, 4 * TT // 2).rearrange("p (h t) -> p h t", h=4); o += 4 * TT // 2
            om = AB16(o, 4 * TT // 2).rearrange("p (h t) -> p h t", h=4); o += 4 * TT // 2
            Pm_ = AB16(o, 128); o += 128
            PTm = AB16(o, 128); o += 128
            osb = AB16(o, 64); o += 64
            rt = AF32(o, TT); o += TT
            assert o <= ARENA_N
            yk = [('yT', c) for c in range(16)]
            for nb in range(4):
                buf, key = wload([(0, 16, 512, w_out_d[l][:, nb * 512:(nb + 1) * 512].rearrange("(k p) n -> p k n", p=128))])
                wv_ = wview(buf, 0, 16, 512)
                for i in range(4):
                    n = nb * 4 + i
                    p, pk = pf()
                    for c in range(16):
                        mm(p[:, :], wv_[:, c, i * 128:(i + 1) * 128], yT[:, c, :], [key, ('yT', c)], [pk], start=(c == 0), stop=(c == 15))
                    cp('act' if n % 2 else 'dve', z[:, n, :], p[:, :], [pk], ['z'])
            post_norm_res(z, 'z', 'mix_post_g')
            pre_norm('mem_pre_g')
            buf, key = wload([(0, 16, 512, wq_d[l].rearrange("(k p) n -> p k n", p=128))])
            wv_ = wview(buf, 0, 16, 512)
            SCM = float(128 ** -0.5)
            for h in range(4):
                p, pk = pf()
                for c in range(16):
                    mm(p[:, :], wv_[:, c, h * 128:(h + 1) * 128], uT[:, c, :], [key, ('uT', c)], [pk], start=(c == 0), stop=(c == 15))
                S.op('act', lambda e, p=p, h=h: e.mul(qm[:, h, :], p[:, :], SCM), [pk], [('qm', h)])
            rsum = small[:, 28:30]
            mx = small[:, 30:31]
            for h in range(4):
                for qb in range(4):
                    qs = slice(qb * 128, (qb + 1) * 128)
                    p, pk = pf()
                    mm(p[:, 0:NMEM], qm[:, h, qs], kmT[:, h, :], [('qm', h), 'kmT'], [pk])
                    S.op('dve', lambda e, p=p: e.reduce_max(mx, p[:, 0:NMEM], mybir.AxisListType.X), [pk], ['mxm'])
                    ts('dve', mx, mx, -1.0, ALU.mult, ['mxm'], ['mxm'])
                    act(Pm_[:, :], p[:, 0:NMEM], AF.Exp, [pk, 'mxm'], ['Pm_', 'rsm'], bias=mx, accum=rsum[:, 0:1])
                    S.op('dve', lambda e: e.reciprocal(rsum[:, 1:2], rsum[:, 0:1]), ['rsm'], ['rim'])
                    pt_, ptk = pb()
                    for i in range(2):
                        tr(pt_[:, i * 128:(i + 1) * 128], Pm_[:, i * 128:(i + 1) * 128], identB[:], ['Pm_', 'identB'], [ptk], inc=(i == 1))
                    cp('dve', PTm[:, :], pt_[:, 0:256], [ptk], ['PTm'])
                    po, pok = pf()
                    for i in range(2):
                        mm(po[:, 0:128], PTm[:, i * 128:(i + 1) * 128], vm[:, i, h * 128:(h + 1) * 128], ['PTm', 'vm'], [pok], start=(i == 0), stop=(i == 1))
                    S.op('act', lambda e, po=po: e.mul(osb[:, :], po[:, 0:128], rsum[:, 1:2]), [pok, 'rim'], ['osbm'])
                    pt_, ptk = pb()
                    tr(pt_[:, 0:128], osb[:, :], identB[:], ['osbm', 'identB'], [ptk])
                    cp('dve', om[:, h, qs], pt_[:, 0:128], [ptk], ['om'])
            buf, key = wload([(0, 4, 2048, wo_d[l].rearrange("(k p) n -> p k n", p=128))])
            wv_ = wview(buf, 0, 4, 2048)
            for n in range(16):
                p, pk = pf()
                for c in range(4):
                    mm(p[:, :], wv_[:, c, n * 128:(n + 1) * 128], om[:, c, :], [key, 'om'], [pk], start=(c == 0), stop=(c == 3))
                cp('act' if n % 2 else 'dve', z[:, n, :], p[:, :], [pk], ['z'])
            post_norm_res(z, 'z', 'mem_post_g')
            pre_norm('ffn_pre_g')
            for blk in range(16):
                buf, key = wload([(0, 16, 512, ff1_d[l][:, blk * 512:(blk + 1) * 512].rearrange("(k p) n -> p k n", p=128))])
                wv_ = wview(buf, 0, 16, 512)
                hb = h1[blk % 2]
                hk = ('h1', blk % 2)
                for i in range(4):
                    p, pk = pf()
                    for c in range(16):
                        mm(p[:, :], wv_[:, c, i * 128:(i + 1) * 128], uT[:, c, :], [key, ('uT', c)], [pk], start=(c == 0), stop=(c == 15))
                    act(rt, p[:, :], AF.Relu, [pk], ['rt'])
                    tt('pool', hb[:, i, :], rt, rt, ALU.mult, ['rt'], [hk])
                buf, key = wload([(0, 4, 2048, ff2_d[l][blk * 512:(blk + 1) * 512, :].rearrange("(k p) n -> p k n", p=128))])
                wv_ = wview(buf, 0, 4, 2048)
                for n in range(16):
                    p, pk = pf()
                    for c in range(4):
                        mm(p[:, :], wv_[:, c, n * 128:(n + 1) * 128], hb[:, c, :], [key, hk], [pk], start=(c == 0), stop=(c == 3))
                    if blk == 0:
                        cp('act' if n % 2 else 'dve', z[:, n, :], p[:, :], [pk], [('zf', n)])
                    else:
                        tt('dve', z[:, n, :], z[:, n, :], p[:, :], ALU.add, [pk, ('zf', n)], [('zf', n)])
            S.op('pool', lambda e: e.memset(small[:, 59:60], 0.0), [('zf', n) for n in range(16)], ['z'])
            post_norm_res(z, 'z', 'ffn_post_g')

        prep_mem()
        for l in range(L):
            layer_setup(l)
            for j in range(NT):
                S.barrier()
                load_tile(l, j)
                phase_rwkv(l, j)
                phase_mla(l, j)
                phase_ffn(l, j)
                store_tile(l, j)
        S.barrier()
        S.emit()
    return nc, cn


def make_in_maps(inputs, T, L, nb):
    cn = make_consts(T)
    maps = []
    f32 = lambda a: np.ascontiguousarray(np.asarray(a, np.float32))
    params = np.stack([pack_params(inputs, l) for l in range(L)], 0)
    shared = {
        "params": f32(params),
        "memg": f32(_fm(inputs['mem_norm_g'], 16)),
        "w_in": f32(inputs['w_in'][:L]),
        "w_in_vres": f32(inputs['w_in_vres'][:max(L - 1, 1)]),
        "w_up": f32(inputs['w_up'][:L]), "a_up": f32(inputs['a_up'][:L]), "g_up": f32(inputs['g_up'][:L]),
        "v_up": f32(inputs['v_up'][:max(L - 1, 1)]),
        "w_uq": f32(inputs['w_uq'][:L]), "w_ukv": f32(inputs['w_ukv'][:L]), "w_out": f32(inputs['w_out'][:L]),
        "wq_mem": f32(inputs['wq_mem'][:L]), "wk_mem": f32(inputs['wk_mem'][:L]), "wv_mem": f32(inputs['wv_mem'][:L]),
        "wo_mem": f32(inputs['wo_mem'][:L]), "w_ff1": f32(inputs['w_ff1'][:L]), "w_ff2": f32(inputs['w_ff2'][:L]),
    }
    for k, v in cn.items():
        shared["c_" + k] = f32(v)
    for b in range(nb):
        m = dict(shared)
        m["x"] = f32(inputs['x'][b][:T])
        m["mem"] = f32(inputs['mem'][b])
        maps.append(m)
    return maps


def kernel(**inputs):
    B, T, _ = inputs['x'].shape
    L = inputs['w_in'].shape[0]
    nc, _ = build(T, L)
    maps = make_in_maps(inputs, T, L, B)
    res = run_bass_kernel_spmd(nc, maps, core_ids=list(range(B)))
    return np.stack([np.asarray(res.results[b]["out"], np.float32) for b in range(B)], 0)
```

```python
import contextlib
import numpy as np
import concourse.bass as bass
import concourse.mybir as mybir
from concourse.bass_utils import run_bass_kernel_spmd

F32 = mybir.dt.float32
BF16 = mybir.dt.bfloat16
AF = mybir.ActivationFunctionType
ALU = mybir.AluOpType

D = 2048
TT = 512
NMEM = 256
RW = 1024
IN_W = 4352
DFF = 8192
GN_EPS = 64e-5
EPS = 1e-6
DECAY_C = -0.6065306597126334


class Sched:
    CE = ('pe', 'dve', 'act', 'pool')
    ENG = ('pe', 'dve', 'act', 'pool', 'sp')

    def __init__(self, nc, stack):
        self.nc = nc
        self.stack = stack
        self.q = {e: [] for e in self.ENG}
        self.semh = {}
        self.cnt = {}
        for e in self.CE:
            self._mksem('s_' + e)
        self.lastw = {}
        self.readers = {}
        self.seen = {e: {} for e in self.ENG}
        self.n_ops = 0

    def _mksem(self, name):
        self.semh[name] = self.stack.enter_context(self.nc.semaphore(name))
        self.cnt[name] = 0

    def _waits(self, eng, reads, writes, is_dma=False):
        need = {}
        own = None if is_dma else 's_' + eng

        def add(d, raw):
            if d is None:
                return
            if d[0] == own and not raw:
                return
            if need.get(d[0], 0) < d[1]:
                need[d[0]] = d[1]
        for k in reads:
            add(self.lastw.get(k), True)
        for k in writes:
            add(self.lastw.get(k), False)
            r = self.readers.get(k)
            if r:
                for s, v in r.items():
                    add((s, v), False)
        waits = []
        for s, v in need.items():
            if eng == 'pe' and s == 's_pe':
                continue
            if self.seen[eng].get(s, 0) < v:
                self.seen[eng][s] = v
                waits.append((s, v))
        return waits

    def _commit(self, ident, reads, writes):
        for k in writes:
            self.lastw[k] = ident
            self.readers[k] = {}
        for k in reads:
            r = self.readers.setdefault(k, {})
            if r.get(ident[0], 0) < ident[1]:
                r[ident[0]] = ident[1]

    def op(self, eng, fn, reads=(), writes=(), inc=True):
        waits = self._waits(eng, reads, writes)
        s = 's_' + eng
        if inc:
            self.cnt[s] += 1
            ident = (s, self.cnt[s])
        else:
            ident = (s, self.cnt[s] + 1)
        self._commit(ident, reads, writes)
        self.q[eng].append((waits, fn, (s, 1) if inc else None))
        self.n_ops += 1

    def dma(self, eng, out, in_, reads=(), writes=(), semkey=None, slow=False):
        assert semkey is not None
        s = 'd_' + semkey
        if s not in self.semh:
            self._mksem(s)
        waits = self._waits(eng, reads, writes, is_dma=True)
        self.cnt[s] += 16
        ident = (s, self.cnt[s])
        self._commit(ident, reads, writes)
        if slow:
            fn = lambda e: e.dma_start(out=out, in_=in_, allow_slow_non_contiguous=True)
        else:
            fn = lambda e: e.dma_start(out=out, in_=in_)
        self.q[eng].append((waits, fn, (s, 16)))
        self.n_ops += 1

    def barrier(self, engines=None):
        for e in (engines or self.ENG):
            waits = []
            for s, c in self.cnt.items():
                if c > 0 and self.seen[e].get(s, 0) < c:
                    if e == 'pe' and s == 's_pe':
                        continue
                    self.seen[e][s] = c
                    waits.append((s, c))
            if waits:
                self.q[e].append((waits, None, None))

    def emit(self):
        engobj = {'pe': 'tensor', 'dve': 'vector', 'act': 'scalar', 'pool': 'gpsimd', 'sp': 'sync'}
        with self.nc.Block() as block:
            for e in self.ENG:
                items = self.q[e]
                if not items:
                    continue

                def body(eo, items=items):
                    for waits, fn, inc in items:
                        for s, v in waits:
                            eo.wait_ge(self.semh[s], v)
                        if fn is not None:
                            ins = fn(eo)
                            if inc is not None:
                                ins.then_inc(self.semh[inc[0]], inc[1])
                getattr(block, engobj[e])(body)


PCOL = {}
_off = 0
for _n, _w in [('mix_pre_g', 16), ('mix_post_g', 16), ('mem_pre_g', 16), ('mem_post_g', 16), ('ffn_pre_g', 16),
               ('ffn_post_g', 16), ('mu_r', 8), ('mu_k', 8), ('mu_v', 8), ('mu_wl', 1), ('mu_al', 1), ('mu_gl', 2),
               ('mu_vr', 1), ('w0', 8), ('a0', 8), ('v0', 8), ('k_k', 8), ('k_a', 8), ('r_k', 8), ('lnx_g', 8),
               ('lnx_b', 8), ('qn_g', 4), ('kvn_g', 2)]:
    PCOL[_n] = (_off, _w)
    _off += _w
NP_RAW = _off
MU_COLS = PCOL['mu_r'][0]
N_MU = 29
PCOL['om'] = (NP_RAW, N_MU)
PCOL['omka'] = (NP_RAW + N_MU, 8)
NPAR = NP_RAW + N_MU + 8


def _fm(v, ncol):
    buf = np.zeros(ncol * 128, np.float32)
    v = np.asarray(v, np.float32).reshape(-1)
    buf[:v.size] = v
    return buf.reshape(ncol, 128).T


def pack_params(inp, l):
    P = np.zeros((128, NPAR), np.float32)

    def put(name, arr):
        o, w = PCOL[name]
        P[:, o:o + w] = arr
    for n in ['mix_pre_g', 'mix_post_g', 'mem_pre_g', 'mem_post_g', 'ffn_pre_g', 'ffn_post_g']:
        put(n, _fm(inp[n][l], 16))
    mu = inp['mu_rwkv'][l]
    put('mu_r', _fm(mu[0:1024], 8))
    put('mu_k', _fm(mu[1024:2048], 8))
    put('mu_v', _fm(mu[2048:3072], 8))
    put('mu_wl', _fm(mu[3072:3168], 1))
    put('mu_al', _fm(mu[3168:3264], 1))
    put('mu_gl', _fm(mu[3264:3520], 2))
    if l > 0:
        put('mu_vr', _fm(inp['mu_vres'][l - 1], 1))
        put('v0', _fm(inp['v0'][l - 1], 8))
    for n in ['w0', 'a0', 'k_k', 'k_a', 'lnx_g', 'lnx_b']:
        put(n, _fm(inp[n][l], 8))
    put('r_k', _fm(inp['r_k'][l].reshape(-1), 8))
    put('qn_g', _fm(inp['q_norm_g'][l], 4))
    put('kvn_g', _fm(inp['kv_norm_g'][l], 2))
    return P


def make_consts(T):
    c = {}
    c['identF'] = np.eye(128, dtype=np.float32)
    bo = np.zeros((128, 128), np.float32)
    bo[:64, :64] = 1.0
    bo[64:, 64:] = 1.0
    c['blockones'] = bo
    r = np.arange(128)
    mk1 = np.zeros((128, 192), np.float32)
    mk1[:, :128] = (r[:, None] < r[None, :]).astype(np.float32)
    mk1[:, 128:] = ((r[:, None] % 64) <= np.arange(64)[None, :]).astype(np.float32)
    c['mk1'] = mk1
    c['mk2'] = (r[:, None] > r[None, :]).astype(np.float32)
    c['cmask'] = np.where(r[:, None] >= r[None, :], 0.0, -1e9).astype(np.float32)
    rs = np.ones((128, TT), np.float32)
    rs[:, ::64] = 0.0
    c['reset'] = rs
    pos = np.arange(T, dtype=np.float32)
    inv_freq = (10000.0 ** (-np.arange(0, 64, 2, dtype=np.float32) / 64)).astype(np.float32)
    ang = (pos[:, None] * inv_freq[None, :]).astype(np.float32)
    cs, sn = np.cos(ang).T.astype(np.float32), np.sin(ang).T.astype(np.float32)
    c['rope'] = np.stack([np.concatenate([cs, cs], 0), np.concatenate([-sn, sn], 0)], 0).astype(np.float32)
    return c


def build(T, L, taps=()):
    NT = T // TT
    nc = bass.Bass("TRN2", target_bir_lowering=False)
    din = lambda name, shape, dt=F32: nc.dram_tensor(name, list(shape), dt, kind="ExternalInput").ap()
    x_d = din("x", [T, D])
    mem_d = din("mem", [NMEM, D])
    memg_d = din("memg", [128, 16])
    par_d = din("params", [L, 128, NPAR])
    w_in_d = din("w_in", [L, D, IN_W])
    w_vres_d = din("w_in_vres", [max(L - 1, 1), D, 64])
    w_up_d = din("w_up", [L, 96, RW])
    a_up_d = din("a_up", [L, 96, RW])
    g_up_d = din("g_up", [L, 256, RW])
    v_up_d = din("v_up", [max(L - 1, 1), 64, RW])
    w_uq_d = din("w_uq", [L, 512, 1536])
    w_ukv_d = din("w_ukv", [L, 256, 2048])
    w_out_d = din("w_out", [L, D, D])
    wq_d = din("wq_mem", [L, D, 512])
    wk_d = din("wk_mem", [L, D, 512])
    wv_d = din("wv_mem", [L, D, 512])
    wo_d = din("wo_mem", [L, 512, D])
    ff1_d = din("w_ff1", [L, D, DFF])
    ff2_d = din("w_ff2", [L, DFF, D])
    cn = make_consts(T)
    cd = {k: din("c_" + k, v.shape) for k, v in cn.items()}
    out_d = nc.dram_tensor("out", [T, D], F32, kind="ExternalOutput").ap()
    tap_d = {}
    for name, shape in taps:
        tap_d[name] = nc.dram_tensor("tap_" + name, list(shape), F32, kind="ExternalOutput").ap()
    hT_d = nc.dram_tensor("hT_s", [D, T], F32).ap()
    vf_d = nc.dram_tensor("vf_s", [RW, T], F32).ap()
    kn_d = nc.dram_tensor("kn_s", [8, 128, T], BF16).ap()
    kr_d = nc.dram_tensor("kr_s", [64, T], BF16).ap()
    vt_d = nc.dram_tensor("vt_s", [T, 1024], BF16).ap()
    memT_d = nc.dram_tensor("memT_s", [128, 16 * NMEM], BF16).ap()

    with contextlib.ExitStack() as st:
        S = Sched(nc, st)
        sb = lambda name, shape, dt=F32: st.enter_context(nc.sbuf_tensor(name, list(shape), dt))
        pst = lambda name, shape, dt=F32: st.enter_context(nc.psum_tensor(name, list(shape), dt))

        identF = sb("identF", [128, 128])
        identB = sb("identB", [128, 128], BF16)
        onesB = sb("onesB", [128, 128], BF16)
        blockones = sb("blockones", [128, 128])
        gnones = sb("gnones", [128, 128])
        mk1 = sb("mk1", [128, 192])
        mk2 = sb("mk2", [128, 128])
        cmask = sb("cmask", [128, 128])
        reset = sb("reset", [128, TT])
        hT = sb("hT", [128, 16, TT])
        uT = sb("uT", [128, 16, TT], BF16)
        yT = sb("yT", [128, 16, TT], BF16)
        wb = [sb("wb0", [128, 16 * 512], BF16), sb("wb1", [128, 16 * 512], BF16)]
        par = sb("par", [128, NPAR])
        lw = sb("lw", [128, 5, RW], BF16)
        kmT = sb("kmT", [128, 4, NMEM], BF16)
        vm = sb("vm", [128, 2, 512], BF16)
        Sbd = sb("Sbd", [128, 8, 2, 128])
        carry = sb("carry", [128, N_MU])
        rstd = sb("rstd", [128, TT])
        small = sb("small", [128, 64])
        ARENA_N = 20480
        arena = sb("arena", [128, ARENA_N])

        psF = [pst("psF%d" % i, [128, 512]) for i in range(6)]
        psB = [pst("psB%d" % i, [128, 1024], BF16) for i in range(2)]
        pctr = {'f': 0, 'b': 0}

        pmode = {'banks': list(range(6))}

        def pf():
            bl = pmode['banks']
            i = bl[pctr['f'] % len(bl)]
            pctr['f'] += 1
            return psF[i], ('psF', i)

        pctr['q'] = 0

        def pq():
            i = pctr['q'] % 16
            pctr['q'] += 1
            b_, q_ = i // 4, i % 4
            return psF[b_][:, q_ * 128:(q_ + 1) * 128], ('psq', b_, q_)

        def run_jobs(jobs):
            loaded = {0: wload(jobs[0][0])}
            for i, (parts, fn) in enumerate(jobs):
                if i + 1 < len(jobs):
                    loaded[i + 1] = wload(jobs[i + 1][0])
                fn(*loaded.pop(i))

        def pb():
            i = pctr['b'] % 2
            pctr['b'] += 1
            return psB[i], ('psB', i)

        def pc(name):
            o, w = PCOL[name]
            return o

        def mm(out, lhsT, rhs, r, w, start=True, stop=True, inc=None):
            S.op('pe', lambda e: e.matmul(out, lhsT, rhs, start=start, stop=stop), r, w, inc=(stop if inc is None else inc))

        def tr(out, in_, ident, r, w, inc=True):
            S.op('pe', lambda e: e.transpose(out, in_, ident), r, w, inc=inc)

        def act(out, in_, func, r, w, bias=0.0, scale=1.0, accum=None):
            if accum is None:
                S.op('act', lambda e: e.activation(out, in_, func, bias=bias, scale=scale), r, w)
            else:
                S.op('act', lambda e: e.activation(out, in_, func, bias=bias, scale=scale, accum_out=accum), r, w)

        def cp(eng, out, in_, r, w):
            if eng == 'act':
                S.op('act', lambda e: e.copy(out, in_), r, w)
            else:
                S.op(eng, lambda e: e.tensor_copy(out, in_), r, w)

        def tt(eng, out, a, b, op, r, w):
            S.op(eng, lambda e: e.tensor_tensor(out, a, b, op), r, w)

        def ts(eng, out, a, s1, op0, r, w, s2=None, op1=None):
            if op1 is None:
                S.op(eng, lambda e: e.tensor_scalar(out, a, s1, None, op0), r, w)
            else:
                S.op(eng, lambda e: e.tensor_scalar(out, a, s1, s2, op0, op1), r, w)

        def stt(out, in0, scalar, in1, op0, op1, r, w):
            S.op('dve', lambda e: e.scalar_tensor_tensor(out, in0, scalar, in1, op0, op1), r, w)

        def memset(eng, ap, val, w):
            S.op(eng, lambda e: e.memset(ap, val), (), w)

        def tap(name, src_ap, r, dst=None):
            if name in tap_d:
                S.dma('sp', tap_d[name] if dst is None else dst, src_ap, reads=r, semkey='tap_' + name)

        def AF32(off, n):
            return arena[:, off:off + n]

        def AB16(off, n):
            return arena[:, off:off + n].bitcast(BF16)

        for nm, t in [('identF', identF), ('blockones', blockones), ('mk1', mk1), ('mk2', mk2), ('cmask', cmask),
                      ('reset', reset)]:
            S.dma('sp', t[:], cd[nm], writes=[nm], semkey='c_' + nm)
        cp('dve', identB[:], identF[:], ['identF'], ['identB'])
        memset('dve', onesB[:], 1.0, ['onesB'])
        ts('dve', gnones[:], blockones[:], 1.0 / 64.0, ALU.mult, ['blockones'], ['gnones'])

        wctr = {'i': 0}

        def wload(parts):
            i = wctr['i'] % 2
            wctr['i'] += 1
            key = ('wb', i)
            for (off, kc, ncols, src) in parts:
                dst = wb[i][:, off:off + kc * ncols].rearrange("p (k n) -> p k n", k=kc)
                S.dma('pool', dst, src, writes=[key], semkey='wb%d' % i)
            return wb[i], key

        def wview(buf, off, kc, ncols):
            return buf[:, off:off + kc * ncols].rearrange("p (k n) -> p k n", k=kc)

        def rms_stats(src_fn, KC, dn, eps, rkeys, sq_buf=None, sqkey=None):
            sq = uT if sq_buf is None else sq_buf
            sqk = 'uT' if sqkey is None else sqkey
            for c in range(KC):
                act(sq[:, c, :], src_fn(c), AF.Square, rkeys, [(sqk, c)])
            p, pk = pf()
            for c in range(KC):
                mm(p[:, :], onesB[:], sq[:, c, :], ['onesB', (sqk, c)], [pk], start=(c == 0), stop=(c == KC - 1))
            act(rstd[:], p[:, :], AF.Sqrt, [pk], ['rstd'], bias=eps, scale=1.0 / dn)
            S.op('dve', lambda e: e.reciprocal(rstd[:], rstd[:]), ['rstd'], ['rstd'])

        def pre_norm(gname):
            rms_stats(lambda c: hT[:, c, :], 16, float(D), EPS, ['hT'])
            g0 = pc(gname)
            for c in range(16):
                stt(uT[:, c, :], hT[:, c, :], par[:, g0 + c:g0 + c + 1], rstd[:], ALU.mult, ALU.mult,
                    ['hT', 'par', 'rstd', ('uT', c)], [('uT', c)])

        def post_norm_res(z, zkey, gname):
            rms_stats(lambda c: z[:, c, :], 16, float(D), EPS, [zkey])
            g0 = pc(gname)
            for c in range(16):
                stt(z[:, c, :], z[:, c, :], par[:, g0 + c:g0 + c + 1], rstd[:], ALU.mult, ALU.mult,
                    [zkey, 'par', 'rstd'], [(zkey, 'n', c)])
                tt('pool', hT[:, c, :], hT[:, c, :], z[:, c, :], ALU.add, [(zkey, 'n', c), 'hT'], ['hT'])
            S.op('pool', lambda e: e.memset(small[:, 60:61], 0.0), [(zkey, 'n', c) for c in range(16)], [zkey])

        def prep_mem():
            mt = AF32(0, 2 * D).rearrange("p (b d) -> p b d", b=2)
            S.dma('sp', mt, mem_d.rearrange("(b p) d -> p b d", p=128), writes=['mt'], semkey='mt')
            mg = small[:, 0:16]
            S.dma('sp', mg, memg_d, writes=['mg'], semkey='mg')
            mf = AF32(2 * D, 16 * NMEM).rearrange("p (c t) -> p c t", c=16)
            for b in range(2):
                for c4 in range(4):
                    p, pk = pf()
                    for i in range(4):
                        c = c4 * 4 + i
                        tr(p[:, i * 128:(i + 1) * 128], mt[:, b, c * 128:(c + 1) * 128], identF[:], ['mt', 'identF'], [pk], inc=(i == 3))
                    cp('act', mf[:, c4 * 4:(c4 + 1) * 4, b * 128:(b + 1) * 128],
                       p[:, :].rearrange("p (i t) -> p i t", i=4), [pk], [('mf', b, c4)])
            mfk = [('mf', b, c4) for b in range(2) for c4 in range(4)]
            sq = AB16(2 * D + 16 * NMEM, 16 * NMEM // 2).rearrange("p (c t) -> p c t", c=16)
            for c in range(16):
                act(sq[:, c, :], mf[:, c, :], AF.Square, mfk, [('msq', c)])
            p, pk = pf()
            for c in range(16):
                mm(p[:, 0:NMEM], onesB[:], sq[:, c, :], ['onesB', ('msq', c)], [pk], start=(c == 0), stop=(c == 15))
            act(rstd[:, 0:NMEM], p[:, 0:NMEM], AF.Sqrt, [pk], ['rstd'], bias=EPS, scale=1.0 / D)
            S.op('dve', lambda e: e.reciprocal(rstd[:, 0:NMEM], rstd[:, 0:NMEM]), ['rstd'], ['rstd'])
            memT = AB16(2 * D + 16 * NMEM + 16 * NMEM // 2, 16 * NMEM // 2).rearrange("p (c t) -> p c t", c=16)
            for c in range(16):
                stt(memT[:, c, :], mf[:, c, :], mg[:, c:c + 1], rstd[:, 0:NMEM], ALU.mult, ALU.mult,
                    mfk + ['mg', 'rstd'], ['memT'])
            S.dma('sp', memT_d, AB16(2 * D + 16 * NMEM + 16 * NMEM // 2, 16 * NMEM // 2), reads=['memT'], writes=['memT_d'], semkey='memTs')

        def layer_setup(l):
            S.barrier()
            S.dma('sp', par[:], par_d[l], writes=['par'], semkey='par')
            o, w = PCOL['om']
            ts('dve', par[:, o:o + N_MU], par[:, MU_COLS:MU_COLS + N_MU], -1.0, ALU.mult, ['par'], ['par'], s2=1.0, op1=ALU.add)
            o2, _ = PCOL['omka']
            ka = pc('k_a')
            ts('dve', par[:, o2:o2 + 8], par[:, ka:ka + 8], -1.0, ALU.mult, ['par'], ['par'], s2=1.0, op1=ALU.add)
            S.dma('pool', lw[0:96, 0, :], w_up_d[l], writes=['lwts'], semkey='lw')
            S.dma('pool', lw[0:96, 1, :], a_up_d[l], writes=['lwts'], semkey='lw')
            S.dma('pool', lw[:, 2:4, :], g_up_d[l].rearrange("(k p) n -> p k n", p=128), writes=['lwts'], semkey='lw')
            if l > 0:
                S.dma('pool', lw[0:64, 4, :], v_up_d[l - 1], writes=['lwts'], semkey='lw')
            memset('dve', carry[:], 0.0, ['carry'])
            memset('dve', Sbd[:], 0.0, ['Sbd'])
            memT = AB16(0, 16 * NMEM // 2).rearrange("p (c t) -> p c t", c=16)
            S.dma('sp', AB16(0, 16 * NMEM // 2), memT_d, reads=['memT_d'], writes=['memT'], semkey='memTl')
            buf, key = wload([(0, 16, 512, wk_d[l].rearrange("(k p) n -> p k n", p=128))])
            wv = wview(buf, 0, 16, 512)
            for h in range(4):
                p, pk = pf()
                for c in range(16):
                    mm(p[:, 0:NMEM], wv[:, c, h * 128:(h + 1) * 128], memT[:, c, :], [key, 'memT'], [pk], start=(c == 0), stop=(c == 15))
                cp('act', kmT[:, h, :], p[:, 0:NMEM], [pk], ['kmT'])
            buf, key = wload([(0, 16, 512, wv_d[l].rearrange("(k p) n -> p k n", p=128))])
            wv = wview(buf, 0, 16, 512)
            for b in range(2):
                p, pk = pf()
                for c in range(16):
                    mm(p[:, :], memT[:, c, b * 128:(b + 1) * 128], wv[:, c, :], [key, 'memT'], [pk], start=(c == 0), stop=(c == 15))
                cp('act', vm[:, b, :], p[:, :], [pk], ['vm'])

        def load_tile(l, j):
            if l == 0:
                xs = AF32(0, D)
                for blk in range(4):
                    S.dma('sp', xs, x_d[j * TT + blk * 128: j * TT + (blk + 1) * 128, :], writes=['xs'], semkey='xs')
                    for c4 in range(4):
                        p, pk = pf()
                        for i in range(4):
                            c = c4 * 4 + i
                            tr(p[:, i * 128:(i + 1) * 128], xs[:, c * 128:(c + 1) * 128], identF[:], ['xs', 'identF'], [pk], inc=(i == 3))
                        cp('act' if c4 % 2 else 'dve', hT[:, c4 * 4:(c4 + 1) * 4, blk * 128:(blk + 1) * 128],
                           p[:, :].rearrange("p (i t) -> p i t", i=4), [pk], [('hT', 'ld', blk, c4)])
                S.op('pool', lambda e: e.memset(small[:, 61:62], 0.0), [('hT', 'ld', blk, c4) for blk in range(4) for c4 in range(4)], ['hT'])
            else:
                S.dma('sp', hT[:], hT_d[:, j * TT:(j + 1) * TT].rearrange("(c p) t -> p c t", p=128),
                      reads=[('hT_d', j)], writes=['hT'], semkey='hT')

        def store_tile(l, j):
            if l == L - 1:
                S.barrier()
                os_ = AF32(0, D)
                for blk in range(4):
                    for c4 in range(4):
                        p, pk = pf()
                        for i in range(4):
                            c = c4 * 4 + i
                            tr(p[:, i * 128:(i + 1) * 128], hT[:, c, blk * 128:(blk + 1) * 128], identF[:], ['hT', 'identF'], [pk], inc=(i == 3))
                        cp('act' if c4 % 2 else 'dve', os_[:, c4 * 512:(c4 + 1) * 512], p[:, :], [pk], [('os', c4)])
                    S.dma('sp', out_d[j * TT + blk * 128: j * TT + (blk + 1) * 128, :], os_,
                          reads=[('os', c4) for c4 in range(4)], writes=[('out', j, blk)], semkey='os')
            else:
                S.dma('sp', hT_d[:, j * TT:(j + 1) * TT].rearrange("(c p) t -> p c t", p=128), hT[:],
                      reads=['hT'], writes=[('hT_d', j)], semkey='hT')

        def phase_rwkv(l, j):
            S.barrier()
            o = 0

            def slot(n):
                nonlocal o
                a_ = (o, n)
                o += n
                return a_
            s_ar = slot(8 * 192)
            s_bt = slot(8 * 128)
            s_kt = slot(8 * 128)
            s_bh = slot(8 * 128)
            s_kh = slot(8 * 128)
            s_vh = slot(8 * 128)
            DEAD = ['r', 'v', 'kk', 'a', 'lw', 'G', 'e1', 'e2', 'k2', 'b']
            names = DEAD + ['k', 'g', 'bon', 't1', 't2']
            dead0 = o
            sl = {n: slot(TT) for n in names}
            s_ymu = [slot(TT + 1), slot(TT + 1)]
            s_x = [slot(128) for _ in range(2)]
            s_u = [slot(128) for _ in range(2)]
            tw = AB16(o, TT // 2); o += TT // 2
            alb = AB16(o, TT // 2); o += TT // 2
            sg = AB16(o, TT).rearrange("p (k t) -> p k t", k=2); o += TT
            vrb = AB16(o, TT // 2); o += TT // 2
            assert o <= ARENA_N, o
            AR = AF32(*s_ar).rearrange("p (c n) -> p c n", c=8)
            BT = AF32(*s_bt).rearrange("p (c n) -> p c n", c=8)
            KT = AF32(*s_kt).rearrange("p (c n) -> p c n", c=8)
            BH = AF32(*s_bh).rearrange("p (c n) -> p c n", c=8)
            KH = AF32(*s_kh).rearrange("p (c n) -> p c n", c=8)
            VH = AF32(*s_vh).rearrange("p (c n) -> p c n", c=8)
            V = {n: AF32(*sl[n]) for n in names}
            CB = {'dead0': dead0}
            for c in range(8):
                base = dead0 + c * 640
                CB[c] = dict(AB=AF32(base, 192), AK=AF32(base + 192, 192), Pt=AF32(base + 384, 128), Mm=AF32(base + 512, 128))
            ck8 = lambda nm: [(nm, c) for c in range(8)]
            for nm, t in [('AR', AR), ('BT', BT), ('KT', KT), ('BH', BH), ('KH', KH), ('VH', VH)]:
                memset('pool', t, 0.0, ck8(nm))

            pre_norm('mix_pre_g')
            ymu_i = [0]

            def shift_evac(p, pk, M, cid, out_ap, wkeys):
                yb = AF32(*s_ymu[ymu_i[0] % 2])
                yk = ('ymu', ymu_i[0] % 2)
                ymu_i[0] += 1
                cp('pool', yb[0:M, 0:1], carry[0:M, cid:cid + 1], ['carry'], [yk])
                S.op('act', lambda e: e.mul(yb[0:M, 1:TT + 1], p, par[0:M, MU_COLS + cid:MU_COLS + cid + 1]), [pk, 'par', yk], [yk])
                oo = PCOL['om'][0]
                stt(out_ap, p, par[0:M, oo + cid:oo + cid + 1], yb[0:M, 0:TT], ALU.mult, ALU.add, [pk, 'par', yk], wkeys)
                cp('pool', carry[0:M, cid:cid + 1], yb[0:M, TT:TT + 1], [yk], ['carry'])

            def proj(lhs_fn, M, rkeys):
                p, pk = pf()
                for c in range(16):
                    mm(p[0:M, :], lhs_fn(c), uT[:, c, :], rkeys + [('uT', c)], [pk], start=(c == 0), stop=(c == 15))
                return p[0:M, :], pk
            t1 = V['t1']

            def lora_job(buf, key):
                wl_ = wview(buf, 0, 16, 448)
                wvr = wview(buf, 16 * 448, 16, 64)
                p, pk = proj(lambda c: wl_[:, c, 0:96], 96, [key])
                shift_evac(p, pk, 96, 24, t1[0:96, :], ['t1'])
                act(tw[0:96, :], t1[0:96, :], AF.Tanh, ['t1'], ['tw'])
                p, pk = proj(lambda c: wl_[:, c, 96:192], 96, [key])
                shift_evac(p, pk, 96, 25, alb[0:96, :], ['alb'])
                for i in range(2):
                    p, pk = proj(lambda c, i=i: wl_[:, c, 192 + 128 * i:320 + 128 * i], 128, [key])
                    shift_evac(p, pk, 128, 26 + i, t1[:, :], ['t1'])
                    act(sg[:, i, :], t1[:, :], AF.Sigmoid, ['t1'], ['sg'])
                if l > 0:
                    p, pk = proj(lambda c: wvr[:, c, :], 64, [key])
                    shift_evac(p, pk, 64, 28, vrb[0:64, :], ['vrb'])

            def pair_job(pr):
                def fn(buf, key):
                    for i, nm in enumerate(['r', 'k', 'v']):
                        wv_ = wview(buf, i * 16 * 128, 16, 128)
                        p, pk = proj(lambda c, wv_=wv_: wv_[:, c, :], 128, [key])
                        shift_evac(p, pk, 128, 8 * i + pr, V[nm], [nm])
                    rwkv_pair(l, j, pr, V, AR, BT, KT, BH, KH, VH, tw, alb, sg, vrb, CB, DEAD, s_x, s_u)
                return fn
            parts = [(0, 16, 448, w_in_d[l][:, 3072:3520].rearrange("(k p) n -> p k n", p=128))]
            if l > 0:
                parts.append((16 * 448, 16, 64, w_vres_d[l - 1].rearrange("(k p) n -> p k n", p=128)))
            jobs = [(parts, lora_job)]
            for pr in range(8):
                parts = [(i * 16 * 128, 16, 128, w_in_d[l][:, i * 1024 + pr * 128: i * 1024 + (pr + 1) * 128].rearrange("(k p) n -> p k n", p=128))
                         for i in range(3)]
                jobs.append((parts, pair_job(pr)))
            run_jobs(jobs)
            pmode['banks'] = list(range(6))

        def rwkv_pair(l, j, pr, V, AR, BT, KT, BH, KH, VH, tw, alb, sg, vrb, CB, DEAD, s_x, s_u):
            cs = slice(pr * 128, (pr + 1) * 128)
            col = lambda n: par[:, pc(n) + pr:pc(n) + pr + 1]
            r, k, v, kk, a, lwv, G, e1, e2, g, bon, k2, b, t1, t2 = [V[n] for n in
                ['r', 'k', 'v', 'kk', 'a', 'lw', 'G', 'e1', 'e2', 'g', 'bon', 'k2', 'b', 't1', 't2']]
            O = k
            ck8 = lambda nm: [(nm, c) for c in range(8)]
            p, pk = pf()
            mm(p[:, :], lw[0:96, 0, cs], tw[0:96, :], ['lwts', 'tw'], [pk])
            act(lwv, p[:, :], AF.Sigmoid, [pk, 'par'], ['lw'], bias=col('w0'))
            ts('pool', lwv, lwv, DECAY_C, ALU.mult, ['lw'], ['lw'])
            p, pk = pf()
            mm(p[:, :], lw[0:96, 1, cs], alb[0:96, :], ['lwts', 'alb'], [pk])
            act(a, p[:, :], AF.Sigmoid, [pk, 'par'], ['a'], bias=col('a0'))
            p, pk = pf()
            for i in range(2):
                mm(p[:, :], lw[:, 2 + i, cs], sg[:, i, :], ['lwts', 'sg'], [pk], start=(i == 0), stop=(i == 1))
            cp('act', g, p[:, :], [pk], ['g'])
            if l > 0:
                p, pk = pf()
                mm(p[:, :], lw[0:64, 4, cs], vrb[0:64, :], ['lwts', 'vrb'], [pk])
                act(t1, p[:, :], AF.Sigmoid, [pk, 'par'], ['t1'], bias=col('v0'))
                S.dma('sp', t2, vf_d[pr * 128:(pr + 1) * 128, j * TT:(j + 1) * TT], reads=[('vf_d', pr, j)], writes=['t2'], semkey='vfl')
                tt('dve', t2, t2, v, ALU.subtract, ['t2', 'v'], ['t2'])
                tt('dve', t2, t2, t1, ALU.mult, ['t2', 't1'], ['t2'])
                tt('dve', v, v, t2, ALU.add, ['v', 't2'], ['v'])
            else:
                S.dma('sp', vf_d[pr * 128:(pr + 1) * 128, j * TT:(j + 1) * TT], v, reads=['v'], writes=[('vf_d', pr, j)], semkey='vfs')
            ts('pool', kk, k, col('k_k'), ALU.mult, ['k', 'par'], ['kk'])
            tt('pool', t1, kk, kk, ALU.mult, ['kk'], ['t1'])
            p, pk = pf()
            mm(p[:, :], blockones[:], t1, ['blockones', 't1'], [pk])
            act(t2, p[:, :], AF.Sqrt, [pk], ['t2'])
            ts('dve', t2, t2, 1e-12, ALU.max, ['t2'], ['t2'])
            S.op('dve', lambda e: e.reciprocal(t2, t2), ['t2'], ['t2'])
            tt('dve', kk, kk, t2, ALU.mult, ['kk', 't2'], ['kk'])
            oka = PCOL['omka'][0]
            ts('dve', t1, a, col('k_a'), ALU.mult, ['a', 'par'], ['t1'], s2=par[:, oka + pr:oka + pr + 1], op1=ALU.add)
            tt('dve', k2, k, t1, ALU.mult, ['k', 't1'], ['k2'])
            tt('pool', b, kk, a, ALU.mult, ['kk', 'a'], ['b'])
            stt(t1, r, col('r_k'), k2, ALU.mult, ALU.mult, ['r', 'par', 'k2'], ['t1'])
            p, pk = pf()
            mm(p[:, :], blockones[:], t1, ['blockones', 't1'], [pk])
            tt('dve', bon, p[:, :], v, ALU.mult, [pk, 'v'], ['bon'])
            S.op('dve', lambda e: e.tensor_tensor_scan(G, reset[:], lwv, 0.0, ALU.mult, ALU.add), ['reset', 'lw'], ['G'])
            c3 = lambda ap: ap.rearrange("p (c t) -> p c t", c=8)
            hs = [(slice(0, 64), slice(0, 64)), (slice(64, 128), slice(64, 128))]
            act(e1, G, AF.Exp, ['G'], ['e1'])
            tt('dve', AR[:, :, 128:192], c3(r), c3(e1), ALU.mult, ['r', 'e1'], ck8('AR'))
            tt('pool', e2, G, lwv, ALU.subtract, ['G', 'lw'], ['e2'])
            act(e2, e2, AF.Exp, ['e2'], ['e2'])
            for ps_, fs_ in hs:
                stt(AR[ps_, :, fs_], c3(kk)[ps_], -1.0, c3(e2)[ps_], ALU.mult, ALU.mult, ['kk', 'e2'], ck8('AR'))
            act(e1, G, AF.Exp, ['G'] + ck8('AR'), ['e1'], scale=-1.0)
            for ps_, fs_ in hs:
                tt('dve', BT[ps_, :, fs_], c3(b)[ps_], c3(e1)[ps_], ALU.mult, ['b', 'e1'], ck8('BT'))
                tt('pool', KT[ps_, :, fs_], c3(k2)[ps_], c3(e1)[ps_], ALU.mult, ['k2', 'e1'], ck8('KT'))
            for c in range(8):
                act(e2[:, c * 64:(c + 1) * 64], G[:, c * 64:(c + 1) * 64], AF.Exp, ['G', 'e2'], ['e2'],
                    bias=G[:, c * 64 + 63:c * 64 + 64], scale=-1.0)
            for ps_, fs_ in hs:
                tt('dve', BH[ps_, :, fs_], c3(b)[ps_], c3(e2)[ps_], ALU.mult, ['b', 'e2'], ck8('BH'))
                tt('pool', KH[ps_, :, fs_], c3(k2)[ps_], c3(e2)[ps_], ALU.mult, ['k2', 'e2'], ck8('KH'))
                cp('pool', VH[ps_, :, fs_], c3(v)[ps_], ['v'], ck8('VH'))
            WC = small[:, 16:24]
            act(WC, G.rearrange("p (c t) -> p c t", c=8)[:, :, 63], AF.Exp, ['G'], ['WC'])
            if l == 0 and j == 0 and pr == 0:
                tap('v', v, ['v'])
                tap('r', r, ['r'])
                tap('kk', kk, ['kk'])
                tap('G', G, ['G'])

            cbk = [(nm, g_) for nm in ('AB', 'AK', 'Pt', 'Mm') for g_ in range(2)]
            S.op('pool', lambda e: e.memset(small[:, 58:59], 0.0), [], list(DEAD) + cbk)

            def gview(g_, field, lo, hi):
                off = {'AB': 0, 'AK': 192, 'Pt': 384, 'Mm': 512}[field]
                base = CB['dead0'] + g_ * 4 * 640
                return arena[:, base:base + 4 * 640].rearrange("p (c n) -> p c n", n=640)[:, :, off + lo:off + hi]
            bc4 = lambda t_: t_[:].rearrange("p (o n) -> p o n", o=1).to_broadcast([128, 4, 128])

            for c in range(8):
                p, pk = pf()
                tr(p[:, 0:128], BH[:, c, :], identF[:], [('BH', c), 'identF'], [pk], inc=False)
                tr(p[:, 128:256], KH[:, c, :], identF[:], [('KH', c), 'identF'], [pk], inc=False)
                tr(p[:, 256:384], VH[:, c, :], identF[:], [('VH', c), 'identF'], [pk], inc=True)
                cp('act', BH[:, c, :], p[:, 0:128], [pk], [('BH', c)])
                cp('act', KH[:, c, :], p[:, 128:256], [pk], [('KH', c)])
                cp('act', VH[:, c, :], p[:, 256:384], [pk], [('VH', c)])
            for c in range(8):
                g_ = c // 4
                p, pk = pf()
                mm(p[:, 0:192], BT[:, c, :], AR[:, c, :], [('BT', c), ('AR', c)], [pk], inc=False)
                mm(p[:, 256:448], KT[:, c, :], AR[:, c, :], [('KT', c), ('AR', c)], [pk], start=True, stop=True)
                tt('dve', CB[c]['AB'], p[:, 0:192], mk1[:], ALU.mult, [pk, 'mk1'], [('AB', g_)])
                tt('dve', CB[c]['AK'], p[:, 256:448], mk1[:], ALU.mult, [pk, 'mk1'], [('AK', g_)])
            for g_ in range(2):
                p, pk = pf()
                for i in range(4):
                    c = g_ * 4 + i
                    mm(p[:, i * 128:(i + 1) * 128], AR[:, c, 0:128], BT[:, c, :], [('BT', c), ('AR', c)], [pk], inc=(i == 3))
                for i in range(4):
                    c = g_ * 4 + i
                    tt('dve', CB[c]['Pt'], p[:, i * 128:(i + 1) * 128], mk2[:], ALU.mult, [pk, 'mk2'], [('Pt', g_)])
                    tt('pool', CB[c]['Mm'], CB[c]['AB'][:, 0:128], identF[:], ALU.add, [('AB', g_), 'identF'], [('Mm', g_)])
            for kq in range(1, 6):
                for g_ in range(2):
                    if kq < 5:
                        pA, pAk = pf()
                        for i in range(4):
                            c = g_ * 4 + i
                            mm(pA[:, i * 128:(i + 1) * 128], CB[c]['Pt'], CB[c]['AB'][:, 0:128], [('Pt', g_), ('AB', g_)], [pAk], inc=(i == 3))
                    pB, pBk = pf()
                    for i in range(4):
                        c = g_ * 4 + i
                        mm(pB[:, i * 128:(i + 1) * 128], CB[c]['AB'][:, 0:128], CB[c]['Pt'], [('Pt', g_), ('AB', g_)], [pBk], inc=(i == 3))
                    if False:
                        for i in range(4):
                            c = g_ * 4 + i
                            if kq < 5:
                                cp('act', CB[c]['AB'][:, 0:128], pA[:, i * 128:(i + 1) * 128], [pAk], [('AB', g_)])
                            cp('dve' if g_ else 'act', CB[c]['Pt'], pB[:, i * 128:(i + 1) * 128], [pBk], [('Pt', g_)])
                    else:
                        if kq < 5:
                            cp('act', gview(g_, 'AB', 0, 128), pA[:, :].rearrange("p (c n) -> p c n", c=4), [pAk], [('AB', g_)])
                        cp('dve' if g_ else 'act', gview(g_, 'Pt', 0, 128), pB[:, :].rearrange("p (c n) -> p c n", c=4), [pBk], [('Pt', g_)])
                for g_ in range(2):
                    pC, pCk = pf()
                    for i in range(4):
                        c = g_ * 4 + i
                        mm(pC[:, i * 128:(i + 1) * 128], CB[c]['Pt'], CB[c]['Mm'], [('Pt', g_), ('Mm', g_)], [pCk], inc=(i == 3))
                    if False:
                        for i in range(4):
                            c = g_ * 4 + i
                            tt('dve', CB[c]['Mm'], pC[:, i * 128:(i + 1) * 128], CB[c]['Mm'], ALU.add, [pCk, ('Mm', g_)], [('Mm', g_)])
                    else:
                        tt('dve', gview(g_, 'Mm', 0, 128), pC[:, :].rearrange("p (c n) -> p c n", c=4), gview(g_, 'Mm', 0, 128), ALU.add,
                           [pCk, ('Mm', g_)], [('Mm', g_)])

            for c in range(8):
                ci = c % 2
                g_ = c // 4
                AB, AK, Tt = CB[c]['AB'], CB[c]['AK'], CB[c]['Mm']
                abk, akk, Ttk = ('AB', g_), ('AK', g_), ('Mm', g_)
                Scur = Sbd[:, pr, c % 2, :]
                Snxt = Sbd[:, pr, (c + 1) % 2, :]
                sck, snk = ('Sbd', pr, c % 2), ('Sbd', pr, (c + 1) % 2)
                X = AF32(*s_x[ci]); xk = ('X', ci)
                U = AF32(*s_u[ci]); uk_ = ('U', ci)
                p, pk = pf()
                mm(p[:, 0:128], AR[:, c, 0:128], Scur, [('AR', c), sck, 'Sbd'], [pk], start=True, stop=False)
                mm(p[:, 0:128], AK[:, 0:128], VH[:, c, :], [akk, ('VH', c)], [pk], start=False, stop=True)
                cp('act', X, p[:, 0:128], [pk], [xk])
                p, pk = pf()
                mm(p[:, 0:128], Tt, X, [Ttk, xk], [pk])
                cp('act', U, p[:, 0:128], [pk], [uk_])
                p, pk = pf()
                mm(p[:, 0:128], BH[:, c, :], U, [('BH', c), uk_], [pk], start=True, stop=False)
                mm(p[:, 0:128], KH[:, c, :], VH[:, c, :], [('KH', c), ('VH', c)], [pk], start=False, stop=True)
                stt(Snxt, Scur, WC[:, c:c + 1], p[:, 0:128], ALU.mult, ALU.add, [sck, 'WC', pk, 'Sbd'], [snk])
                p, pk = pf()
                mm(p[:, 0:64], Scur, AR[:, c, 128:192], [sck, ('AR', c), 'Sbd'], [pk], start=True, stop=False)
                mm(p[:, 0:64], U, AB[:, 128:192], [uk_, abk], [pk], start=False, stop=False)
                mm(p[:, 0:64], VH[:, c, :], AK[:, 128:192], [('VH', c), akk], [pk], start=False, stop=True)
                cp('dve', O[:, c * 64:(c + 1) * 64], p[:, 0:64], [pk, 'k'], ['k'])
            S.op('pool', lambda e: e.memset(small[:, 57:58], 0.0), [], list(DEAD) + cbk)
            p, pk = pf()
            mm(p[:, :], gnones[:], O, ['gnones', 'k'], [pk])
            tt('dve', t1, O, p[:, :], ALU.subtract, ['k', pk], ['t1'])
            tt('pool', t2, t1, t1, ALU.mult, ['t1'], ['t2'])
            p, pk = pf()
            mm(p[:, :], gnones[:], t2, ['gnones', 't2'], [pk])
            act(t2, p[:, :], AF.Sqrt, [pk], ['t2'], bias=GN_EPS)
            S.op('dve', lambda e: e.reciprocal(t2, t2), ['t2'], ['t2'])
            tt('dve', t1, t1, t2, ALU.mult, ['t1', 't2'], ['t1'])
            ts('dve', t1, t1, col('lnx_g'), ALU.mult, ['t1', 'par'], ['t1'], s2=col('lnx_b'), op1=ALU.add)
            tt('pool', t1, t1, bon, ALU.add, ['t1', 'bon'], ['t1'])
            tt('dve', yT[:, pr, :], t1, g, ALU.mult, ['t1', 'g'], [('yT', pr)])
            if l == 0 and j == 0 and pr == 0:
                tap('O', O, ['k'])
                tap('yr', t1, ['t1'])

        def phase_mla(l, j):
            S.barrier()
            o = 0

            def slot(n):
                nonlocal o
                a_ = (o, n)
                o += n
                return a_
            NK = (j + 1) * TT
            s_S = slot(max(T, 8 * TT))
            s_cq = (s_S[0], 4 * TT)
            s_ckv = (s_S[0] + 4 * TT, 2 * TT)
            s_t1 = (s_S[0] + 6 * TT, TT)
            s_t2 = (s_S[0] + 7 * TT, TT)

            def b16(n_units):
                nonlocal o
                v_ = AB16(o, n_units)
                o += n_units
                return v_
            s_rope = (o, 2 * TT)
            P_ = b16(max(T // 2, 2 * TT))
            Kh = b16(T // 2)
            Vh = b16(T // 2).rearrange("p (b d) -> p b d", d=128)
            krl = b16(T // 2)
            qT = b16(8 * TT // 2).rearrange("p (h t) -> p h t", h=8)
            qrT = b16(8 * TT // 2).rearrange("p (h t) -> p h t", h=8)
            cqn = b16(4 * TT // 2).rearrange("p (c t) -> p c t", c=4)
            ckvn = b16(2 * TT // 2).rearrange("p (c t) -> p c t", c=2)
            knst = b16(TT // 2)
            krst = b16(TT // 2)
            vst = [b16(512), b16(512)]
            PT = [b16(256), b16(256)]
            osb = b16(64)
            rsum = small[:, 24:26]
            mx = small[:, 26:27]
            assert o <= ARENA_N, o
            Ssb = AF32(*s_S)
            cq = AF32(*s_cq).rearrange("p (c t) -> p c t", c=4)
            ckv = AF32(*s_ckv).rearrange("p (c t) -> p c t", c=2)
            tmp1 = AF32(*s_t1)
            tmp2 = AF32(*s_t2)
            rope = AF32(*s_rope).rearrange("p (a t) -> p a t", a=2)
            S.dma('sp', rope[0:64], cd['rope'][:, :, j * TT:(j + 1) * TT].rearrange("a p t -> p a t"), writes=['rope'], semkey='rope')

            def proj(lhs_fn, M, rkeys):
                p, pk = pf()
                for c in range(16):
                    mm(p[0:M, :], lhs_fn(c), uT[:, c, :], rkeys + [('uT', c)], [pk], start=(c == 0), stop=(c == 15))
                return p, pk
            buf, key = wload([(0, 16, 512, w_in_d[l][:, 3520:4032].rearrange("(k p) n -> p k n", p=128))])
            wv_ = wview(buf, 0, 16, 512)
            for c in range(4):
                p, pk = proj(lambda cc, c=c: wv_[:, cc, c * 128:(c + 1) * 128], 128, [key])
                cp('act', cq[:, c, :], p[:, :], [pk], ['cq'])
            buf, key = wload([(0, 16, 320, w_in_d[l][:, 4032:4352].rearrange("(k p) n -> p k n", p=128))])
            wv_ = wview(buf, 0, 16, 320)
            for c in range(2):
                p, pk = proj(lambda cc, c=c: wv_[:, cc, c * 128:(c + 1) * 128], 128, [key])
                cp('act', ckv[:, c, :], p[:, :], [pk], ['ckv'])

            def rope_mm(lhs_fn, nkc, rhs_fn, rkeys):
                p1, pk1 = pf()
                for c in range(nkc):
                    mm(p1[0:64, :], lhs_fn(c, 0, 64), rhs_fn(c), rkeys(c), [pk1], start=(c == 0), stop=(c == nkc - 1))
                p2, pk2 = pf()
                for c in range(nkc):
                    mm(p2[0:32, :], lhs_fn(c, 32, 64), rhs_fn(c), rkeys(c), [pk2], start=(c == 0), stop=(c == nkc - 1))
                for c in range(nkc):
                    mm(p2[32:64, :], lhs_fn(c, 0, 32), rhs_fn(c), rkeys(c), [pk2], start=(c == 0), stop=(c == nkc - 1))
                tt('dve', tmp1[0:64, :], p1[0:64, :], rope[0:64, 0, :], ALU.mult, [pk1, 'rope'], ['tmp1'])
                tt('dve', tmp2[0:64, :], p2[0:64, :], rope[0:64, 1, :], ALU.mult, [pk2, 'rope'], ['tmp2'])
                tt('dve', tmp1[0:64, :], tmp1[0:64, :], tmp2[0:64, :], ALU.add, ['tmp1', 'tmp2'], ['tmp1'])

            kb = 256
            rope_mm(lambda c, lo, hi: wv_[:, c, kb + lo:kb + hi], 16, lambda c: uT[:, c, :], lambda c: [key, ('uT', c)])
            cp('act', krst[0:64, :], tmp1[0:64, :], ['tmp1'], ['krst'])
            S.dma('sp', kr_d[:, j * TT:(j + 1) * TT], krst[0:64, :], reads=['krst'], writes=[('kr_d', j)], semkey='krst')

            rms_stats(lambda c: cq[:, c, :], 4, 512.0, EPS, ['cq'], sq_buf=qT, sqkey='qTs')
            g0 = pc('qn_g')
            for c in range(4):
                stt(cqn[:, c, :], cq[:, c, :], par[:, g0 + c:g0 + c + 1], rstd[:], ALU.mult, ALU.mult, ['cq', 'par', 'rstd'], ['cqn'])
            rms_stats(lambda c: ckv[:, c, :], 2, 256.0, EPS, ['ckv'], sq_buf=qT, sqkey='qTs')
            g0 = pc('kvn_g')
            for c in range(2):
                stt(ckvn[:, c, :], ckv[:, c, :], par[:, g0 + c:g0 + c + 1], rstd[:], ALU.mult, ALU.mult, ['ckv', 'par', 'rstd'], ['ckvn'])

            SC = float((128 + 64) ** -0.5)
            buf, key = wload([(0, 4, 1536, w_uq_d[l].rearrange("(k p) n -> p k n", p=128))])
            wq_ = wview(buf, 0, 4, 1536)
            sqk = [('qTs', c) for c in range(4)]
            for h in range(8):
                p, pk = pf()
                for c in range(4):
                    mm(p[:, :], wq_[:, c, h * 192:h * 192 + 128], cqn[:, c, :], [key, 'cqn'], [pk], start=(c == 0), stop=(c == 3))
                S.op('act', lambda e, p=p, h=h: e.mul(qT[:, h, :], p[:, :], SC), [pk] + sqk, [('qTh', h)] + (sqk if h < 4 else []))
                rb = h * 192 + 128
                rope_mm(lambda c, lo, hi, rb=rb: wq_[:, c, rb + lo:rb + hi], 4, lambda c: cqn[:, c, :], lambda c: [key, 'cqn'])
                S.op('act', lambda e, h=h: e.mul(qrT[0:64, h, :], tmp1[0:64, :], SC), ['tmp1'], [('qrT', h)])
            buf, key = wload([(0, 2, 2048, w_ukv_d[l].rearrange("(k p) n -> p k n", p=128))])
            wkv_ = wview(buf, 0, 2, 2048)
            for h in range(8):
                p, pk = pf()
                for c in range(2):
                    mm(p[:, :], wkv_[:, c, h * 256:h * 256 + 128], ckvn[:, c, :], [key, 'ckvn'], [pk], start=(c == 0), stop=(c == 1))
                cp('act', knst[:, :], p[:, :], [pk], ['knst'])
                S.dma('sp', kn_d[h][:, j * TT:(j + 1) * TT], knst[:, :], reads=['knst'], writes=[('kn_d', h, j)], semkey='knst')
            wkv4 = wkv_.rearrange("p k (h two d) -> p k h two d", h=8, two=2)
            for tb in range(4):
                for hg in range(2):
                    p, pk = pf()
                    for c in range(2):
                        mm(p[:, :].rearrange("p (h d) -> p h d", h=4), ckvn[:, c, tb * 128:(tb + 1) * 128],
                           wkv4[:, c, hg * 4:(hg + 1) * 4, 1, :], [key, 'ckvn'], [pk], start=(c == 0), stop=(c == 1))
                    cp('act', vst[tb % 2][:, hg * 512:(hg + 1) * 512], p[:, :], [pk], [('vst', tb % 2)])
                S.dma('sp', vt_d[j * TT + tb * 128:j * TT + (tb + 1) * 128, :], vst[tb % 2], reads=[('vst', tb % 2)],
                      writes=[('vt_d', j, tb)], semkey='vst%d' % (tb % 2))
            S.barrier()
            S.dma('sp', krl[0:64, 0:NK], kr_d[:, 0:NK], reads=[('kr_d', jj) for jj in range(j + 1)], writes=['krl'], semkey='krl')
            for h in range(8):
                S.dma('sp', Kh[:, 0:NK], kn_d[h][:, 0:NK], reads=[('kn_d', h, jj) for jj in range(j + 1)], writes=['Kh'], semkey='Kh')
                S.dma('sp', Vh[:, 0:NK // 128, :], vt_d[0:NK, h * 128:(h + 1) * 128].rearrange("(b p) d -> p b d", p=128),
                      reads=[('vt_d', jj, tb_) for jj in range(j + 1) for tb_ in range(4)], writes=['Vh'], semkey='Vh')
                for qb in range(4):
                    q0 = j * TT + qb * 128
                    nk = q0 + 128
                    qs = slice(qb * 128, (qb + 1) * 128)
                    ng = (nk + 511) // 512
                    for gi in range(ng):
                        k0 = gi * 512
                        kw = min(512, nk - k0)
                        p, pk = pf()
                        mm(p[:, 0:kw], qT[:, h, qs], Kh[:, k0:k0 + kw], [('qTh', h), 'Kh'], [pk], start=True, stop=False)
                        mm(p[:, 0:kw], qrT[0:64, h, qs], krl[0:64, k0:k0 + kw], [('qrT', h), 'krl'], [pk], start=False, stop=True)
                        if k0 + kw == nk:
                            if kw > 128:
                                cp('act', Ssb[:, k0:k0 + kw - 128], p[:, 0:kw - 128], [pk], ['Ssb'])
                            tt('dve', Ssb[:, nk - 128:nk], p[:, kw - 128:kw], cmask[:], ALU.add, [pk, 'cmask'], ['Ssb'])
                        else:
                            cp('act', Ssb[:, k0:k0 + kw], p[:, 0:kw], [pk], ['Ssb'])
                    S.op('dve', lambda e, nk=nk: e.reduce_max(mx, Ssb[:, 0:nk], mybir.AxisListType.X), ['Ssb'], ['mx'])
                    ts('dve', mx, mx, -1.0, ALU.mult, ['mx'], ['mx'])
                    act(P_[:, 0:nk], Ssb[:, 0:nk], AF.Exp, ['Ssb', 'mx'], ['P_', 'rsum'], bias=mx, accum=rsum[:, 0:1])
                    S.op('dve', lambda e: e.reciprocal(rsum[:, 1:2], rsum[:, 0:1]), ['rsum'], ['rinv'])
                    nb = nk // 128
                    po, pok = pf()
                    for b4 in range(0, nb, 4):
                        nbb = min(4, nb - b4)
                        pt_, ptk = pb()
                        for i in range(nbb):
                            tr(pt_[:, i * 128:(i + 1) * 128], P_[:, (b4 + i) * 128:(b4 + i + 1) * 128], identB[:], ['P_', 'identB'], [ptk], inc=(i == nbb - 1))
                        pti = (b4 // 4) % 2
                        cp('act' if pti else 'dve', PT[pti][:, 0:nbb * 128], pt_[:, 0:nbb * 128], [ptk], [('PT', pti)])
                        for i in range(nbb):
                            bb = b4 + i
                            mm(po[:, 0:128], PT[pti][:, i * 128:(i + 1) * 128], Vh[:, bb, :], [('PT', pti), 'Vh'], [pok],
                               start=(bb == 0), stop=(bb == nb - 1))
                    S.op('act', lambda e, po=po: e.mul(osb[:, :], po[:, 0:128], rsum[:, 1:2]), [pok, 'rinv'], ['osb'])
                    pt_, ptk = pb()
                    tr(pt_[:, 0:128], osb[:, :], identB[:], ['osb', 'identB'], [ptk])
                    cp('dve', yT[:, 8 + h, qs], pt_[:, 0:128], [ptk], [('yT', 8 + h)])

        def phase_ffn(l, j):
            S.barrier()
            z = AF32(0, 16 * TT).rearrange("p (c t) -> p c t", c=16)
            o = 16 * TT
            h1 = [AB16(o, 4 * TT // 2).rearrange("p (c t) -> p c t", c=4), AB16(o + 4 * TT // 2, 4 * TT // 2).rearrange("p (c t) -> p c t", c=4)]
            o += 4 * TT
            qm = AB16(o, 4 * TT // 2).rearrange("p (h t) -> p h t", h=4); o += 4 * TT // 2
            om = AB16(o, 4 * TT // 2).rearrange("p (h t) -> p h t", h=4); o += 4 * TT // 2
            Pm_ = AB16(o, 128); o += 128
            PTm = AB16(o, 128); o += 128
            osb = AB16(o, 64); o += 64
            rt = AF32(o, TT); o += TT
            assert o <= ARENA_N
            yk = [('yT', c) for c in range(16)]
            for nb in range(4):
                buf, key = wload([(0, 16, 512, w_out_d[l][:, nb * 512:(nb + 1) * 512].rearrange("(k p) n -> p k n", p=128))])
                wv_ = wview(buf, 0, 16, 512)
                for i in range(4):
                    n = nb * 4 + i
                    p, pk = pf()
                    for c in range(16):
                        mm(p[:, :], wv_[:, c, i * 128:(i + 1) * 128], yT[:, c, :], [key, ('yT', c)], [pk], start=(c == 0), stop=(c == 15))
                    cp('act' if n % 2 else 'dve', z[:, n, :], p[:, :], [pk], ['z'])
            post_norm_res(z, 'z', 'mix_post_g')
            pre_norm('mem_pre_g')
            buf, key = wload([(0, 16, 512, wq_d[l].rearrange("(k p) n -> p k n", p=128))])
            wv_ = wview(buf, 0, 16, 512)
            SCM = float(128 ** -0.5)
            for h in range(4):
                p, pk = pf()
                for c in range(16):
                    mm(p[:, :], wv_[:, c, h * 128:(h + 1) * 128], uT[:, c, :], [key, ('uT', c)], [pk], start=(c == 0), stop=(c == 15))
                S.op('act', lambda e, p=p, h=h: e.mul(qm[:, h, :], p[:, :], SCM), [pk], [('qm', h)])
            rsum = small[:, 28:30]
            mx = small[:, 30:31]
            for h in range(4):
                for qb in range(4):
                    qs = slice(qb * 128, (qb + 1) * 128)
                    p, pk = pf()
                    mm(p[:, 0:NMEM], qm[:, h, qs], kmT[:, h, :], [('qm', h), 'kmT'], [pk])
                    S.op('dve', lambda e, p=p: e.reduce_max(mx, p[:, 0:NMEM], mybir.AxisListType.X), [pk], ['mxm'])
                    ts('dve', mx, mx, -1.0, ALU.mult, ['mxm'], ['mxm'])
                    act(Pm_[:, :], p[:, 0:NMEM], AF.Exp, [pk, 'mxm'], ['Pm_', 'rsm'], bias=mx, accum=rsum[:, 0:1])
                    S.op('dve', lambda e: e.reciprocal(rsum[:, 1:2], rsum[:, 0:1]), ['rsm'], ['rim'])
                    pt_, ptk = pb()
                    for i in range(2):
                        tr(pt_[:, i * 128:(i + 1) * 128], Pm_[:, i * 128:(i + 1) * 128], identB[:], ['Pm_', 'identB'], [ptk], inc=(i == 1))
                    cp('dve', PTm[:, :], pt_[:, 0:256], [ptk], ['PTm'])
                    po, pok = pf()
                    for i in range(2):
                        mm(po[:, 0:128], PTm[:, i * 128:(i + 1) * 128], vm[:, i, h * 128:(h + 1) * 128], ['PTm', 'vm'], [pok], start=(i == 0), stop=(i == 1))
                    S.op('act', lambda e, po=po: e.mul(osb[:, :], po[:, 0:128], rsum[:, 1:2]), [pok, 'rim'], ['osbm'])
                    pt_, ptk = pb()
                    tr(pt_[:, 0:128], osb[:, :], identB[:], ['osbm', 'identB'], [ptk])
                    cp('dve', om[:, h, qs], pt_[:, 0:128], [ptk], ['om'])
            buf, key = wload([(0, 4, 2048, wo_d[l].rearrange("(k p) n -> p k n", p=128))])
            wv_ = wview(buf, 0, 4, 2048)
            for n in range(16):
                p, pk = pf()
                for c in range(4):
                    mm(p[:, :], wv_[:, c, n * 128:(n + 1) * 128], om[:, c, :], [key, 'om'], [pk], start=(c == 0), stop=(c == 3))
                cp('act' if n % 2 else 'dve', z[:, n, :], p[:, :], [pk], ['z'])
            post_norm_res(z, 'z', 'mem_post_g')
            pre_norm('ffn_pre_g')
            for blk in range(16):
                buf, key = wload([(0, 16, 512, ff1_d[l][:, blk * 512:(blk + 1) * 512].rearrange("(k p) n -> p k n", p=128))])
                wv_ = wview(buf, 0, 16, 512)
                hb = h1[blk % 2]
                hk = ('h1', blk % 2)
                for i in range(4):
                    p, pk = pf()
                    for c in range(16):
                        mm(p[:, :], wv_[:, c, i * 128:(i + 1) * 128], uT[:, c, :], [key, ('uT', c)], [pk], start=(c == 0), stop=(c == 15))
                    act(rt, p[:, :], AF.Relu, [pk], ['rt'])
                    tt('pool', hb[:, i, :], rt, rt, ALU.mult, ['rt'], [hk])
                buf, key = wload([(0, 4, 2048, ff2_d[l][blk * 512:(blk + 1) * 512, :].rearrange("(k p) n -> p k n", p=128))])
                wv_ = wview(buf, 0, 4, 2048)
                for n in range(16):
                    p, pk = pf()
                    for c in range(4):
                        mm(p[:, :], wv_[:, c, n * 128:(n + 1) * 128], hb[:, c, :], [key, hk], [pk], start=(c == 0), stop=(c == 3))
                    if blk == 0:
                        cp('act' if n % 2 else 'dve', z[:, n, :], p[:, :], [pk], [('zf', n)])
                    else:
                        tt('dve', z[:, n, :], z[:, n, :], p[:, :], ALU.add, [pk, ('zf', n)], [('zf', n)])
            S.op('pool', lambda e: e.memset(small[:, 59:60], 0.0), [('zf', n) for n in range(16)], ['z'])
            post_norm_res(z, 'z', 'ffn_post_g')

        prep_mem()
        for l in range(L):
            layer_setup(l)
            for j in range(NT):
                S.barrier()
                load_tile(l, j)
                phase_rwkv(l, j)
                phase_mla(l, j)
                phase_ffn(l, j)
                store_tile(l, j)
        S.barrier()
        S.emit()
    return nc, cn


def make_in_maps(inputs, T, L, nb):
    cn = make_consts(T)
    maps = []
    f32 = lambda a: np.ascontiguousarray(np.asarray(a, np.float32))
    params = np.stack([pack_params(inputs, l) for l in range(L)], 0)
    shared = {
        "params": f32(params),
        "memg": f32(_fm(inputs['mem_norm_g'], 16)),
        "w_in": f32(inputs['w_in'][:L]),
        "w_in_vres": f32(inputs['w_in_vres'][:max(L - 1, 1)]),
        "w_up": f32(inputs['w_up'][:L]), "a_up": f32(inputs['a_up'][:L]), "g_up": f32(inputs['g_up'][:L]),
        "v_up": f32(inputs['v_up'][:max(L - 1, 1)]),
        "w_uq": f32(inputs['w_uq'][:L]), "w_ukv": f32(inputs['w_ukv'][:L]), "w_out": f32(inputs['w_out'][:L]),
        "wq_mem": f32(inputs['wq_mem'][:L]), "wk_mem": f32(inputs['wk_mem'][:L]), "wv_mem": f32(inputs['wv_mem'][:L]),
        "wo_mem": f32(inputs['wo_mem'][:L]), "w_ff1": f32(inputs['w_ff1'][:L]), "w_ff2": f32(inputs['w_ff2'][:L]),
    }
    for k, v in cn.items():
        shared["c_" + k] = f32(v)
    for b in range(nb):
        m = dict(shared)
        m["x"] = f32(inputs['x'][b][:T])
        m["mem"] = f32(inputs['mem'][b])
        maps.append(m)
    return maps


def kernel(**inputs):
    B, T, _ = inputs['x'].shape
    L = inputs['w_in'].shape[0]
    nc, _ = build(T, L)
    maps = make_in_maps(inputs, T, L, B)
    res = run_bass_kernel_spmd(nc, maps, core_ids=list(range(B)))
    return np.stack([np.asarray(res.results[b]["out"], np.float32) for b in range(B)], 0)
```

```python
import contextlib
import numpy as np
import concourse.bass as bass
import concourse.mybir as mybir
from concourse.bass_utils import run_bass_kernel_spmd

F32 = mybir.dt.float32
BF16 = mybir.dt.bfloat16
AF = mybir.ActivationFunctionType
ALU = mybir.AluOpType

D = 2048
TT = 512
NMEM = 256
RW = 1024
IN_W = 4352
DFF = 8192
GN_EPS = 64e-5
EPS = 1e-6
DECAY_C = -0.6065306597126334


class Sched:
    CE = ('pe', 'dve', 'act', 'pool')
    ENG = ('pe', 'dve', 'act', 'pool', 'sp')

    def __init__(self, nc, stack):
        self.nc = nc
        self.stack = stack
        self.q = {e: [] for e in self.ENG}
        self.semh = {}
        self.cnt = {}
        for e in self.CE:
            self._mksem('s_' + e)
        self.lastw = {}
        self.readers = {}
        self.seen = {e: {} for e in self.ENG}
        self.n_ops = 0

    def _mksem(self, name):
        self.semh[name] = self.stack.enter_context(self.nc.semaphore(name))
        self.cnt[name] = 0

    def _waits(self, eng, reads, writes, is_dma=False):
        need = {}
        own = None if is_dma else 's_' + eng

        def add(d, raw):
            if d is None:
                return
            if d[0] == own and not raw:
                return
            if need.get(d[0], 0) < d[1]:
                need[d[0]] = d[1]
        for k in reads:
            add(self.lastw.get(k), True)
        for k in writes:
            add(self.lastw.get(k), False)
            r = self.readers.get(k)
            if r:
                for s, v in r.items():
                    add((s, v), False)
        waits = []
        for s, v in need.items():
            if eng == 'pe' and s == 's_pe':
                continue
            if self.seen[eng].get(s, 0) < v:
                self.seen[eng][s] = v
                waits.append((s, v))
        return waits

    def _commit(self, ident, reads, writes):
        for k in writes:
            self.lastw[k] = ident
            self.readers[k] = {}
        for k in reads:
            r = self.readers.setdefault(k, {})
            if r.get(ident[0], 0) < ident[1]:
                r[ident[0]] = ident[1]

    def op(self, eng, fn, reads=(), writes=(), inc=True):
        waits = self._waits(eng, reads, writes)
        s = 's_' + eng
        if inc:
            self.cnt[s] += 1
            ident = (s, self.cnt[s])
        else:
            ident = (s, self.cnt[s] + 1)
        self._commit(ident, reads, writes)
        self.q[eng].append((waits, fn, (s, 1) if inc else None))
        self.n_ops += 1

    def dma(self, eng, out, in_, reads=(), writes=(), semkey=None, slow=False):
        assert semkey is not None
        s = 'd_' + semkey
        if s not in self.semh:
            self._mksem(s)
        waits = self._waits(eng, reads, writes, is_dma=True)
        self.cnt[s] += 16
        ident = (s, self.cnt[s])
        self._commit(ident, reads, writes)
        if slow:
            fn = lambda e: e.dma_start(out=out, in_=in_, allow_slow_non_contiguous=True)
        else:
            fn = lambda e: e.dma_start(out=out, in_=in_)
        self.q[eng].append((waits, fn, (s, 16)))
        self.n_ops += 1

    def barrier(self, engines=None, final=False):
        for e in (engines or self.ENG):
            waits = []
            for s, c in self.cnt.items():
                if c > 0 and self.seen[e].get(s, 0) < c:
                    if e == 'pe' and s == 's_pe':
                        continue
                    if s.startswith('d_cvt') and not final:
                        continue
                    self.seen[e][s] = c
                    waits.append((s, c))
            if waits:
                self.q[e].append((waits, None, None))

    def emit(self):
        engobj = {'pe': 'tensor', 'dve': 'vector', 'act': 'scalar', 'pool': 'gpsimd', 'sp': 'sync'}
        with self.nc.Block() as block:
            for e in self.ENG:
                items = self.q[e]
                if not items:
                    continue

                def body(eo, items=items):
                    for waits, fn, inc in items:
                        for s, v in waits:
                            eo.wait_ge(self.semh[s], v)
                        if fn is not None:
                            ins = fn(eo)
                            if inc is not None:
                                ins.then_inc(self.semh[inc[0]], inc[1])
                getattr(block, engobj[e])(body)


PCOL = {}
_off = 0
for _n, _w in [('mix_pre_g', 16), ('mix_post_g', 16), ('mem_pre_g', 16), ('mem_post_g', 16), ('ffn_pre_g', 16),
               ('ffn_post_g', 16), ('mu_r', 8), ('mu_k', 8), ('mu_v', 8), ('mu_wl', 1), ('mu_al', 1), ('mu_gl', 2),
               ('mu_vr', 1), ('w0', 8), ('a0', 8), ('v0', 8), ('k_k', 8), ('k_a', 8), ('r_k', 8), ('lnx_g', 8),
               ('lnx_b', 8), ('qn_g', 4), ('kvn_g', 2)]:
    PCOL[_n] = (_off, _w)
    _off += _w
NP_RAW = _off
MU_COLS = PCOL['mu_r'][0]
N_MU = 29
PCOL['om'] = (NP_RAW, N_MU)
PCOL['omka'] = (NP_RAW + N_MU, 8)
NPAR = NP_RAW + N_MU + 8


def _fm(v, ncol):
    buf = np.zeros(ncol * 128, np.float32)
    v = np.asarray(v, np.float32).reshape(-1)
    buf[:v.size] = v
    return buf.reshape(ncol, 128).T


def pack_params(inp, l):
    P = np.zeros((128, NPAR), np.float32)

    def put(name, arr):
        o, w = PCOL[name]
        P[:, o:o + w] = arr
    for n in ['mix_pre_g', 'mix_post_g', 'mem_pre_g', 'mem_post_g', 'ffn_pre_g', 'ffn_post_g']:
        put(n, _fm(inp[n][l], 16))
    mu = inp['mu_rwkv'][l]
    put('mu_r', _fm(mu[0:1024], 8))
    put('mu_k', _fm(mu[1024:2048], 8))
    put('mu_v', _fm(mu[2048:3072], 8))
    put('mu_wl', _fm(mu[3072:3168], 1))
    put('mu_al', _fm(mu[3168:3264], 1))
    put('mu_gl', _fm(mu[3264:3520], 2))
    if l > 0:
        put('mu_vr', _fm(inp['mu_vres'][l - 1], 1))
        put('v0', _fm(inp['v0'][l - 1], 8))
    for n in ['w0', 'a0', 'k_k', 'k_a', 'lnx_g', 'lnx_b']:
        put(n, _fm(inp[n][l], 8))
    put('r_k', _fm(inp['r_k'][l].reshape(-1), 8))
    put('qn_g', _fm(inp['q_norm_g'][l], 4))
    put('kvn_g', _fm(inp['kv_norm_g'][l], 2))
    return P


def make_consts(T):
    c = {}
    c['identF'] = np.eye(128, dtype=np.float32)
    bo = np.zeros((128, 128), np.float32)
    bo[:64, :64] = 1.0
    bo[64:, 64:] = 1.0
    c['blockones'] = bo
    r = np.arange(128)
    mk1 = np.zeros((128, 192), np.float32)
    mk1[:, :128] = (r[:, None] < r[None, :]).astype(np.float32)
    mk1[:, 128:] = ((r[:, None] % 64) <= np.arange(64)[None, :]).astype(np.float32)
    c['mk1'] = mk1
    c['mk2'] = (r[:, None] > r[None, :]).astype(np.float32)
    c['cmask'] = np.where(r[:, None] >= r[None, :], 0.0, -1e9).astype(np.float32)
    rs = np.ones((128, TT), np.float32)
    rs[:, ::64] = 0.0
    c['reset'] = rs
    pos = np.arange(T, dtype=np.float32)
    inv_freq = (10000.0 ** (-np.arange(0, 64, 2, dtype=np.float32) / 64)).astype(np.float32)
    ang = (pos[:, None] * inv_freq[None, :]).astype(np.float32)
    cs, sn = np.cos(ang).T.astype(np.float32), np.sin(ang).T.astype(np.float32)
    c['rope'] = np.stack([np.concatenate([cs, cs], 0), np.concatenate([-sn, sn], 0)], 0).astype(np.float32)
    return c


def build(T, L, taps=()):
    NT = T // TT
    nc = bass.Bass("TRN2", target_bir_lowering=False)
    din = lambda name, shape, dt=F32: nc.dram_tensor(name, list(shape), dt, kind="ExternalInput").ap()
    x_d = din("x", [T, D])
    mem_d = din("mem", [NMEM, D])
    memg_d = din("memg", [128, 16])
    par_d = din("params", [L, 128, NPAR])
    w_in_d = din("w_in", [L, D, IN_W])
    w_vres_d = din("w_in_vres", [max(L - 1, 1), D, 64])
    w_up_d = din("w_up", [L, 96, RW])
    a_up_d = din("a_up", [L, 96, RW])
    g_up_d = din("g_up", [L, 256, RW])
    v_up_d = din("v_up", [max(L - 1, 1), 64, RW])
    w_uq_d = din("w_uq", [L, 512, 1536])
    w_ukv_d = din("w_ukv", [L, 256, 2048])
    w_out_d = din("w_out", [L, D, D])
    wq_d = din("wq_mem", [L, D, 512])
    wk_d = din("wk_mem", [L, D, 512])
    wv_d = din("wv_mem", [L, D, 512])
    wo_d = din("wo_mem", [L, 512, D])
    ff1_d = din("w_ff1", [L, D, DFF])
    ff2_d = din("w_ff2", [L, DFF, D])
    cn = make_consts(T)
    cd = {k: din("c_" + k, v.shape) for k, v in cn.items()}
    out_d = nc.dram_tensor("out", [T, D], F32, kind="ExternalOutput").ap()
    tap_d = {}
    for name, shape in taps:
        tap_d[name] = nc.dram_tensor("tap_" + name, list(shape), F32, kind="ExternalOutput").ap()
    hT_d = nc.dram_tensor("hT_s", [D, T], F32).ap()
    vf_d = nc.dram_tensor("vf_s", [RW, T], F32).ap()
    kn_d = nc.dram_tensor("kn_s", [8, 128, T], BF16).ap()
    kr_d = nc.dram_tensor("kr_s", [64, T], BF16).ap()
    vt_d = nc.dram_tensor("vt_s", [T, 1024], BF16).ap()
    memT_d = nc.dram_tensor("memT_s", [128, 16 * NMEM], BF16).ap()

    with contextlib.ExitStack() as st:
        S = Sched(nc, st)
        sb = lambda name, shape, dt=F32: st.enter_context(nc.sbuf_tensor(name, list(shape), dt))
        pst = lambda name, shape, dt=F32: st.enter_context(nc.psum_tensor(name, list(shape), dt))

        identF = sb("identF", [128, 128])
        identB = sb("identB", [128, 128], BF16)
        onesB = sb("onesB", [128, 128], BF16)
        blockones = sb("blockones", [128, 128])
        gnones = sb("gnones", [128, 128])
        mk1 = sb("mk1", [128, 192])
        mk2 = sb("mk2", [128, 128])
        cmask = sb("cmask", [128, 128])
        reset = sb("reset", [128, TT])
        hT = sb("hT", [128, 16, TT])
        uT = sb("uT", [128, 16, TT], BF16)
        yT = sb("yT", [128, 16, TT], BF16)
        wb = [sb("wb0", [128, 16 * 512], BF16), sb("wb1", [128, 16 * 512], BF16)]
        par = sb("par", [128, NPAR])
        lw = sb("lw", [128, 5, RW], BF16)
        kmT = sb("kmT", [128, 4, NMEM], BF16)
        vm = sb("vm", [128, 2, 512], BF16)
        Sbd = sb("Sbd", [128, 8, 2, 128])
        carry = sb("carry", [128, N_MU])
        rstd = sb("rstd", [128, TT])
        small = sb("small", [128, 64])
        ARENA_N = 20480
        arena = sb("arena", [128, ARENA_N])

        psF = [pst("psF%d" % i, [128, 512]) for i in range(6)]
        psB = [pst("psB%d" % i, [128, 1024], BF16) for i in range(2)]
        pctr = {'f': 0, 'b': 0}

        pmode = {'banks': list(range(6))}

        def pf():
            bl = pmode['banks']
            i = bl[pctr['f'] % len(bl)]
            pctr['f'] += 1
            return psF[i], ('psF', i)

        pctr['q'] = 0

        def pq():
            i = pctr['q'] % 16
            pctr['q'] += 1
            b_, q_ = i // 4, i % 4
            return psF[b_][:, q_ * 128:(q_ + 1) * 128], ('psq', b_, q_)

        def run_jobs(jobs):
            loaded = {0: wload(jobs[0][0])}
            for i, (parts, fn) in enumerate(jobs):
                if i + 1 < len(jobs):
                    loaded[i + 1] = wload(jobs[i + 1][0])
                fn(*loaded.pop(i))

        def pb():
            i = pctr['b'] % 2
            pctr['b'] += 1
            return psB[i], ('psB', i)

        def pc(name):
            o, w = PCOL[name]
            return o

        def mm(out, lhsT, rhs, r, w, start=True, stop=True, inc=None):
            S.op('pe', lambda e: e.matmul(out, lhsT, rhs, start=start, stop=stop), r, w, inc=(stop if inc is None else inc))

        def tr(out, in_, ident, r, w, inc=True):
            S.op('pe', lambda e: e.transpose(out, in_, ident), r, w, inc=inc)

        def act(out, in_, func, r, w, bias=0.0, scale=1.0, accum=None):
            if accum is None:
                S.op('act', lambda e: e.activation(out, in_, func, bias=bias, scale=scale), r, w)
            else:
                S.op('act', lambda e: e.activation(out, in_, func, bias=bias, scale=scale, accum_out=accum), r, w)

        def cp(eng, out, in_, r, w):
            if eng == 'act':
                S.op('act', lambda e: e.copy(out, in_), r, w)
            else:
                S.op(eng, lambda e: e.tensor_copy(out, in_), r, w)

        def tt(eng, out, a, b, op, r, w):
            S.op(eng, lambda e: e.tensor_tensor(out, a, b, op), r, w)

        def ts(eng, out, a, s1, op0, r, w, s2=None, op1=None):
            if op1 is None:
                S.op(eng, lambda e: e.tensor_scalar(out, a, s1, None, op0), r, w)
            else:
                S.op(eng, lambda e: e.tensor_scalar(out, a, s1, s2, op0, op1), r, w)

        def stt(out, in0, scalar, in1, op0, op1, r, w):
            S.op('dve', lambda e: e.scalar_tensor_tensor(out, in0, scalar, in1, op0, op1), r, w)

        def memset(eng, ap, val, w):
            S.op(eng, lambda e: e.memset(ap, val), (), w)

        def tap(name, src_ap, r, dst=None):
            if name in tap_d:
                S.dma('sp', tap_d[name] if dst is None else dst, src_ap, reads=r, semkey='tap_' + name)

        def AF32(off, n):
            return arena[:, off:off + n]

        def AB16(off, n):
            return arena[:, off:off + n].bitcast(BF16)

        for nm, t in [('identF', identF), ('blockones', blockones), ('mk1', mk1), ('mk2', mk2), ('cmask', cmask),
                      ('reset', reset)]:
            S.dma('sp', t[:], cd[nm], writes=[nm], semkey='c_' + nm)
        cp('dve', identB[:], identF[:], ['identF'], ['identB'])
        memset('dve', onesB[:], 1.0, ['onesB'])
        ts('dve', gnones[:], blockones[:], 1.0 / 64.0, ALU.mult, ['blockones'], ['gnones'])

        wctr = {'i': 0}

        twin = {}
        wsrc = {'w_in': w_in_d, 'w_in_vres': w_vres_d, 'w_uq': w_uq_d, 'w_ukv': w_ukv_d, 'w_out': w_out_d,
                'wq_mem': wq_d, 'wk_mem': wk_d, 'wv_mem': wv_d, 'wo_mem': wo_d, 'w_ff1': ff1_d, 'w_ff2': ff2_d}
        for nm_, ap_ in wsrc.items():
            twin[nm_] = nc.dram_tensor(nm_ + "_b", list(ap_.shape), BF16).ap()
        cur = {'l': 0, 'cc': 0}

        def wload(parts):
            i = wctr['i'] % 2
            wctr['i'] += 1
            key = ('wb', i)
            for (off, kc, ncols, src) in parts:
                tw_ = twin[src.name]
                src_b = bass.AP(tw_.tensor, src.offset, [list(x) for x in src.ap])
                dst = wb[i][:, off:off + kc * ncols].rearrange("p (k n) -> p k n", k=kc)
                lyr = cur['l'] - 1 if src.name == 'w_in_vres' else cur['l']
                S.dma('sp', dst, src_b, reads=[('wq', cur['l'])], writes=[key], semkey='wb%d' % i)
            return wb[i], key

        def convert_list(lyr):
            out_ = []
            for nm_, ap_ in wsrc.items():
                li = lyr - 1 if nm_ == 'w_in_vres' else lyr
                if li < 0:
                    continue
                sf = ap_[li].rearrange("(p a) c -> p (a c)", p=128)
                df = twin[nm_][li].rearrange("(p a) c -> p (a c)", p=128)
                n = sf.shape[1]
                nsp = 4 if n > 65536 else 1
                step = n // nsp
                for q_ in range(nsp):
                    out_.append((df[:, q_ * step:(q_ + 1) * step], sf[:, q_ * step:(q_ + 1) * step]))
            return out_

        def emit_convert(lyr, items):
            for dst, src in items:
                i = cur['cc']
                cur['cc'] += 1
                S.dma('pool', dst, src, reads=[('cc', i - 2)], writes=[('wq', lyr), ('cc', i)], semkey='cvt%d' % lyr)

        def wview(buf, off, kc, ncols):
            return buf[:, off:off + kc * ncols].rearrange("p (k n) -> p k n", k=kc)

        def rms_stats(src_fn, KC, dn, eps, rkeys, sq_buf=None, sqkey=None):
            sq = uT if sq_buf is None else sq_buf
            sqk = 'uT' if sqkey is None else sqkey
            for c in range(KC):
                act(sq[:, c, :], src_fn(c), AF.Square, rkeys, [(sqk, c)])
            p, pk = pf()
            for c in range(KC):
                mm(p[:, :], onesB[:], sq[:, c, :], ['onesB', (sqk, c)], [pk], start=(c == 0), stop=(c == KC - 1))
            act(rstd[:], p[:, :], AF.Sqrt, [pk], ['rstd'], bias=eps, scale=1.0 / dn)
            S.op('dve', lambda e: e.reciprocal(rstd[:], rstd[:]), ['rstd'], ['rstd'])

        def pre_norm(gname):
            rms_stats(lambda c: hT[:, c, :], 16, float(D), EPS, ['hT'])
            g0 = pc(gname)
            for c in range(16):
                stt(uT[:, c, :], hT[:, c, :], par[:, g0 + c:g0 + c + 1], rstd[:], ALU.mult, ALU.mult,
                    ['hT', 'par', 'rstd', ('uT', c)], [('uT', c)])

        def post_norm_res(z, zkey, gname):
            rms_stats(lambda c: z[:, c, :], 16, float(D), EPS, [zkey])
            g0 = pc(gname)
            for c in range(16):
                stt(z[:, c, :], z[:, c, :], par[:, g0 + c:g0 + c + 1], rstd[:], ALU.mult, ALU.mult,
                    [zkey, 'par', 'rstd'], [(zkey, 'n', c)])
                tt('pool', hT[:, c, :], hT[:, c, :], z[:, c, :], ALU.add, [(zkey, 'n', c), 'hT'], ['hT'])
            S.op('pool', lambda e: e.memset(small[:, 60:61], 0.0), [(zkey, 'n', c) for c in range(16)], [zkey])

        def prep_mem():
            mt = AF32(0, 2 * D).rearrange("p (b d) -> p b d", b=2)
            S.dma('sp', mt, mem_d.rearrange("(b p) d -> p b d", p=128), writes=['mt'], semkey='mt')
            mg = small[:, 0:16]
            S.dma('sp', mg, memg_d, writes=['mg'], semkey='mg')
            mf = AF32(2 * D, 16 * NMEM).rearrange("p (c t) -> p c t", c=16)
            for b in range(2):
                for c4 in range(4):
                    p, pk = pf()
                    for i in range(4):
                        c = c4 * 4 + i
                        tr(p[:, i * 128:(i + 1) * 128], mt[:, b, c * 128:(c + 1) * 128], identF[:], ['mt', 'identF'], [pk], inc=(i == 3))
                    cp('act', mf[:, c4 * 4:(c4 + 1) * 4, b * 128:(b + 1) * 128],
                       p[:, :].rearrange("p (i t) -> p i t", i=4), [pk], [('mf', b, c4)])
            mfk = [('mf', b, c4) for b in range(2) for c4 in range(4)]
            sq = AB16(2 * D + 16 * NMEM, 16 * NMEM // 2).rearrange("p (c t) -> p c t", c=16)
            for c in range(16):
                act(sq[:, c, :], mf[:, c, :], AF.Square, mfk, [('msq', c)])
            p, pk = pf()
            for c in range(16):
                mm(p[:, 0:NMEM], onesB[:], sq[:, c, :], ['onesB', ('msq', c)], [pk], start=(c == 0), stop=(c == 15))
            act(rstd[:, 0:NMEM], p[:, 0:NMEM], AF.Sqrt, [pk], ['rstd'], bias=EPS, scale=1.0 / D)
            S.op('dve', lambda e: e.reciprocal(rstd[:, 0:NMEM], rstd[:, 0:NMEM]), ['rstd'], ['rstd'])
            memT = AB16(2 * D + 16 * NMEM + 16 * NMEM // 2, 16 * NMEM // 2).rearrange("p (c t) -> p c t", c=16)
            for c in range(16):
                stt(memT[:, c, :], mf[:, c, :], mg[:, c:c + 1], rstd[:, 0:NMEM], ALU.mult, ALU.mult,
                    mfk + ['mg', 'rstd'], ['memT'])
            S.dma('sp', memT_d, AB16(2 * D + 16 * NMEM + 16 * NMEM // 2, 16 * NMEM // 2), reads=['memT'], writes=['memT_d'], semkey='memTs')

        def layer_setup(l):
            S.barrier()
            S.dma('sp', par[:], par_d[l], writes=['par'], semkey='par')
            o, w = PCOL['om']
            ts('dve', par[:, o:o + N_MU], par[:, MU_COLS:MU_COLS + N_MU], -1.0, ALU.mult, ['par'], ['par'], s2=1.0, op1=ALU.add)
            o2, _ = PCOL['omka']
            ka = pc('k_a')
            ts('dve', par[:, o2:o2 + 8], par[:, ka:ka + 8], -1.0, ALU.mult, ['par'], ['par'], s2=1.0, op1=ALU.add)
            S.dma('pool', lw[0:96, 0, :], w_up_d[l], writes=['lwts'], semkey='lw')
            S.dma('pool', lw[0:96, 1, :], a_up_d[l], writes=['lwts'], semkey='lw')
            S.dma('pool', lw[:, 2:4, :], g_up_d[l].rearrange("(k p) n -> p k n", p=128), writes=['lwts'], semkey='lw')
            if l > 0:
                S.dma('pool', lw[0:64, 4, :], v_up_d[l - 1], writes=['lwts'], semkey='lw')
            memset('dve', carry[:], 0.0, ['carry'])
            memset('dve', Sbd[:], 0.0, ['Sbd'])
            memT = AB16(0, 16 * NMEM // 2).rearrange("p (c t) -> p c t", c=16)
            S.dma('sp', AB16(0, 16 * NMEM // 2), memT_d, reads=['memT_d'], writes=['memT'], semkey='memTl')
            buf, key = wload([(0, 16, 512, wk_d[l].rearrange("(k p) n -> p k n", p=128))])
            wv = wview(buf, 0, 16, 512)
            for h in range(4):
                p, pk = pf()
                for c in range(16):
                    mm(p[:, 0:NMEM], wv[:, c, h * 128:(h + 1) * 128], memT[:, c, :], [key, 'memT'], [pk], start=(c == 0), stop=(c == 15))
                cp('act', kmT[:, h, :], p[:, 0:NMEM], [pk], ['kmT'])
            buf, key = wload([(0, 16, 512, wv_d[l].rearrange("(k p) n -> p k n", p=128))])
            wv = wview(buf, 0, 16, 512)
            for b in range(2):
                p, pk = pf()
                for c in range(16):
                    mm(p[:, :], memT[:, c, b * 128:(b + 1) * 128], wv[:, c, :], [key, 'memT'], [pk], start=(c == 0), stop=(c == 15))
                cp('act', vm[:, b, :], p[:, :], [pk], ['vm'])

        def load_tile(l, j):
            if l == 0:
                xs = AF32(0, D)
                for blk in range(4):
                    S.dma('sp', xs, x_d[j * TT + blk * 128: j * TT + (blk + 1) * 128, :], writes=['xs'], semkey='xs')
                    for c4 in range(4):
                        p, pk = pf()
                        for i in range(4):
                            c = c4 * 4 + i
                            tr(p[:, i * 128:(i + 1) * 128], xs[:, c * 128:(c + 1) * 128], identF[:], ['xs', 'identF'], [pk], inc=(i == 3))
                        cp('act' if c4 % 2 else 'dve', hT[:, c4 * 4:(c4 + 1) * 4, blk * 128:(blk + 1) * 128],
                           p[:, :].rearrange("p (i t) -> p i t", i=4), [pk], [('hT', 'ld', blk, c4)])
                S.op('pool', lambda e: e.memset(small[:, 61:62], 0.0), [('hT', 'ld', blk, c4) for blk in range(4) for c4 in range(4)], ['hT'])
            else:
                S.dma('sp', hT[:], hT_d[:, j * TT:(j + 1) * TT].rearrange("(c p) t -> p c t", p=128),
                      reads=[('hT_d', j)], writes=['hT'], semkey='hT')

        def store_tile(l, j):
            if l == L - 1:
                S.barrier()
                os_ = AF32(0, D)
                for blk in range(4):
                    for c4 in range(4):
                        p, pk = pf()
                        for i in range(4):
                            c = c4 * 4 + i
                            tr(p[:, i * 128:(i + 1) * 128], hT[:, c, blk * 128:(blk + 1) * 128], identF[:], ['hT', 'identF'], [pk], inc=(i == 3))
                        cp('act' if c4 % 2 else 'dve', os_[:, c4 * 512:(c4 + 1) * 512], p[:, :], [pk], [('os', c4)])
                    S.dma('sp', out_d[j * TT + blk * 128: j * TT + (blk + 1) * 128, :], os_,
                          reads=[('os', c4) for c4 in range(4)], writes=[('out', j, blk)], semkey='os')
            else:
                S.dma('sp', hT_d[:, j * TT:(j + 1) * TT].rearrange("(c p) t -> p c t", p=128), hT[:],
                      reads=['hT'], writes=[('hT_d', j)], semkey='hT')

        def phase_rwkv(l, j):
            S.barrier()
            o = 0

            def slot(n):
                nonlocal o
                a_ = (o, n)
                o += n
                return a_
            s_ar = slot(8 * 192)
            s_bt = slot(8 * 128)
            s_kt = slot(8 * 128)
            s_bh = slot(8 * 128)
            s_kh = slot(8 * 128)
            s_vh = slot(8 * 128)
            DEAD = ['r', 'v', 'kk', 'a', 'lw', 'G', 'e1', 'e2', 'k2', 'b']
            names = DEAD + ['k', 'g', 'bon', 't1', 't2']
            dead0 = o
            sl = {n: slot(TT) for n in names}
            s_ymu = [slot(TT + 1), slot(TT + 1)]
            s_x = [slot(128) for _ in range(2)]
            s_u = [slot(128) for _ in range(2)]
            tw = AB16(o, TT // 2); o += TT // 2
            alb = AB16(o, TT // 2); o += TT // 2
            sg = AB16(o, TT).rearrange("p (k t) -> p k t", k=2); o += TT
            vrb = AB16(o, TT // 2); o += TT // 2
            assert o <= ARENA_N, o
            AR = AF32(*s_ar).rearrange("p (c n) -> p c n", c=8)
            BT = AF32(*s_bt).rearrange("p (c n) -> p c n", c=8)
            KT = AF32(*s_kt).rearrange("p (c n) -> p c n", c=8)
            BH = AF32(*s_bh).rearrange("p (c n) -> p c n", c=8)
            KH = AF32(*s_kh).rearrange("p (c n) -> p c n", c=8)
            VH = AF32(*s_vh).rearrange("p (c n) -> p c n", c=8)
            V = {n: AF32(*sl[n]) for n in names}
            CB = {'dead0': dead0}
            for c in range(8):
                base = dead0 + c * 640
                CB[c] = dict(AB=AF32(base, 192), AK=AF32(base + 192, 192), Pt=AF32(base + 384, 128), Mm=AF32(base + 512, 128))
            ck8 = lambda nm: [(nm, c) for c in range(8)]
            for nm, t in [('AR', AR), ('BT', BT), ('KT', KT), ('BH', BH), ('KH', KH), ('VH', VH)]:
                memset('pool', t, 0.0, ck8(nm))

            pre_norm('mix_pre_g')
            ymu_i = [0]

            def shift_evac(p, pk, M, cid, out_ap, wkeys):
                yb = AF32(*s_ymu[ymu_i[0] % 2])
                yk = ('ymu', ymu_i[0] % 2)
                ymu_i[0] += 1
                cp('pool', yb[0:M, 0:1], carry[0:M, cid:cid + 1], ['carry'], [yk])
                S.op('act', lambda e: e.mul(yb[0:M, 1:TT + 1], p, par[0:M, MU_COLS + cid:MU_COLS + cid + 1]), [pk, 'par', yk], [yk])
                oo = PCOL['om'][0]
                stt(out_ap, p, par[0:M, oo + cid:oo + cid + 1], yb[0:M, 0:TT], ALU.mult, ALU.add, [pk, 'par', yk], wkeys)
                cp('pool', carry[0:M, cid:cid + 1], yb[0:M, TT:TT + 1], [yk], ['carry'])

            def proj(lhs_fn, M, rkeys):
                p, pk = pf()
                for c in range(16):
                    mm(p[0:M, :], lhs_fn(c), uT[:, c, :], rkeys + [('uT', c)], [pk], start=(c == 0), stop=(c == 15))
                return p[0:M, :], pk
            t1 = V['t1']

            def lora_job(buf, key):
                wl_ = wview(buf, 0, 16, 448)
                wvr = wview(buf, 16 * 448, 16, 64)
                p, pk = proj(lambda c: wl_[:, c, 0:96], 96, [key])
                shift_evac(p, pk, 96, 24, t1[0:96, :], ['t1'])
                act(tw[0:96, :], t1[0:96, :], AF.Tanh, ['t1'], ['tw'])
                p, pk = proj(lambda c: wl_[:, c, 96:192], 96, [key])
                shift_evac(p, pk, 96, 25, alb[0:96, :], ['alb'])
                for i in range(2):
                    p, pk = proj(lambda c, i=i: wl_[:, c, 192 + 128 * i:320 + 128 * i], 128, [key])
                    shift_evac(p, pk, 128, 26 + i, t1[:, :], ['t1'])
                    act(sg[:, i, :], t1[:, :], AF.Sigmoid, ['t1'], ['sg'])
                if l > 0:
                    p, pk = proj(lambda c: wvr[:, c, :], 64, [key])
                    shift_evac(p, pk, 64, 28, vrb[0:64, :], ['vrb'])

            def pair_job(pr):
                def fn(buf, key):
                    for i, nm in enumerate(['r', 'k', 'v']):
                        wv_ = wview(buf, i * 16 * 128, 16, 128)
                        p, pk = proj(lambda c, wv_=wv_: wv_[:, c, :], 128, [key])
                        shift_evac(p, pk, 128, 8 * i + pr, V[nm], [nm])
                    rwkv_pair(l, j, pr, V, AR, BT, KT, BH, KH, VH, tw, alb, sg, vrb, CB, DEAD, s_x, s_u)
                return fn
            parts = [(0, 16, 448, w_in_d[l][:, 3072:3520].rearrange("(k p) n -> p k n", p=128))]
            if l > 0:
                parts.append((16 * 448, 16, 64, w_vres_d[l - 1].rearrange("(k p) n -> p k n", p=128)))
            jobs = [(parts, lora_job)]
            for pr in range(8):
                parts = [(i * 16 * 128, 16, 128, w_in_d[l][:, i * 1024 + pr * 128: i * 1024 + (pr + 1) * 128].rearrange("(k p) n -> p k n", p=128))
                         for i in range(3)]
                jobs.append((parts, pair_job(pr)))
            run_jobs(jobs)
            pmode['banks'] = list(range(6))

        def rwkv_pair(l, j, pr, V, AR, BT, KT, BH, KH, VH, tw, alb, sg, vrb, CB, DEAD, s_x, s_u):
            cs = slice(pr * 128, (pr + 1) * 128)
            col = lambda n: par[:, pc(n) + pr:pc(n) + pr + 1]
            r, k, v, kk, a, lwv, G, e1, e2, g, bon, k2, b, t1, t2 = [V[n] for n in
                ['r', 'k', 'v', 'kk', 'a', 'lw', 'G', 'e1', 'e2', 'g', 'bon', 'k2', 'b', 't1', 't2']]
            O = k
            ck8 = lambda nm: [(nm, c) for c in range(8)]
            p, pk = pf()
            mm(p[:, :], lw[0:96, 0, cs], tw[0:96, :], ['lwts', 'tw'], [pk])
            act(lwv, p[:, :], AF.Sigmoid, [pk, 'par'], ['lw'], bias=col('w0'))
            ts('pool', lwv, lwv, DECAY_C, ALU.mult, ['lw'], ['lw'])
            p, pk = pf()
            mm(p[:, :], lw[0:96, 1, cs], alb[0:96, :], ['lwts', 'alb'], [pk])
            act(a, p[:, :], AF.Sigmoid, [pk, 'par'], ['a'], bias=col('a0'))
            p, pk = pf()
            for i in range(2):
                mm(p[:, :], lw[:, 2 + i, cs], sg[:, i, :], ['lwts', 'sg'], [pk], start=(i == 0), stop=(i == 1))
            cp('act', g, p[:, :], [pk], ['g'])
            if l > 0:
                p, pk = pf()
                mm(p[:, :], lw[0:64, 4, cs], vrb[0:64, :], ['lwts', 'vrb'], [pk])
                act(t1, p[:, :], AF.Sigmoid, [pk, 'par'], ['t1'], bias=col('v0'))
                S.dma('sp', t2, vf_d[pr * 128:(pr + 1) * 128, j * TT:(j + 1) * TT], reads=[('vf_d', pr, j)], writes=['t2'], semkey='vfl')
                tt('dve', t2, t2, v, ALU.subtract, ['t2', 'v'], ['t2'])
                tt('dve', t2, t2, t1, ALU.mult, ['t2', 't1'], ['t2'])
                tt('dve', v, v, t2, ALU.add, ['v', 't2'], ['v'])
            else:
                S.dma('sp', vf_d[pr * 128:(pr + 1) * 128, j * TT:(j + 1) * TT], v, reads=['v'], writes=[('vf_d', pr, j)], semkey='vfs')
            ts('pool', kk, k, col('k_k'), ALU.mult, ['k', 'par'], ['kk'])
            tt('pool', t1, kk, kk, ALU.mult, ['kk'], ['t1'])
            p, pk = pf()
            mm(p[:, :], blockones[:], t1, ['blockones', 't1'], [pk])
            act(t2, p[:, :], AF.Sqrt, [pk], ['t2'])
            ts('dve', t2, t2, 1e-12, ALU.max, ['t2'], ['t2'])
            S.op('dve', lambda e: e.reciprocal(t2, t2), ['t2'], ['t2'])
            tt('dve', kk, kk, t2, ALU.mult, ['kk', 't2'], ['kk'])
            oka = PCOL['omka'][0]
            ts('dve', t1, a, col('k_a'), ALU.mult, ['a', 'par'], ['t1'], s2=par[:, oka + pr:oka + pr + 1], op1=ALU.add)
            tt('dve', k2, k, t1, ALU.mult, ['k', 't1'], ['k2'])
            tt('pool', b, kk, a, ALU.mult, ['kk', 'a'], ['b'])
            stt(t1, r, col('r_k'), k2, ALU.mult, ALU.mult, ['r', 'par', 'k2'], ['t1'])
            p, pk = pf()
            mm(p[:, :], blockones[:], t1, ['blockones', 't1'], [pk])
            tt('dve', bon, p[:, :], v, ALU.mult, [pk, 'v'], ['bon'])
            S.op('dve', lambda e: e.tensor_tensor_scan(G, reset[:], lwv, 0.0, ALU.mult, ALU.add), ['reset', 'lw'], ['G'])
            c3 = lambda ap: ap.rearrange("p (c t) -> p c t", c=8)
            hs = [(slice(0, 64), slice(0, 64)), (slice(64, 128), slice(64, 128))]
            act(e1, G, AF.Exp, ['G'], ['e1'])
            tt('dve', AR[:, :, 128:192], c3(r), c3(e1), ALU.mult, ['r', 'e1'], ck8('AR'))
            tt('pool', e2, G, lwv, ALU.subtract, ['G', 'lw'], ['e2'])
            act(e2, e2, AF.Exp, ['e2'], ['e2'])
            for ps_, fs_ in hs:
                stt(AR[ps_, :, fs_], c3(kk)[ps_], -1.0, c3(e2)[ps_], ALU.mult, ALU.mult, ['kk', 'e2'], ck8('AR'))
            act(e1, G, AF.Exp, ['G'] + ck8('AR'), ['e1'], scale=-1.0)
            for ps_, fs_ in hs:
                tt('dve', BT[ps_, :, fs_], c3(b)[ps_], c3(e1)[ps_], ALU.mult, ['b', 'e1'], ck8('BT'))
                tt('pool', KT[ps_, :, fs_], c3(k2)[ps_], c3(e1)[ps_], ALU.mult, ['k2', 'e1'], ck8('KT'))
            for c in range(8):
                act(e2[:, c * 64:(c + 1) * 64], G[:, c * 64:(c + 1) * 64], AF.Exp, ['G', 'e2'], ['e2'],
                    bias=G[:, c * 64 + 63:c * 64 + 64], scale=-1.0)
            for ps_, fs_ in hs:
                tt('dve', BH[ps_, :, fs_], c3(b)[ps_], c3(e2)[ps_], ALU.mult, ['b', 'e2'], ck8('BH'))
                tt('pool', KH[ps_, :, fs_], c3(k2)[ps_], c3(e2)[ps_], ALU.mult, ['k2', 'e2'], ck8('KH'))
                cp('pool', VH[ps_, :, fs_], c3(v)[ps_], ['v'], ck8('VH'))
            WC = small[:, 16:24]
            act(WC, G.rearrange("p (c t) -> p c t", c=8)[:, :, 63], AF.Exp, ['G'], ['WC'])
            if l == 0 and j == 0 and pr == 0:
                tap('v', v, ['v'])
                tap('r', r, ['r'])
                tap('kk', kk, ['kk'])
                tap('G', G, ['G'])

            cbk = [(nm, g_) for nm in ('AB', 'AK', 'Pt', 'Mm') for g_ in range(2)]
            S.op('pool', lambda e: e.memset(small[:, 58:59], 0.0), [], list(DEAD) + cbk)

            def gview(g_, field, lo, hi):
                off = {'AB': 0, 'AK': 192, 'Pt': 384, 'Mm': 512}[field]
                base = CB['dead0'] + g_ * 4 * 640
                return arena[:, base:base + 4 * 640].rearrange("p (c n) -> p c n", n=640)[:, :, off + lo:off + hi]
            bc4 = lambda t_: t_[:].rearrange("p (o n) -> p o n", o=1).to_broadcast([128, 4, 128])

            for c in range(8):
                p, pk = pf()
                tr(p[:, 0:128], BH[:, c, :], identF[:], [('BH', c), 'identF'], [pk], inc=False)
                tr(p[:, 128:256], KH[:, c, :], identF[:], [('KH', c), 'identF'], [pk], inc=False)
                tr(p[:, 256:384], VH[:, c, :], identF[:], [('VH', c), 'identF'], [pk], inc=True)
                cp('act', BH[:, c, :], p[:, 0:128], [pk], [('BH', c)])
                cp('act', KH[:, c, :], p[:, 128:256], [pk], [('KH', c)])
                cp('act', VH[:, c, :], p[:, 256:384], [pk], [('VH', c)])
            for c in range(8):
                g_ = c // 4
                p, pk = pf()
                mm(p[:, 0:192], BT[:, c, :], AR[:, c, :], [('BT', c), ('AR', c)], [pk], inc=False)
                mm(p[:, 256:448], KT[:, c, :], AR[:, c, :], [('KT', c), ('AR', c)], [pk], start=True, stop=True)
                tt('dve', CB[c]['AB'], p[:, 0:192], mk1[:], ALU.mult, [pk, 'mk1'], [('AB', g_)])
                tt('dve', CB[c]['AK'], p[:, 256:448], mk1[:], ALU.mult, [pk, 'mk1'], [('AK', g_)])
            for g_ in range(2):
                p, pk = pf()
                for i in range(4):
                    c = g_ * 4 + i
                    mm(p[:, i * 128:(i + 1) * 128], AR[:, c, 0:128], BT[:, c, :], [('BT', c), ('AR', c)], [pk], inc=(i == 3))
                for i in range(4):
                    c = g_ * 4 + i
                    tt('dve', CB[c]['Pt'], p[:, i * 128:(i + 1) * 128], mk2[:], ALU.mult, [pk, 'mk2'], [('Pt', g_)])
                    tt('pool', CB[c]['Mm'], CB[c]['AB'][:, 0:128], identF[:], ALU.add, [('AB', g_), 'identF'], [('Mm', g_)])
            for kq in range(1, 6):
                for g_ in range(2):
                    if kq < 5:
                        pA, pAk = pf()
                        for i in range(4):
                            c = g_ * 4 + i
                            mm(pA[:, i * 128:(i + 1) * 128], CB[c]['Pt'], CB[c]['AB'][:, 0:128], [('Pt', g_), ('AB', g_)], [pAk], inc=(i == 3))
                    pB, pBk = pf()
                    for i in range(4):
                        c = g_ * 4 + i
                        mm(pB[:, i * 128:(i + 1) * 128], CB[c]['AB'][:, 0:128], CB[c]['Pt'], [('Pt', g_), ('AB', g_)], [pBk], inc=(i == 3))
                    if False:
                        for i in range(4):
                            c = g_ * 4 + i
                            if kq < 5:
                                cp('act', CB[c]['AB'][:, 0:128], pA[:, i * 128:(i + 1) * 128], [pAk], [('AB', g_)])
                            cp('dve' if g_ else 'act', CB[c]['Pt'], pB[:, i * 128:(i + 1) * 128], [pBk], [('Pt', g_)])
                    else:
                        if kq < 5:
                            cp('act', gview(g_, 'AB', 0, 128), pA[:, :].rearrange("p (c n) -> p c n", c=4), [pAk], [('AB', g_)])
                        cp('dve' if g_ else 'act', gview(g_, 'Pt', 0, 128), pB[:, :].rearrange("p (c n) -> p c n", c=4), [pBk], [('Pt', g_)])
                for g_ in range(2):
                    pC, pCk = pf()
                    for i in range(4):
                        c = g_ * 4 + i
                        mm(pC[:, i * 128:(i + 1) * 128], CB[c]['Pt'], CB[c]['Mm'], [('Pt', g_), ('Mm', g_)], [pCk], inc=(i == 3))
                    if False:
                        for i in range(4):
                            c = g_ * 4 + i
                            tt('dve', CB[c]['Mm'], pC[:, i * 128:(i + 1) * 128], CB[c]['Mm'], ALU.add, [pCk, ('Mm', g_)], [('Mm', g_)])
                    else:
                        tt('dve', gview(g_, 'Mm', 0, 128), pC[:, :].rearrange("p (c n) -> p c n", c=4), gview(g_, 'Mm', 0, 128), ALU.add,
                           [pCk, ('Mm', g_)], [('Mm', g_)])

            for c in range(8):
                ci = c % 2
                g_ = c // 4
                AB, AK, Tt = CB[c]['AB'], CB[c]['AK'], CB[c]['Mm']
                abk, akk, Ttk = ('AB', g_), ('AK', g_), ('Mm', g_)
                Scur = Sbd[:, pr, c % 2, :]
                Snxt = Sbd[:, pr, (c + 1) % 2, :]
                sck, snk = ('Sbd', pr, c % 2), ('Sbd', pr, (c + 1) % 2)
                X = AF32(*s_x[ci]); xk = ('X', ci)
                U = AF32(*s_u[ci]); uk_ = ('U', ci)
                p, pk = pf()
                mm(p[:, 0:128], AR[:, c, 0:128], Scur, [('AR', c), sck, 'Sbd'], [pk], start=True, stop=False)
                mm(p[:, 0:128], AK[:, 0:128], VH[:, c, :], [akk, ('VH', c)], [pk], start=False, stop=True)
                cp('act', X, p[:, 0:128], [pk], [xk])
                p, pk = pf()
                mm(p[:, 0:128], Tt, X, [Ttk, xk], [pk])
                cp('act', U, p[:, 0:128], [pk], [uk_])
                p, pk = pf()
                mm(p[:, 0:128], BH[:, c, :], U, [('BH', c), uk_], [pk], start=True, stop=False)
                mm(p[:, 0:128], KH[:, c, :], VH[:, c, :], [('KH', c), ('VH', c)], [pk], start=False, stop=True)
                stt(Snxt, Scur, WC[:, c:c + 1], p[:, 0:128], ALU.mult, ALU.add, [sck, 'WC', pk, 'Sbd'], [snk])
                p, pk = pf()
                mm(p[:, 0:64], Scur, AR[:, c, 128:192], [sck, ('AR', c), 'Sbd'], [pk], start=True, stop=False)
                mm(p[:, 0:64], U, AB[:, 128:192], [uk_, abk], [pk], start=False, stop=False)
                mm(p[:, 0:64], VH[:, c, :], AK[:, 128:192], [('VH', c), akk], [pk], start=False, stop=True)
                cp('dve', O[:, c * 64:(c + 1) * 64], p[:, 0:64], [pk, 'k'], ['k'])
            S.op('pool', lambda e: e.memset(small[:, 57:58], 0.0), [], list(DEAD) + cbk)
            p, pk = pf()
            mm(p[:, :], gnones[:], O, ['gnones', 'k'], [pk])
            tt('dve', t1, O, p[:, :], ALU.subtract, ['k', pk], ['t1'])
            tt('pool', t2, t1, t1, ALU.mult, ['t1'], ['t2'])
            p, pk = pf()
            mm(p[:, :], gnones[:], t2, ['gnones', 't2'], [pk])
            act(t2, p[:, :], AF.Sqrt, [pk], ['t2'], bias=GN_EPS)
            S.op('dve', lambda e: e.reciprocal(t2, t2), ['t2'], ['t2'])
            tt('dve', t1, t1, t2, ALU.mult, ['t1', 't2'], ['t1'])
            ts('dve', t1, t1, col('lnx_g'), ALU.mult, ['t1', 'par'], ['t1'], s2=col('lnx_b'), op1=ALU.add)
            tt('pool', t1, t1, bon, ALU.add, ['t1', 'bon'], ['t1'])
            tt('dve', yT[:, pr, :], t1, g, ALU.mult, ['t1', 'g'], [('yT', pr)])
            if l == 0 and j == 0 and pr == 0:
                tap('O', O, ['k'])
                tap('yr', t1, ['t1'])

        def phase_mla(l, j):
            S.barrier()
            o = 0

            def slot(n):
                nonlocal o
                a_ = (o, n)
                o += n
                return a_
            NK = (j + 1) * TT
            s_S = slot(max(T, 8 * TT))
            s_cq = (s_S[0], 4 * TT)
            s_ckv = (s_S[0] + 4 * TT, 2 * TT)
            s_t1 = (s_S[0] + 6 * TT, TT)
            s_t2 = (s_S[0] + 7 * TT, TT)

            def b16(n_units):
                nonlocal o
                v_ = AB16(o, n_units)
                o += n_units
                return v_
            s_rope = (o, 2 * TT)
            P_ = b16(max(T // 2, 2 * TT))
            Kh = b16(T // 2)
            Vh = b16(T // 2).rearrange("p (b d) -> p b d", d=128)
            krl = b16(T // 2)
            qT = b16(8 * TT // 2).rearrange("p (h t) -> p h t", h=8)
            qrT = b16(8 * TT // 2).rearrange("p (h t) -> p h t", h=8)
            cqn = b16(4 * TT // 2).rearrange("p (c t) -> p c t", c=4)
            ckvn = b16(2 * TT // 2).rearrange("p (c t) -> p c t", c=2)
            knst = b16(TT // 2)
            krst = b16(TT // 2)
            vst = [b16(512), b16(512)]
            PT = [b16(256), b16(256)]
            osb = b16(64)
            rsum = small[:, 24:26]
            mx = small[:, 26:27]
            assert o <= ARENA_N, o
            Ssb = AF32(*s_S)
            cq = AF32(*s_cq).rearrange("p (c t) -> p c t", c=4)
            ckv = AF32(*s_ckv).rearrange("p (c t) -> p c t", c=2)
            tmp1 = AF32(*s_t1)
            tmp2 = AF32(*s_t2)
            rope = AF32(*s_rope).rearrange("p (a t) -> p a t", a=2)
            S.dma('sp', rope[0:64], cd['rope'][:, :, j * TT:(j + 1) * TT].rearrange("a p t -> p a t"), writes=['rope'], semkey='rope')

            def proj(lhs_fn, M, rkeys):
                p, pk = pf()
                for c in range(16):
                    mm(p[0:M, :], lhs_fn(c), uT[:, c, :], rkeys + [('uT', c)], [pk], start=(c == 0), stop=(c == 15))
                return p, pk
            buf, key = wload([(0, 16, 512, w_in_d[l][:, 3520:4032].rearrange("(k p) n -> p k n", p=128))])
            wv_ = wview(buf, 0, 16, 512)
            for c in range(4):
                p, pk = proj(lambda cc, c=c: wv_[:, cc, c * 128:(c + 1) * 128], 128, [key])
                cp('act', cq[:, c, :], p[:, :], [pk], ['cq'])
            buf, key = wload([(0, 16, 320, w_in_d[l][:, 4032:4352].rearrange("(k p) n -> p k n", p=128))])
            wv_ = wview(buf, 0, 16, 320)
            for c in range(2):
                p, pk = proj(lambda cc, c=c: wv_[:, cc, c * 128:(c + 1) * 128], 128, [key])
                cp('act', ckv[:, c, :], p[:, :], [pk], ['ckv'])

            def rope_mm(lhs_fn, nkc, rhs_fn, rkeys):
                p1, pk1 = pf()
                for c in range(nkc):
                    mm(p1[0:64, :], lhs_fn(c, 0, 64), rhs_fn(c), rkeys(c), [pk1], start=(c == 0), stop=(c == nkc - 1))
                p2, pk2 = pf()
                for c in range(nkc):
                    mm(p2[0:32, :], lhs_fn(c, 32, 64), rhs_fn(c), rkeys(c), [pk2], start=(c == 0), stop=(c == nkc - 1))
                for c in range(nkc):
                    mm(p2[32:64, :], lhs_fn(c, 0, 32), rhs_fn(c), rkeys(c), [pk2], start=(c == 0), stop=(c == nkc - 1))
                tt('dve', tmp1[0:64, :], p1[0:64, :], rope[0:64, 0, :], ALU.mult, [pk1, 'rope'], ['tmp1'])
                tt('dve', tmp2[0:64, :], p2[0:64, :], rope[0:64, 1, :], ALU.mult, [pk2, 'rope'], ['tmp2'])
                tt('dve', tmp1[0:64, :], tmp1[0:64, :], tmp2[0:64, :], ALU.add, ['tmp1', 'tmp2'], ['tmp1'])

            kb = 256
            rope_mm(lambda c, lo, hi: wv_[:, c, kb + lo:kb + hi], 16, lambda c: uT[:, c, :], lambda c: [key, ('uT', c)])
            cp('act', krst[0:64, :], tmp1[0:64, :], ['tmp1'], ['krst'])
            S.dma('sp', kr_d[:, j * TT:(j + 1) * TT], krst[0:64, :], reads=['krst'], writes=[('kr_d', j)], semkey='krst')

            rms_stats(lambda c: cq[:, c, :], 4, 512.0, EPS, ['cq'], sq_buf=qT, sqkey='qTs')
            g0 = pc('qn_g')
            for c in range(4):
                stt(cqn[:, c, :], cq[:, c, :], par[:, g0 + c:g0 + c + 1], rstd[:], ALU.mult, ALU.mult, ['cq', 'par', 'rstd'], ['cqn'])
            rms_stats(lambda c: ckv[:, c, :], 2, 256.0, EPS, ['ckv'], sq_buf=qT, sqkey='qTs')
            g0 = pc('kvn_g')
            for c in range(2):
                stt(ckvn[:, c, :], ckv[:, c, :], par[:, g0 + c:g0 + c + 1], rstd[:], ALU.mult, ALU.mult, ['ckv', 'par', 'rstd'], ['ckvn'])

            SC = float((128 + 64) ** -0.5)
            buf, key = wload([(0, 4, 1536, w_uq_d[l].rearrange("(k p) n -> p k n", p=128))])
            wq_ = wview(buf, 0, 4, 1536)
            sqk = [('qTs', c) for c in range(4)]
            for h in range(8):
                p, pk = pf()
                for c in range(4):
                    mm(p[:, :], wq_[:, c, h * 192:h * 192 + 128], cqn[:, c, :], [key, 'cqn'], [pk], start=(c == 0), stop=(c == 3))
                S.op('act', lambda e, p=p, h=h: e.mul(qT[:, h, :], p[:, :], SC), [pk] + sqk, [('qTh', h)] + (sqk if h < 4 else []))
                rb = h * 192 + 128
                rope_mm(lambda c, lo, hi, rb=rb: wq_[:, c, rb + lo:rb + hi], 4, lambda c: cqn[:, c, :], lambda c: [key, 'cqn'])
                S.op('act', lambda e, h=h: e.mul(qrT[0:64, h, :], tmp1[0:64, :], SC), ['tmp1'], [('qrT', h)])
            buf, key = wload([(0, 2, 2048, w_ukv_d[l].rearrange("(k p) n -> p k n", p=128))])
            wkv_ = wview(buf, 0, 2, 2048)
            for h in range(8):
                p, pk = pf()
                for c in range(2):
                    mm(p[:, :], wkv_[:, c, h * 256:h * 256 + 128], ckvn[:, c, :], [key, 'ckvn'], [pk], start=(c == 0), stop=(c == 1))
                cp('act', knst[:, :], p[:, :], [pk], ['knst'])
                S.dma('sp', kn_d[h][:, j * TT:(j + 1) * TT], knst[:, :], reads=['knst'], writes=[('kn_d', h, j)], semkey='knst')
            wkv4 = wkv_.rearrange("p k (h two d) -> p k h two d", h=8, two=2)
            for tb in range(4):
                for hg in range(2):
                    p, pk = pf()
                    for c in range(2):
                        mm(p[:, :].rearrange("p (h d) -> p h d", h=4), ckvn[:, c, tb * 128:(tb + 1) * 128],
                           wkv4[:, c, hg * 4:(hg + 1) * 4, 1, :], [key, 'ckvn'], [pk], start=(c == 0), stop=(c == 1))
                    cp('act', vst[tb % 2][:, hg * 512:(hg + 1) * 512], p[:, :], [pk], [('vst', tb % 2)])
                S.dma('sp', vt_d[j * TT + tb * 128:j * TT + (tb + 1) * 128, :], vst[tb % 2], reads=[('vst', tb % 2)],
                      writes=[('vt_d', j, tb)], semkey='vst%d' % (tb % 2))
            S.barrier()
            S.dma('sp', krl[0:64, 0:NK], kr_d[:, 0:NK], reads=[('kr_d', jj) for jj in range(j + 1)], writes=['krl'], semkey='krl')
            for h in range(8):
                S.dma('sp', Kh[:, 0:NK], kn_d[h][:, 0:NK], reads=[('kn_d', h, jj) for jj in range(j + 1)], writes=['Kh'], semkey='Kh')
                S.dma('sp', Vh[:, 0:NK // 128, :], vt_d[0:NK, h * 128:(h + 1) * 128].rearrange("(b p) d -> p b d", p=128),
                      reads=[('vt_d', jj, tb_) for jj in range(j + 1) for tb_ in range(4)], writes=['Vh'], semkey='Vh')
                for qb in range(4):
                    q0 = j * TT + qb * 128
                    nk = q0 + 128
                    qs = slice(qb * 128, (qb + 1) * 128)
                    ng = (nk + 511) // 512
                    for gi in range(ng):
                        k0 = gi * 512
                        kw = min(512, nk - k0)
                        p, pk = pf()
                        mm(p[:, 0:kw], qT[:, h, qs], Kh[:, k0:k0 + kw], [('qTh', h), 'Kh'], [pk], start=True, stop=False)
                        mm(p[:, 0:kw], qrT[0:64, h, qs], krl[0:64, k0:k0 + kw], [('qrT', h), 'krl'], [pk], start=False, stop=True)
                        if k0 + kw == nk:
                            if kw > 128:
                                cp('act', Ssb[:, k0:k0 + kw - 128], p[:, 0:kw - 128], [pk], ['Ssb'])
                            tt('dve', Ssb[:, nk - 128:nk], p[:, kw - 128:kw], cmask[:], ALU.add, [pk, 'cmask'], ['Ssb'])
                        else:
                            cp('act', Ssb[:, k0:k0 + kw], p[:, 0:kw], [pk], ['Ssb'])
                    S.op('dve', lambda e, nk=nk: e.reduce_max(mx, Ssb[:, 0:nk], mybir.AxisListType.X), ['Ssb'], ['mx'])
                    ts('dve', mx, mx, -1.0, ALU.mult, ['mx'], ['mx'])
                    act(P_[:, 0:nk], Ssb[:, 0:nk], AF.Exp, ['Ssb', 'mx'], ['P_', 'rsum'], bias=mx, accum=rsum[:, 0:1])
                    S.op('dve', lambda e: e.reciprocal(rsum[:, 1:2], rsum[:, 0:1]), ['rsum'], ['rinv'])
                    nb = nk // 128
                    po, pok = pf()
                    for b4 in range(0, nb, 4):
                        nbb = min(4, nb - b4)
                        pt_, ptk = pb()
                        for i in range(nbb):
                            tr(pt_[:, i * 128:(i + 1) * 128], P_[:, (b4 + i) * 128:(b4 + i + 1) * 128], identB[:], ['P_', 'identB'], [ptk], inc=(i == nbb - 1))
                        pti = (b4 // 4) % 2
                        cp('act' if pti else 'dve', PT[pti][:, 0:nbb * 128], pt_[:, 0:nbb * 128], [ptk], [('PT', pti)])
                        for i in range(nbb):
                            bb = b4 + i
                            mm(po[:, 0:128], PT[pti][:, i * 128:(i + 1) * 128], Vh[:, bb, :], [('PT', pti), 'Vh'], [pok],
                               start=(bb == 0), stop=(bb == nb - 1))
                    S.op('act', lambda e, po=po: e.mul(osb[:, :], po[:, 0:128], rsum[:, 1:2]), [pok, 'rinv'], ['osb'])
                    pt_, ptk = pb()
                    tr(pt_[:, 0:128], osb[:, :], identB[:], ['osb', 'identB'], [ptk])
                    cp('dve', yT[:, 8 + h, qs], pt_[:, 0:128], [ptk], [('yT', 8 + h)])

        def phase_ffn(l, j):
            S.barrier()
            z = AF32(0, 16 * TT).rearrange("p (c t) -> p c t", c=16)
            o = 16 * TT
            h1 = [AB16(o, 4 * TT // 2).rearrange("p (c t) -> p c t", c=4), AB16(o + 4 * TT // 2, 4 * TT // 2).rearrange("p (c t) -> p c t", c=4)]
            o += 4 * TT
            qm = AB16(o, 4 * TT // 2).rearrange("p (h t) -> p h t", h=4); o += 4 * TT // 2
            om = AB16(o, 4 * TT // 2).rearrange("p (h t) -> p h t", h=4); o += 4 * TT // 2
            Pm_ = AB16(o, 128); o += 128
            PTm = AB16(o, 128); o += 128
            osb = AB16(o, 64); o += 64
            rt = AF32(o, TT); o += TT
            assert o <= ARENA_N
            yk = [('yT', c) for c in range(16)]
            for nb in range(4):
                buf, key = wload([(0, 16, 512, w_out_d[l][:, nb * 512:(nb + 1) * 512].rearrange("(k p) n -> p k n", p=128))])
                wv_ = wview(buf, 0, 16, 512)
                for i in range(4):
                    n = nb * 4 + i
                    p, pk = pf()
                    for c in range(16):
                        mm(p[:, :], wv_[:, c, i * 128:(i + 1) * 128], yT[:, c, :], [key, ('yT', c)], [pk], start=(c == 0), stop=(c == 15))
                    cp('act' if n % 2 else 'dve', z[:, n, :], p[:, :], [pk], ['z'])
            post_norm_res(z, 'z', 'mix_post_g')
            pre_norm('mem_pre_g')
            buf, key = wload([(0, 16, 512, wq_d[l].rearrange("(k p) n -> p k n", p=128))])
            wv_ = wview(buf, 0, 16, 512)
            SCM = float(128 ** -0.5)
            for h in range(4):
                p, pk = pf()
                for c in range(16):
                    mm(p[:, :], wv_[:, c, h * 128:(h + 1) * 128], uT[:, c, :], [key, ('uT', c)], [pk], start=(c == 0), stop=(c == 15))
                S.op('act', lambda e, p=p, h=h: e.mul(qm[:, h, :], p[:, :], SCM), [pk], [('qm', h)])
            rsum = small[:, 28:30]
            mx = small[:, 30:31]
            for h in range(4):
                for qb in range(4):
                    qs = slice(qb * 128, (qb + 1) * 128)
                    p, pk = pf()
                    mm(p[:, 0:NMEM], qm[:, h, qs], kmT[:, h, :], [('qm', h), 'kmT'], [pk])
                    S.op('dve', lambda e, p=p: e.reduce_max(mx, p[:, 0:NMEM], mybir.AxisListType.X), [pk], ['mxm'])
                    ts('dve', mx, mx, -1.0, ALU.mult, ['mxm'], ['mxm'])
                    act(Pm_[:, :], p[:, 0:NMEM], AF.Exp, [pk, 'mxm'], ['Pm_', 'rsm'], bias=mx, accum=rsum[:, 0:1])
                    S.op('dve', lambda e: e.reciprocal(rsum[:, 1:2], rsum[:, 0:1]), ['rsm'], ['rim'])
                    pt_, ptk = pb()
                    for i in range(2):
                        tr(pt_[:, i * 128:(i + 1) * 128], Pm_[:, i * 128:(i + 1) * 128], identB[:], ['Pm_', 'identB'], [ptk], inc=(i == 1))
                    cp('dve', PTm[:, :], pt_[:, 0:256], [ptk], ['PTm'])
                    po, pok = pf()
                    for i in range(2):
                        mm(po[:, 0:128], PTm[:, i * 128:(i + 1) * 128], vm[:, i, h * 128:(h + 1) * 128], ['PTm', 'vm'], [pok], start=(i == 0), stop=(i == 1))
                    S.op('act', lambda e, po=po: e.mul(osb[:, :], po[:, 0:128], rsum[:, 1:2]), [pok, 'rim'], ['osbm'])
                    pt_, ptk = pb()
                    tr(pt_[:, 0:128], osb[:, :], identB[:], ['osbm', 'identB'], [ptk])
                    cp('dve', om[:, h, qs], pt_[:, 0:128], [ptk], ['om'])
            buf, key = wload([(0, 4, 2048, wo_d[l].rearrange("(k p) n -> p k n", p=128))])
            wv_ = wview(buf, 0, 4, 2048)
            for n in range(16):
                p, pk = pf()
                for c in range(4):
                    mm(p[:, :], wv_[:, c, n * 128:(n + 1) * 128], om[:, c, :], [key, 'om'], [pk], start=(c == 0), stop=(c == 3))
                cp('act' if n % 2 else 'dve', z[:, n, :], p[:, :], [pk], ['z'])
            post_norm_res(z, 'z', 'mem_post_g')
            pre_norm('ffn_pre_g')
            for blk in range(16):
                buf, key = wload([(0, 16, 512, ff1_d[l][:, blk * 512:(blk + 1) * 512].rearrange("(k p) n -> p k n", p=128))])
                wv_ = wview(buf, 0, 16, 512)
                hb = h1[blk % 2]
                hk = ('h1', blk % 2)
                for i in range(4):
                    p, pk = pf()
                    for c in range(16):
                        mm(p[:, :], wv_[:, c, i * 128:(i + 1) * 128], uT[:, c, :], [key, ('uT', c)], [pk], start=(c == 0), stop=(c == 15))
                    act(rt, p[:, :], AF.Relu, [pk], ['rt'])
                    tt('pool', hb[:, i, :], rt, rt, ALU.mult, ['rt'], [hk])
                buf, key = wload([(0, 4, 2048, ff2_d[l][blk * 512:(blk + 1) * 512, :].rearrange("(k p) n -> p k n", p=128))])
                wv_ = wview(buf, 0, 4, 2048)
                for n in range(16):
                    p, pk = pf()
                    for c in range(4):
                        mm(p[:, :], wv_[:, c, n * 128:(n + 1) * 128], hb[:, c, :], [key, hk], [pk], start=(c == 0), stop=(c == 3))
                    if blk == 0:
                        cp('act' if n % 2 else 'dve', z[:, n, :], p[:, :], [pk], [('zf', n)])
                    else:
                        tt('dve', z[:, n, :], z[:, n, :], p[:, :], ALU.add, [pk, ('zf', n)], [('zf', n)])
            S.op('pool', lambda e: e.memset(small[:, 59:60], 0.0), [('zf', n) for n in range(16)], ['z'])
            post_norm_res(z, 'z', 'ffn_post_g')

        emit_convert(0, convert_list(0))
        prep_mem()
        for l in range(L):
            cur['l'] = l
            layer_setup(l)
            nxt = convert_list(l + 1) if l + 1 < L else []
            per = (len(nxt) + NT - 1) // NT if nxt else 0
            for j in range(NT):
                S.barrier()
                if per:
                    emit_convert(l + 1, nxt[j * per:(j + 1) * per])
                load_tile(l, j)
                phase_rwkv(l, j)
                phase_mla(l, j)
                phase_ffn(l, j)
                store_tile(l, j)
        S.barrier(final=True)
        S.emit()
    return nc, cn


def make_in_maps(inputs, T, L, nb):
    cn = make_consts(T)
    maps = []
    f32 = lambda a: np.ascontiguousarray(np.asarray(a, np.float32))
    params = np.stack([pack_params(inputs, l) for l in range(L)], 0)
    shared = {
        "params": f32(params),
        "memg": f32(_fm(inputs['mem_norm_g'], 16)),
        "w_in": f32(inputs['w_in'][:L]),
        "w_in_vres": f32(inputs['w_in_vres'][:max(L - 1, 1)]),
        "w_up": f32(inputs['w_up'][:L]), "a_up": f32(inputs['a_up'][:L]), "g_up": f32(inputs['g_up'][:L]),
        "v_up": f32(inputs['v_up'][:max(L - 1, 1)]),
        "w_uq": f32(inputs['w_uq'][:L]), "w_ukv": f32(inputs['w_ukv'][:L]), "w_out": f32(inputs['w_out'][:L]),
        "wq_mem": f32(inputs['wq_mem'][:L]), "wk_mem": f32(inputs['wk_mem'][:L]), "wv_mem": f32(inputs['wv_mem'][:L]),
        "wo_mem": f32(inputs['wo_mem'][:L]), "w_ff1": f32(inputs['w_ff1'][:L]), "w_ff2": f32(inputs['w_ff2'][:L]),
    }
    for k, v in cn.items():
        shared["c_" + k] = f32(v)
    for b in range(nb):
        m = dict(shared)
        m["x"] = f32(inputs['x'][b][:T])
        m["mem"] = f32(inputs['mem'][b])
        maps.append(m)
    return maps


def kernel(**inputs):
    B, T, _ = inputs['x'].shape
    L = inputs['w_in'].shape[0]
    nc, _ = build(T, L)
    maps = make_in_maps(inputs, T, L, B)
    res = run_bass_kernel_spmd(nc, maps, core_ids=list(range(B)))
    return np.stack([np.asarray(res.results[b]["out"], np.float32) for b in range(B)], 0)
```

```python
import contextlib
import numpy as np
import concourse.bass as bass
import concourse.mybir as mybir
from concourse.bass_utils import run_bass_kernel_spmd

F32 = mybir.dt.float32
BF16 = mybir.dt.bfloat16
AF = mybir.ActivationFunctionType
ALU = mybir.AluOpType

D = 2048
TT = 512
NMEM = 256
RW = 1024
IN_W = 4352
DFF = 8192
GN_EPS = 64e-5
EPS = 1e-6
DECAY_C = -0.6065306597126334


class Sched:
    CE = ('pe', 'dve', 'act', 'pool')
    ENG = ('pe', 'dve', 'act', 'pool', 'sp')

    def __init__(self, nc, stack):
        self.nc = nc
        self.stack = stack
        self.q = {e: [] for e in self.ENG}
        self.semh = {}
        self.cnt = {}
        for e in self.CE:
            self._mksem('s_' + e)
        self.lastw = {}
        self.readers = {}
        self.seen = {e: {} for e in self.ENG}
        self.n_ops = 0

    def _mksem(self, name):
        self.semh[name] = self.stack.enter_context(self.nc.semaphore(name))
        self.cnt[name] = 0

    def _waits(self, eng, reads, writes, is_dma=False):
        need = {}
        own = None if is_dma else 's_' + eng

        def add(d, raw):
            if d is None:
                return
            if d[0] == own and not raw:
                return
            if need.get(d[0], 0) < d[1]:
                need[d[0]] = d[1]
        for k in reads:
            add(self.lastw.get(k), True)
        for k in writes:
            add(self.lastw.get(k), False)
            r = self.readers.get(k)
            if r:
                for s, v in r.items():
                    add((s, v), False)
        waits = []
        for s, v in need.items():
            if eng == 'pe' and s == 's_pe':
                continue
            if self.seen[eng].get(s, 0) < v:
                self.seen[eng][s] = v
                waits.append((s, v))
        return waits

    def _commit(self, ident, reads, writes):
        for k in writes:
            self.lastw[k] = ident
            self.readers[k] = {}
        for k in reads:
            r = self.readers.setdefault(k, {})
            if r.get(ident[0], 0) < ident[1]:
                r[ident[0]] = ident[1]

    def op(self, eng, fn, reads=(), writes=(), inc=True):
        waits = self._waits(eng, reads, writes)
        s = 's_' + eng
        if inc:
            self.cnt[s] += 1
            ident = (s, self.cnt[s])
        else:
            ident = (s, self.cnt[s] + 1)
        self._commit(ident, reads, writes)
        self.q[eng].append((waits, fn, (s, 1) if inc else None))
        self.n_ops += 1

    def dma(self, eng, out, in_, reads=(), writes=(), semkey=None, slow=False):
        assert semkey is not None
        s = 'd_' + semkey
        if s not in self.semh:
            self._mksem(s)
        waits = self._waits(eng, reads, writes, is_dma=True)
        self.cnt[s] += 16
        ident = (s, self.cnt[s])
        self._commit(ident, reads, writes)
        if slow:
            fn = lambda e: e.dma_start(out=out, in_=in_, allow_slow_non_contiguous=True)
        else:
            fn = lambda e: e.dma_start(out=out, in_=in_)
        self.q[eng].append((waits, fn, (s, 16)))
        self.n_ops += 1

    def barrier(self, engines=None, final=False):
        for e in (engines or self.ENG):
            waits = []
            for s, c in self.cnt.items():
                if c > 0 and self.seen[e].get(s, 0) < c:
                    if e == 'pe' and s == 's_pe':
                        continue
                    if s.startswith('d_cvt') and not final:
                        continue
                    self.seen[e][s] = c
                    waits.append((s, c))
            if waits:
                self.q[e].append((waits, None, None))

    def emit(self):
        engobj = {'pe': 'tensor', 'dve': 'vector', 'act': 'scalar', 'pool': 'gpsimd', 'sp': 'sync'}
        with self.nc.Block() as block:
            for e in self.ENG:
                items = self.q[e]
                if not items:
                    continue

                def body(eo, items=items):
                    for waits, fn, inc in items:
                        for s, v in waits:
                            eo.wait_ge(self.semh[s], v)
                        if fn is not None:
                            ins = fn(eo)
                            if inc is not None:
                                ins.then_inc(self.semh[inc[0]], inc[1])
                getattr(block, engobj[e])(body)


PCOL = {}
_off = 0
for _n, _w in [('mix_pre_g', 16), ('mix_post_g', 16), ('mem_pre_g', 16), ('mem_post_g', 16), ('ffn_pre_g', 16),
               ('ffn_post_g', 16), ('mu_r', 8), ('mu_k', 8), ('mu_v', 8), ('mu_wl', 1), ('mu_al', 1), ('mu_gl', 2),
               ('mu_vr', 1), ('w0', 8), ('a0', 8), ('v0', 8), ('k_k', 8), ('k_a', 8), ('r_k', 8), ('lnx_g', 8),
               ('lnx_b', 8), ('qn_g', 4), ('kvn_g', 2)]:
    PCOL[_n] = (_off, _w)
    _off += _w
NP_RAW = _off
MU_COLS = PCOL['mu_r'][0]
N_MU = 29
PCOL['om'] = (NP_RAW, N_MU)
PCOL['omka'] = (NP_RAW + N_MU, 8)
NPAR = NP_RAW + N_MU + 8


def _fm(v, ncol):
    buf = np.zeros(ncol * 128, np.float32)
    v = np.asarray(v, np.float32).reshape(-1)
    buf[:v.size] = v
    return buf.reshape(ncol, 128).T


def pack_params(inp, l):
    P = np.zeros((128, NPAR), np.float32)

    def put(name, arr):
        o, w = PCOL[name]
        P[:, o:o + w] = arr
    for n in ['mix_pre_g', 'mix_post_g', 'mem_pre_g', 'mem_post_g', 'ffn_pre_g', 'ffn_post_g']:
        put(n, _fm(inp[n][l], 16))
    mu = inp['mu_rwkv'][l]
    put('mu_r', _fm(mu[0:1024], 8))
    put('mu_k', _fm(mu[1024:2048], 8))
    put('mu_v', _fm(mu[2048:3072], 8))
    put('mu_wl', _fm(mu[3072:3168], 1))
    put('mu_al', _fm(mu[3168:3264], 1))
    put('mu_gl', _fm(mu[3264:3520], 2))
    if l > 0:
        put('mu_vr', _fm(inp['mu_vres'][l - 1], 1))
        put('v0', _fm(inp['v0'][l - 1], 8))
    for n in ['w0', 'a0', 'k_k', 'k_a', 'lnx_g', 'lnx_b']:
        put(n, _fm(inp[n][l], 8))
    put('r_k', _fm(inp['r_k'][l].reshape(-1), 8))
    put('qn_g', _fm(inp['q_norm_g'][l], 4))
    put('kvn_g', _fm(inp['kv_norm_g'][l], 2))
    return P


def make_consts(T):
    c = {}
    c['identF'] = np.eye(128, dtype=np.float32)
    bo = np.zeros((128, 128), np.float32)
    bo[:64, :64] = 1.0
    bo[64:, 64:] = 1.0
    c['blockones'] = bo
    r = np.arange(128)
    mk1 = np.zeros((128, 192), np.float32)
    mk1[:, :128] = (r[:, None] < r[None, :]).astype(np.float32)
    mk1[:, 128:] = ((r[:, None] % 64) <= np.arange(64)[None, :]).astype(np.float32)
    c['mk1'] = mk1
    c['mk2'] = (r[:, None] > r[None, :]).astype(np.float32)
    c['cmask'] = np.where(r[:, None] >= r[None, :], 0.0, -1e9).astype(np.float32)
    rs = np.ones((128, TT), np.float32)
    rs[:, ::64] = 0.0
    c['reset'] = rs
    pos = np.arange(T, dtype=np.float32)
    inv_freq = (10000.0 ** (-np.arange(0, 64, 2, dtype=np.float32) / 64)).astype(np.float32)
    ang = (pos[:, None] * inv_freq[None, :]).astype(np.float32)
    cs, sn = np.cos(ang).T.astype(np.float32), np.sin(ang).T.astype(np.float32)
    c['rope'] = np.stack([np.concatenate([cs, cs], 0), np.concatenate([-sn, sn], 0)], 0).astype(np.float32)
    return c


def unit_table():
    U = {}
    U['wk'] = [(0, 16, 512, 'wk_mem', 0, 0)]
    U['wv'] = [(0, 16, 512, 'wv_mem', 0, 0)]
    U['lora'] = [(0, 16, 448, 'w_in', 0, 3072), (7168, 16, 64, 'w_in_vres', 0, 0)]
    for pr in range(8):
        U[('rkv', pr)] = [(i * 2048, 16, 128, 'w_in', 0, i * 1024 + pr * 128) for i in range(3)]
    U['cq'] = [(0, 16, 512, 'w_in', 0, 3520)]
    U['ckvkr'] = [(0, 16, 320, 'w_in', 0, 4032)]
    U['uq'] = [(0, 4, 1536, 'w_uq', 0, 0)]
    U['ukv'] = [(0, 2, 2048, 'w_ukv', 0, 0)]
    for nb in range(4):
        U[('wout', nb)] = [(0, 16, 512, 'w_out', 0, nb * 512)]
    U['wq'] = [(0, 16, 512, 'wq_mem', 0, 0)]
    U['wo'] = [(0, 4, 2048, 'wo_mem', 0, 0)]
    for blk in range(16):
        U[('ff1', blk)] = [(0, 16, 512, 'w_ff1', 0, blk * 512)]
        U[('ff2', blk)] = [(0, 4, 2048, 'w_ff2', blk * 512, 0)]
    off = {}
    tot = 0
    for k_, parts in U.items():
        total = max(o + kc * n for (o, kc, n, _w, _r, _c) in parts)
        off[k_] = (tot, total)
        tot += total
    return U, off, tot


def build_wimg(inp, L):
    U, off, tot = unit_table()
    img = np.zeros((L, 128, tot), np.float32)
    for l in range(L):
        for k_, parts in U.items():
            d0 = off[k_][0]
            for (o, kc, n, wname, r0, c0) in parts:
                li = l - 1 if wname == 'w_in_vres' else l
                if li < 0:
                    continue
                blk = np.asarray(inp[wname][li], np.float32)[r0:r0 + kc * 128, c0:c0 + n]
                img[l, :, d0 + o:d0 + o + kc * n] = blk.reshape(kc, 128, n).transpose(1, 0, 2).reshape(128, kc * n)
    return img


def build(T, L, taps=()):
    NT = T // TT
    nc = bass.Bass("TRN2", target_bir_lowering=False)
    din = lambda name, shape, dt=F32: nc.dram_tensor(name, list(shape), dt, kind="ExternalInput").ap()
    x_d = din("x", [T, D])
    mem_d = din("mem", [NMEM, D])
    memg_d = din("memg", [128, 16])
    par_d = din("params", [L, 128, NPAR])
    U_TAB, U_OFF, U_TOT = unit_table()
    wimg_d = din("wimg", [L, 128, U_TOT])
    w_up_d = din("w_up", [L, 96, RW])
    a_up_d = din("a_up", [L, 96, RW])
    g_up_d = din("g_up", [L, 256, RW])
    v_up_d = din("v_up", [max(L - 1, 1), 64, RW])
    cn = make_consts(T)
    cd = {k: din("c_" + k, v.shape) for k, v in cn.items()}
    out_d = nc.dram_tensor("out", [T, D], F32, kind="ExternalOutput").ap()
    tap_d = {}
    for name, shape in taps:
        tap_d[name] = nc.dram_tensor("tap_" + name, list(shape), F32, kind="ExternalOutput").ap()
    hT_d = nc.dram_tensor("hT_s", [D, T], F32).ap()
    vf_d = nc.dram_tensor("vf_s", [RW, T], F32).ap()
    kn_d = nc.dram_tensor("kn_s", [8, 128, T], BF16).ap()
    kr_d = nc.dram_tensor("kr_s", [64, T], BF16).ap()
    vt_d = nc.dram_tensor("vt_s", [T, 1024], BF16).ap()
    memT_d = nc.dram_tensor("memT_s", [128, 16 * NMEM], BF16).ap()

    with contextlib.ExitStack() as st:
        S = Sched(nc, st)
        sb = lambda name, shape, dt=F32: st.enter_context(nc.sbuf_tensor(name, list(shape), dt))
        pst = lambda name, shape, dt=F32: st.enter_context(nc.psum_tensor(name, list(shape), dt))

        identF = sb("identF", [128, 128])
        identB = sb("identB", [128, 128], BF16)
        onesB = sb("onesB", [128, 128], BF16)
        blockones = sb("blockones", [128, 128])
        gnones = sb("gnones", [128, 128])
        mk1 = sb("mk1", [128, 192])
        mk2 = sb("mk2", [128, 128])
        cmask = sb("cmask", [128, 128])
        reset = sb("reset", [128, TT])
        hT = sb("hT", [128, 16, TT])
        uT = sb("uT", [128, 16, TT], BF16)
        yT = sb("yT", [128, 16, TT], BF16)
        wb = [sb("wb0", [128, 16 * 512], BF16), sb("wb1", [128, 16 * 512], BF16)]
        par = sb("par", [128, NPAR])
        lw = sb("lw", [128, 5, RW], BF16)
        kmT = sb("kmT", [128, 4, NMEM], BF16)
        vm = sb("vm", [128, 2, 512], BF16)
        Sbd = sb("Sbd", [128, 8, 2, 128])
        carry = sb("carry", [128, N_MU])
        rstd = sb("rstd", [128, TT])
        small = sb("small", [128, 64])
        ARENA_N = 20480
        arena = sb("arena", [128, ARENA_N])

        psF = [pst("psF%d" % i, [128, 512]) for i in range(6)]
        psB = [pst("psB%d" % i, [128, 1024], BF16) for i in range(2)]
        pctr = {'f': 0, 'b': 0}

        pmode = {'banks': list(range(6))}

        def pf():
            bl = pmode['banks']
            i = bl[pctr['f'] % len(bl)]
            pctr['f'] += 1
            return psF[i], ('psF', i)

        pctr['q'] = 0

        def pq():
            i = pctr['q'] % 16
            pctr['q'] += 1
            b_, q_ = i // 4, i % 4
            return psF[b_][:, q_ * 128:(q_ + 1) * 128], ('psq', b_, q_)

        def run_jobs(jobs):
            loaded = {0: wload(jobs[0][0])}
            for i, (parts, fn) in enumerate(jobs):
                if i + 1 < len(jobs):
                    loaded[i + 1] = wload(jobs[i + 1][0])
                fn(*loaded.pop(i))

        def pb():
            i = pctr['b'] % 2
            pctr['b'] += 1
            return psB[i], ('psB', i)

        def pc(name):
            o, w = PCOL[name]
            return o

        def mm(out, lhsT, rhs, r, w, start=True, stop=True, inc=None):
            S.op('pe', lambda e: e.matmul(out, lhsT, rhs, start=start, stop=stop), r, w, inc=(stop if inc is None else inc))

        def tr(out, in_, ident, r, w, inc=True):
            S.op('pe', lambda e: e.transpose(out, in_, ident), r, w, inc=inc)

        def act(out, in_, func, r, w, bias=0.0, scale=1.0, accum=None):
            if accum is None:
                S.op('act', lambda e: e.activation(out, in_, func, bias=bias, scale=scale), r, w)
            else:
                S.op('act', lambda e: e.activation(out, in_, func, bias=bias, scale=scale, accum_out=accum), r, w)

        def cp(eng, out, in_, r, w):
            if eng == 'act':
                S.op('act', lambda e: e.copy(out, in_), r, w)
            else:
                S.op(eng, lambda e: e.tensor_copy(out, in_), r, w)

        def tt(eng, out, a, b, op, r, w):
            S.op(eng, lambda e: e.tensor_tensor(out, a, b, op), r, w)

        def ts(eng, out, a, s1, op0, r, w, s2=None, op1=None):
            if op1 is None:
                S.op(eng, lambda e: e.tensor_scalar(out, a, s1, None, op0), r, w)
            else:
                S.op(eng, lambda e: e.tensor_scalar(out, a, s1, s2, op0, op1), r, w)

        def stt(out, in0, scalar, in1, op0, op1, r, w):
            S.op('dve', lambda e: e.scalar_tensor_tensor(out, in0, scalar, in1, op0, op1), r, w)

        def memset(eng, ap, val, w):
            S.op(eng, lambda e: e.memset(ap, val), (), w)

        def tap(name, src_ap, r, dst=None):
            if name in tap_d:
                S.dma('sp', tap_d[name] if dst is None else dst, src_ap, reads=r, semkey='tap_' + name)

        def AF32(off, n):
            return arena[:, off:off + n]

        def AB16(off, n):
            return arena[:, off:off + n].bitcast(BF16)

        for nm, t in [('identF', identF), ('blockones', blockones), ('mk1', mk1), ('mk2', mk2), ('cmask', cmask),
                      ('reset', reset)]:
            S.dma('sp', t[:], cd[nm], writes=[nm], semkey='c_' + nm)
        cp('dve', identB[:], identF[:], ['identF'], ['identB'])
        memset('dve', onesB[:], 1.0, ['onesB'])
        ts('dve', gnones[:], blockones[:], 1.0 / 64.0, ALU.mult, ['blockones'], ['gnones'])

        wctr = {'i': 0}

        wimg_b = [nc.dram_tensor("wimg_b%d" % l_, [128, U_TOT], BF16).ap() for l_ in range(L)]
        cur = {'l': 0, 'cc': 0}

        def wload(uname):
            i = wctr['i'] % 2
            wctr['i'] += 1
            key = ('wb', i)
            doff, total = U_OFF[uname]
            S.dma('sp', wb[i][:, 0:total], wimg_b[cur['l']][:, doff:doff + total], reads=[('wq', cur['l'])], writes=[key], semkey='wb%d' % i)
            return wb[i], key

        def convert_list(lyr):
            NCH = 16
            step = (U_TOT + NCH - 1) // NCH
            out_ = []
            for q_ in range(NCH):
                lo, hi = q_ * step, min(U_TOT, (q_ + 1) * step)
                if lo < hi:
                    out_.append((wimg_b[lyr][:, lo:hi], wimg_d[lyr][:, lo:hi]))
            return out_

        def emit_convert(lyr, items):
            for dst, src in items:
                i = cur['cc']
                cur['cc'] += 1
                S.dma('pool', dst, src, reads=[('cc', i - 2)], writes=[('wq', lyr), ('cc', i)], semkey='cvt%d' % lyr)

        def wview(buf, off, kc, ncols):
            return buf[:, off:off + kc * ncols].rearrange("p (k n) -> p k n", k=kc)

        def rms_stats(src_fn, KC, dn, eps, rkeys, sq_buf=None, sqkey=None):
            sq = uT if sq_buf is None else sq_buf
            sqk = 'uT' if sqkey is None else sqkey
            for c in range(KC):
                act(sq[:, c, :], src_fn(c), AF.Square, rkeys, [(sqk, c)])
            p, pk = pf()
            for c in range(KC):
                mm(p[:, :], onesB[:], sq[:, c, :], ['onesB', (sqk, c)], [pk], start=(c == 0), stop=(c == KC - 1))
            act(rstd[:], p[:, :], AF.Sqrt, [pk], ['rstd'], bias=eps, scale=1.0 / dn)
            S.op('dve', lambda e: e.reciprocal(rstd[:], rstd[:]), ['rstd'], ['rstd'])

        def pre_norm(gname):
            rms_stats(lambda c: hT[:, c, :], 16, float(D), EPS, ['hT'])
            g0 = pc(gname)
            for c in range(16):
                stt(uT[:, c, :], hT[:, c, :], par[:, g0 + c:g0 + c + 1], rstd[:], ALU.mult, ALU.mult,
                    ['hT', 'par', 'rstd', ('uT', c)], [('uT', c)])

        def post_norm_res(z, zkey, gname):
            rms_stats(lambda c: z[:, c, :], 16, float(D), EPS, [zkey])
            g0 = pc(gname)
            for c in range(16):
                stt(z[:, c, :], z[:, c, :], par[:, g0 + c:g0 + c + 1], rstd[:], ALU.mult, ALU.mult,
                    [zkey, 'par', 'rstd'], [(zkey, 'n', c)])
                tt('pool', hT[:, c, :], hT[:, c, :], z[:, c, :], ALU.add, [(zkey, 'n', c), 'hT'], ['hT'])
            S.op('pool', lambda e: e.memset(small[:, 60:61], 0.0), [(zkey, 'n', c) for c in range(16)], [zkey])

        def prep_mem():
            mt = AF32(0, 2 * D).rearrange("p (b d) -> p b d", b=2)
            S.dma('sp', mt, mem_d.rearrange("(b p) d -> p b d", p=128), writes=['mt'], semkey='mt')
            mg = small[:, 0:16]
            S.dma('sp', mg, memg_d, writes=['mg'], semkey='mg')
            mf = AF32(2 * D, 16 * NMEM).rearrange("p (c t) -> p c t", c=16)
            for b in range(2):
                for c4 in range(4):
                    p, pk = pf()
                    for i in range(4):
                        c = c4 * 4 + i
                        tr(p[:, i * 128:(i + 1) * 128], mt[:, b, c * 128:(c + 1) * 128], identF[:], ['mt', 'identF'], [pk], inc=(i == 3))
                    cp('act', mf[:, c4 * 4:(c4 + 1) * 4, b * 128:(b + 1) * 128],
                       p[:, :].rearrange("p (i t) -> p i t", i=4), [pk], [('mf', b, c4)])
            mfk = [('mf', b, c4) for b in range(2) for c4 in range(4)]
            sq = AB16(2 * D + 16 * NMEM, 16 * NMEM // 2).rearrange("p (c t) -> p c t", c=16)
            for c in range(16):
                act(sq[:, c, :], mf[:, c, :], AF.Square, mfk, [('msq', c)])
            p, pk = pf()
            for c in range(16):
                mm(p[:, 0:NMEM], onesB[:], sq[:, c, :], ['onesB', ('msq', c)], [pk], start=(c == 0), stop=(c == 15))
            act(rstd[:, 0:NMEM], p[:, 0:NMEM], AF.Sqrt, [pk], ['rstd'], bias=EPS, scale=1.0 / D)
            S.op('dve', lambda e: e.reciprocal(rstd[:, 0:NMEM], rstd[:, 0:NMEM]), ['rstd'], ['rstd'])
            memT = AB16(2 * D + 16 * NMEM + 16 * NMEM // 2, 16 * NMEM // 2).rearrange("p (c t) -> p c t", c=16)
            for c in range(16):
                stt(memT[:, c, :], mf[:, c, :], mg[:, c:c + 1], rstd[:, 0:NMEM], ALU.mult, ALU.mult,
                    mfk + ['mg', 'rstd'], ['memT'])
            S.dma('sp', memT_d, AB16(2 * D + 16 * NMEM + 16 * NMEM // 2, 16 * NMEM // 2), reads=['memT'], writes=['memT_d'], semkey='memTs')

        def layer_setup(l):
            S.barrier()
            S.dma('sp', par[:], par_d[l], writes=['par'], semkey='par')
            o, w = PCOL['om']
            ts('dve', par[:, o:o + N_MU], par[:, MU_COLS:MU_COLS + N_MU], -1.0, ALU.mult, ['par'], ['par'], s2=1.0, op1=ALU.add)
            o2, _ = PCOL['omka']
            ka = pc('k_a')
            ts('dve', par[:, o2:o2 + 8], par[:, ka:ka + 8], -1.0, ALU.mult, ['par'], ['par'], s2=1.0, op1=ALU.add)
            S.dma('pool', lw[0:96, 0, :], w_up_d[l], writes=['lwts'], semkey='lw')
            S.dma('pool', lw[0:96, 1, :], a_up_d[l], writes=['lwts'], semkey='lw')
            S.dma('pool', lw[:, 2:4, :], g_up_d[l].rearrange("(k p) n -> p k n", p=128), writes=['lwts'], semkey='lw')
            if l > 0:
                S.dma('pool', lw[0:64, 4, :], v_up_d[l - 1], writes=['lwts'], semkey='lw')
            memset('dve', carry[:], 0.0, ['carry'])
            memset('dve', Sbd[:], 0.0, ['Sbd'])
            memT = AB16(0, 16 * NMEM // 2).rearrange("p (c t) -> p c t", c=16)
            S.dma('sp', AB16(0, 16 * NMEM // 2), memT_d, reads=['memT_d'], writes=['memT'], semkey='memTl')
            buf, key = wload('wk')
            wv = wview(buf, 0, 16, 512)
            for h in range(4):
                p, pk = pf()
                for c in range(16):
                    mm(p[:, 0:NMEM], wv[:, c, h * 128:(h + 1) * 128], memT[:, c, :], [key, 'memT'], [pk], start=(c == 0), stop=(c == 15))
                cp('act', kmT[:, h, :], p[:, 0:NMEM], [pk], ['kmT'])
            buf, key = wload('wv')
            wv = wview(buf, 0, 16, 512)
            for b in range(2):
                p, pk = pf()
                for c in range(16):
                    mm(p[:, :], memT[:, c, b * 128:(b + 1) * 128], wv[:, c, :], [key, 'memT'], [pk], start=(c == 0), stop=(c == 15))
                cp('act', vm[:, b, :], p[:, :], [pk], ['vm'])

        def load_tile(l, j):
            if l == 0:
                xs = AF32(0, D)
                for blk in range(4):
                    S.dma('sp', xs, x_d[j * TT + blk * 128: j * TT + (blk + 1) * 128, :], writes=['xs'], semkey='xs')
                    for c4 in range(4):
                        p, pk = pf()
                        for i in range(4):
                            c = c4 * 4 + i
                            tr(p[:, i * 128:(i + 1) * 128], xs[:, c * 128:(c + 1) * 128], identF[:], ['xs', 'identF'], [pk], inc=(i == 3))
                        cp('act' if c4 % 2 else 'dve', hT[:, c4 * 4:(c4 + 1) * 4, blk * 128:(blk + 1) * 128],
                           p[:, :].rearrange("p (i t) -> p i t", i=4), [pk], [('hT', 'ld', blk, c4)])
                S.op('pool', lambda e: e.memset(small[:, 61:62], 0.0), [('hT', 'ld', blk, c4) for blk in range(4) for c4 in range(4)], ['hT'])
            else:
                S.dma('sp', hT[:], hT_d[:, j * TT:(j + 1) * TT].rearrange("(c p) t -> p c t", p=128),
                      reads=[('hT_d', j)], writes=['hT'], semkey='hT')

        def store_tile(l, j):
            if l == L - 1:
                S.barrier()
                os_ = AF32(0, D)
                for blk in range(4):
                    for c4 in range(4):
                        p, pk = pf()
                        for i in range(4):
                            c = c4 * 4 + i
                            tr(p[:, i * 128:(i + 1) * 128], hT[:, c, blk * 128:(blk + 1) * 128], identF[:], ['hT', 'identF'], [pk], inc=(i == 3))
                        cp('act' if c4 % 2 else 'dve', os_[:, c4 * 512:(c4 + 1) * 512], p[:, :], [pk], [('os', c4)])
                    S.dma('sp', out_d[j * TT + blk * 128: j * TT + (blk + 1) * 128, :], os_,
                          reads=[('os', c4) for c4 in range(4)], writes=[('out', j, blk)], semkey='os')
            else:
                S.dma('sp', hT_d[:, j * TT:(j + 1) * TT].rearrange("(c p) t -> p c t", p=128), hT[:],
                      reads=['hT'], writes=[('hT_d', j)], semkey='hT')

        def phase_rwkv(l, j):
            S.barrier()
            o = 0

            def slot(n):
                nonlocal o
                a_ = (o, n)
                o += n
                return a_
            s_ar = slot(8 * 192)
            s_bt = slot(8 * 128)
            s_kt = slot(8 * 128)
            s_bh = slot(8 * 128)
            s_kh = slot(8 * 128)
            s_vh = slot(8 * 128)
            DEAD = ['r', 'v', 'kk', 'a', 'lw', 'G', 'e1', 'e2', 'k2', 'b']
            names = DEAD + ['k', 'g', 'bon', 't1', 't2']
            dead0 = o
            sl = {n: slot(TT) for n in names}
            s_ymu = [slot(TT + 1), slot(TT + 1)]
            s_x = [slot(128) for _ in range(2)]
            s_u = [slot(128) for _ in range(2)]
            tw = AB16(o, TT // 2); o += TT // 2
            alb = AB16(o, TT // 2); o += TT // 2
            sg = AB16(o, TT).rearrange("p (k t) -> p k t", k=2); o += TT
            vrb = AB16(o, TT // 2); o += TT // 2
            assert o <= ARENA_N, o
            AR = AF32(*s_ar).rearrange("p (c n) -> p c n", c=8)
            BT = AF32(*s_bt).rearrange("p (c n) -> p c n", c=8)
            KT = AF32(*s_kt).rearrange("p (c n) -> p c n", c=8)
            BH = AF32(*s_bh).rearrange("p (c n) -> p c n", c=8)
            KH = AF32(*s_kh).rearrange("p (c n) -> p c n", c=8)
            VH = AF32(*s_vh).rearrange("p (c n) -> p c n", c=8)
            V = {n: AF32(*sl[n]) for n in names}
            CB = {'dead0': dead0}
            for c in range(8):
                base = dead0 + c * 640
                CB[c] = dict(AB=AF32(base, 192), AK=AF32(base + 192, 192), Pt=AF32(base + 384, 128), Mm=AF32(base + 512, 128))
            ck8 = lambda nm: [(nm, c) for c in range(8)]
            for nm, t in [('AR', AR), ('BT', BT), ('KT', KT), ('BH', BH), ('KH', KH), ('VH', VH)]:
                memset('pool', t, 0.0, ck8(nm))

            pre_norm('mix_pre_g')
            ymu_i = [0]

            def shift_evac(p, pk, M, cid, out_ap, wkeys):
                yb = AF32(*s_ymu[ymu_i[0] % 2])
                yk = ('ymu', ymu_i[0] % 2)
                ymu_i[0] += 1
                cp('pool', yb[0:M, 0:1], carry[0:M, cid:cid + 1], ['carry'], [yk])
                S.op('act', lambda e: e.mul(yb[0:M, 1:TT + 1], p, par[0:M, MU_COLS + cid:MU_COLS + cid + 1]), [pk, 'par', yk], [yk])
                oo = PCOL['om'][0]
                stt(out_ap, p, par[0:M, oo + cid:oo + cid + 1], yb[0:M, 0:TT], ALU.mult, ALU.add, [pk, 'par', yk], wkeys)
                cp('pool', carry[0:M, cid:cid + 1], yb[0:M, TT:TT + 1], [yk], ['carry'])

            def proj(lhs_fn, M, rkeys):
                p, pk = pf()
                for c in range(16):
                    mm(p[0:M, :], lhs_fn(c), uT[:, c, :], rkeys + [('uT', c)], [pk], start=(c == 0), stop=(c == 15))
                return p[0:M, :], pk
            t1 = V['t1']

            def lora_job(buf, key):
                wl_ = wview(buf, 0, 16, 448)
                wvr = wview(buf, 16 * 448, 16, 64)
                p, pk = proj(lambda c: wl_[:, c, 0:96], 96, [key])
                shift_evac(p, pk, 96, 24, t1[0:96, :], ['t1'])
                act(tw[0:96, :], t1[0:96, :], AF.Tanh, ['t1'], ['tw'])
                p, pk = proj(lambda c: wl_[:, c, 96:192], 96, [key])
                shift_evac(p, pk, 96, 25, alb[0:96, :], ['alb'])
                for i in range(2):
                    p, pk = proj(lambda c, i=i: wl_[:, c, 192 + 128 * i:320 + 128 * i], 128, [key])
                    shift_evac(p, pk, 128, 26 + i, t1[:, :], ['t1'])
                    act(sg[:, i, :], t1[:, :], AF.Sigmoid, ['t1'], ['sg'])
                if l > 0:
                    p, pk = proj(lambda c: wvr[:, c, :], 64, [key])
                    shift_evac(p, pk, 64, 28, vrb[0:64, :], ['vrb'])

            def pair_job(pr):
                def fn(buf, key):
                    for i, nm in enumerate(['r', 'k', 'v']):
                        wv_ = wview(buf, i * 16 * 128, 16, 128)
                        p, pk = proj(lambda c, wv_=wv_: wv_[:, c, :], 128, [key])
                        shift_evac(p, pk, 128, 8 * i + pr, V[nm], [nm])
                    rwkv_pair(l, j, pr, V, AR, BT, KT, BH, KH, VH, tw, alb, sg, vrb, CB, DEAD, s_x, s_u)
                return fn
            jobs = [('lora', lora_job)]
            for pr in range(8):
                jobs.append((('rkv', pr), pair_job(pr)))
            run_jobs(jobs)
            pmode['banks'] = list(range(6))

        def rwkv_pair(l, j, pr, V, AR, BT, KT, BH, KH, VH, tw, alb, sg, vrb, CB, DEAD, s_x, s_u):
            cs = slice(pr * 128, (pr + 1) * 128)
            col = lambda n: par[:, pc(n) + pr:pc(n) + pr + 1]
            r, k, v, kk, a, lwv, G, e1, e2, g, bon, k2, b, t1, t2 = [V[n] for n in
                ['r', 'k', 'v', 'kk', 'a', 'lw', 'G', 'e1', 'e2', 'g', 'bon', 'k2', 'b', 't1', 't2']]
            O = k
            ck8 = lambda nm: [(nm, c) for c in range(8)]
            p, pk = pf()
            mm(p[:, :], lw[0:96, 0, cs], tw[0:96, :], ['lwts', 'tw'], [pk])
            act(lwv, p[:, :], AF.Sigmoid, [pk, 'par'], ['lw'], bias=col('w0'))
            ts('pool', lwv, lwv, DECAY_C, ALU.mult, ['lw'], ['lw'])
            p, pk = pf()
            mm(p[:, :], lw[0:96, 1, cs], alb[0:96, :], ['lwts', 'alb'], [pk])
            act(a, p[:, :], AF.Sigmoid, [pk, 'par'], ['a'], bias=col('a0'))
            p, pk = pf()
            for i in range(2):
                mm(p[:, :], lw[:, 2 + i, cs], sg[:, i, :], ['lwts', 'sg'], [pk], start=(i == 0), stop=(i == 1))
            cp('act', g, p[:, :], [pk], ['g'])
            if l > 0:
                p, pk = pf()
                mm(p[:, :], lw[0:64, 4, cs], vrb[0:64, :], ['lwts', 'vrb'], [pk])
                act(t1, p[:, :], AF.Sigmoid, [pk, 'par'], ['t1'], bias=col('v0'))
                S.dma('sp', t2, vf_d[pr * 128:(pr + 1) * 128, j * TT:(j + 1) * TT], reads=[('vf_d', pr, j)], writes=['t2'], semkey='vfl')
                tt('dve', t2, t2, v, ALU.subtract, ['t2', 'v'], ['t2'])
                tt('dve', t2, t2, t1, ALU.mult, ['t2', 't1'], ['t2'])
                tt('dve', v, v, t2, ALU.add, ['v', 't2'], ['v'])
            else:
                S.dma('sp', vf_d[pr * 128:(pr + 1) * 128, j * TT:(j + 1) * TT], v, reads=['v'], writes=[('vf_d', pr, j)], semkey='vfs')
            ts('pool', kk, k, col('k_k'), ALU.mult, ['k', 'par'], ['kk'])
            tt('pool', t1, kk, kk, ALU.mult, ['kk'], ['t1'])
            p, pk = pf()
            mm(p[:, :], blockones[:], t1, ['blockones', 't1'], [pk])
            act(t2, p[:, :], AF.Sqrt, [pk], ['t2'])
            ts('dve', t2, t2, 1e-12, ALU.max, ['t2'], ['t2'])
            S.op('dve', lambda e: e.reciprocal(t2, t2), ['t2'], ['t2'])
            tt('dve', kk, kk, t2, ALU.mult, ['kk', 't2'], ['kk'])
            oka = PCOL['omka'][0]
            ts('dve', t1, a, col('k_a'), ALU.mult, ['a', 'par'], ['t1'], s2=par[:, oka + pr:oka + pr + 1], op1=ALU.add)
            tt('dve', k2, k, t1, ALU.mult, ['k', 't1'], ['k2'])
            tt('pool', b, kk, a, ALU.mult, ['kk', 'a'], ['b'])
            stt(t1, r, col('r_k'), k2, ALU.mult, ALU.mult, ['r', 'par', 'k2'], ['t1'])
            p, pk = pf()
            mm(p[:, :], blockones[:], t1, ['blockones', 't1'], [pk])
            tt('dve', bon, p[:, :], v, ALU.mult, [pk, 'v'], ['bon'])
            S.op('dve', lambda e: e.tensor_tensor_scan(G, reset[:], lwv, 0.0, ALU.mult, ALU.add), ['reset', 'lw'], ['G'])
            c3 = lambda ap: ap.rearrange("p (c t) -> p c t", c=8)
            hs = [(slice(0, 64), slice(0, 64)), (slice(64, 128), slice(64, 128))]
            act(e1, G, AF.Exp, ['G'], ['e1'])
            tt('dve', AR[:, :, 128:192], c3(r), c3(e1), ALU.mult, ['r', 'e1'], ck8('AR'))
            tt('pool', e2, G, lwv, ALU.subtract, ['G', 'lw'], ['e2'])
            act(e2, e2, AF.Exp, ['e2'], ['e2'])
            for ps_, fs_ in hs:
                stt(AR[ps_, :, fs_], c3(kk)[ps_], -1.0, c3(e2)[ps_], ALU.mult, ALU.mult, ['kk', 'e2'], ck8('AR'))
            act(e1, G, AF.Exp, ['G'] + ck8('AR'), ['e1'], scale=-1.0)
            for ps_, fs_ in hs:
                tt('dve', BT[ps_, :, fs_], c3(b)[ps_], c3(e1)[ps_], ALU.mult, ['b', 'e1'], ck8('BT'))
                tt('pool', KT[ps_, :, fs_], c3(k2)[ps_], c3(e1)[ps_], ALU.mult, ['k2', 'e1'], ck8('KT'))
            for c in range(8):
                act(e2[:, c * 64:(c + 1) * 64], G[:, c * 64:(c + 1) * 64], AF.Exp, ['G', 'e2'], ['e2'],
                    bias=G[:, c * 64 + 63:c * 64 + 64], scale=-1.0)
            for ps_, fs_ in hs:
                tt('dve', BH[ps_, :, fs_], c3(b)[ps_], c3(e2)[ps_], ALU.mult, ['b', 'e2'], ck8('BH'))
                tt('pool', KH[ps_, :, fs_], c3(k2)[ps_], c3(e2)[ps_], ALU.mult, ['k2', 'e2'], ck8('KH'))
                cp('pool', VH[ps_, :, fs_], c3(v)[ps_], ['v'], ck8('VH'))
            WC = small[:, 16:24]
            act(WC, G.rearrange("p (c t) -> p c t", c=8)[:, :, 63], AF.Exp, ['G'], ['WC'])
            if l == 0 and j == 0 and pr == 0:
                tap('v', v, ['v'])
                tap('r', r, ['r'])
                tap('kk', kk, ['kk'])
                tap('G', G, ['G'])

            cbk = [(nm, g_) for nm in ('AB', 'AK', 'Pt', 'Mm') for g_ in range(2)]
            S.op('pool', lambda e: e.memset(small[:, 58:59], 0.0), [], list(DEAD) + cbk)

            def gview(g_, field, lo, hi):
                off = {'AB': 0, 'AK': 192, 'Pt': 384, 'Mm': 512}[field]
                base = CB['dead0'] + g_ * 4 * 640
                return arena[:, base:base + 4 * 640].rearrange("p (c n) -> p c n", n=640)[:, :, off + lo:off + hi]
            bc4 = lambda t_: t_[:].rearrange("p (o n) -> p o n", o=1).to_broadcast([128, 4, 128])

            for c in range(8):
                p, pk = pf()
                tr(p[:, 0:128], BH[:, c, :], identF[:], [('BH', c), 'identF'], [pk], inc=False)
                tr(p[:, 128:256], KH[:, c, :], identF[:], [('KH', c), 'identF'], [pk], inc=False)
                tr(p[:, 256:384], VH[:, c, :], identF[:], [('VH', c), 'identF'], [pk], inc=True)
                cp('act', BH[:, c, :], p[:, 0:128], [pk], [('BH', c)])
                cp('act', KH[:, c, :], p[:, 128:256], [pk], [('KH', c)])
                cp('act', VH[:, c, :], p[:, 256:384], [pk], [('VH', c)])
            for c in range(8):
                g_ = c // 4
                p, pk = pf()
                mm(p[:, 0:192], BT[:, c, :], AR[:, c, :], [('BT', c), ('AR', c)], [pk], inc=False)
                mm(p[:, 256:448], KT[:, c, :], AR[:, c, :], [('KT', c), ('AR', c)], [pk], start=True, stop=True)
                tt('dve', CB[c]['AB'], p[:, 0:192], mk1[:], ALU.mult, [pk, 'mk1'], [('AB', g_)])
                tt('dve', CB[c]['AK'], p[:, 256:448], mk1[:], ALU.mult, [pk, 'mk1'], [('AK', g_)])
            for g_ in range(2):
                p, pk = pf()
                for i in range(4):
                    c = g_ * 4 + i
                    mm(p[:, i * 128:(i + 1) * 128], AR[:, c, 0:128], BT[:, c, :], [('BT', c), ('AR', c)], [pk], inc=(i == 3))
                for i in range(4):
                    c = g_ * 4 + i
                    tt('dve', CB[c]['Pt'], p[:, i * 128:(i + 1) * 128], mk2[:], ALU.mult, [pk, 'mk2'], [('Pt', g_)])
                    tt('pool', CB[c]['Mm'], CB[c]['AB'][:, 0:128], identF[:], ALU.add, [('AB', g_), 'identF'], [('Mm', g_)])
            for kq in range(1, 6):
                for g_ in range(2):
                    if kq < 5:
                        pA, pAk = pf()
                        for i in range(4):
                            c = g_ * 4 + i
                            mm(pA[:, i * 128:(i + 1) * 128], CB[c]['Pt'], CB[c]['AB'][:, 0:128], [('Pt', g_), ('AB', g_)], [pAk], inc=(i == 3))
                    pB, pBk = pf()
                    for i in range(4):
                        c = g_ * 4 + i
                        mm(pB[:, i * 128:(i + 1) * 128], CB[c]['AB'][:, 0:128], CB[c]['Pt'], [('Pt', g_), ('AB', g_)], [pBk], inc=(i == 3))
                    if False:
                        for i in range(4):
                            c = g_ * 4 + i
                            if kq < 5:
                                cp('act', CB[c]['AB'][:, 0:128], pA[:, i * 128:(i + 1) * 128], [pAk], [('AB', g_)])
                            cp('dve' if g_ else 'act', CB[c]['Pt'], pB[:, i * 128:(i + 1) * 128], [pBk], [('Pt', g_)])
                    else:
                        if kq < 5:
                            cp('act', gview(g_, 'AB', 0, 128), pA[:, :].rearrange("p (c n) -> p c n", c=4), [pAk], [('AB', g_)])
                        cp('dve' if g_ else 'act', gview(g_, 'Pt', 0, 128), pB[:, :].rearrange("p (c n) -> p c n", c=4), [pBk], [('Pt', g_)])
                for g_ in range(2):
                    pC, pCk = pf()
                    for i in range(4):
                        c = g_ * 4 + i
                        mm(pC[:, i * 128:(i + 1) * 128], CB[c]['Pt'], CB[c]['Mm'], [('Pt', g_), ('Mm', g_)], [pCk], inc=(i == 3))
                    if False:
                        for i in range(4):
                            c = g_ * 4 + i
                            tt('dve', CB[c]['Mm'], pC[:, i * 128:(i + 1) * 128], CB[c]['Mm'], ALU.add, [pCk, ('Mm', g_)], [('Mm', g_)])
                    else:
                        tt('dve', gview(g_, 'Mm', 0, 128), pC[:, :].rearrange("p (c n) -> p c n", c=4), gview(g_, 'Mm', 0, 128), ALU.add,
                           [pCk, ('Mm', g_)], [('Mm', g_)])

            for c in range(8):
                ci = c % 2
                g_ = c // 4
                AB, AK, Tt = CB[c]['AB'], CB[c]['AK'], CB[c]['Mm']
                abk, akk, Ttk = ('AB', g_), ('AK', g_), ('Mm', g_)
                Scur = Sbd[:, pr, c % 2, :]
                Snxt = Sbd[:, pr, (c + 1) % 2, :]
                sck, snk = ('Sbd', pr, c % 2), ('Sbd', pr, (c + 1) % 2)
                X = AF32(*s_x[ci]); xk = ('X', ci)
                U = AF32(*s_u[ci]); uk_ = ('U', ci)
                p, pk = pf()
                mm(p[:, 0:128], AR[:, c, 0:128], Scur, [('AR', c), sck, 'Sbd'], [pk], start=True, stop=False)
                mm(p[:, 0:128], AK[:, 0:128], VH[:, c, :], [akk, ('VH', c)], [pk], start=False, stop=True)
                cp('act', X, p[:, 0:128], [pk], [xk])
                p, pk = pf()
                mm(p[:, 0:128], Tt, X, [Ttk, xk], [pk])
                cp('act', U, p[:, 0:128], [pk], [uk_])
                p, pk = pf()
                mm(p[:, 0:128], BH[:, c, :], U, [('BH', c), uk_], [pk], start=True, stop=False)
                mm(p[:, 0:128], KH[:, c, :], VH[:, c, :], [('KH', c), ('VH', c)], [pk], start=False, stop=True)
                stt(Snxt, Scur, WC[:, c:c + 1], p[:, 0:128], ALU.mult, ALU.add, [sck, 'WC', pk, 'Sbd'], [snk])
                p, pk = pf()
                mm(p[:, 0:64], Scur, AR[:, c, 128:192], [sck, ('AR', c), 'Sbd'], [pk], start=True, stop=False)
                mm(p[:, 0:64], U, AB[:, 128:192], [uk_, abk], [pk], start=False, stop=False)
                mm(p[:, 0:64], VH[:, c, :], AK[:, 128:192], [('VH', c), akk], [pk], start=False, stop=True)
                cp('dve', O[:, c * 64:(c + 1) * 64], p[:, 0:64], [pk, 'k'], ['k'])
            S.op('pool', lambda e: e.memset(small[:, 57:58], 0.0), [], list(DEAD) + cbk)
            p, pk = pf()
            mm(p[:, :], gnones[:], O, ['gnones', 'k'], [pk])
            tt('dve', t1, O, p[:, :], ALU.subtract, ['k', pk], ['t1'])
            tt('pool', t2, t1, t1, ALU.mult, ['t1'], ['t2'])
            p, pk = pf()
            mm(p[:, :], gnones[:], t2, ['gnones', 't2'], [pk])
            act(t2, p[:, :], AF.Sqrt, [pk], ['t2'], bias=GN_EPS)
            S.op('dve', lambda e: e.reciprocal(t2, t2), ['t2'], ['t2'])
            tt('dve', t1, t1, t2, ALU.mult, ['t1', 't2'], ['t1'])
            ts('dve', t1, t1, col('lnx_g'), ALU.mult, ['t1', 'par'], ['t1'], s2=col('lnx_b'), op1=ALU.add)
            tt('pool', t1, t1, bon, ALU.add, ['t1', 'bon'], ['t1'])
            tt('dve', yT[:, pr, :], t1, g, ALU.mult, ['t1', 'g'], [('yT', pr)])
            if l == 0 and j == 0 and pr == 0:
                tap('O', O, ['k'])
                tap('yr', t1, ['t1'])

        def phase_mla(l, j):
            S.barrier()
            o = 0

            def slot(n):
                nonlocal o
                a_ = (o, n)
                o += n
                return a_
            NK = (j + 1) * TT
            s_S = slot(max(T, 8 * TT))
            s_cq = (s_S[0], 4 * TT)
            s_ckv = (s_S[0] + 4 * TT, 2 * TT)
            s_t1 = (s_S[0] + 6 * TT, TT)
            s_t2 = (s_S[0] + 7 * TT, TT)

            def b16(n_units):
                nonlocal o
                v_ = AB16(o, n_units)
                o += n_units
                return v_
            s_rope = (o, 2 * TT)
            P_ = b16(max(T // 2, 2 * TT))
            Kh = b16(T // 2)
            Vh = b16(T // 2).rearrange("p (b d) -> p b d", d=128)
            krl = b16(T // 2)
            qT = b16(8 * TT // 2).rearrange("p (h t) -> p h t", h=8)
            qrT = b16(8 * TT // 2).rearrange("p (h t) -> p h t", h=8)
            cqn = b16(4 * TT // 2).rearrange("p (c t) -> p c t", c=4)
            ckvn = b16(2 * TT // 2).rearrange("p (c t) -> p c t", c=2)
            knst = b16(TT // 2)
            krst = b16(TT // 2)
            vst = [b16(512), b16(512)]
            PT = [b16(256), b16(256)]
            osb = b16(64)
            rsum = small[:, 24:26]
            mx = small[:, 26:27]
            assert o <= ARENA_N, o
            Ssb = AF32(*s_S)
            cq = AF32(*s_cq).rearrange("p (c t) -> p c t", c=4)
            ckv = AF32(*s_ckv).rearrange("p (c t) -> p c t", c=2)
            tmp1 = AF32(*s_t1)
            tmp2 = AF32(*s_t2)
            rope = AF32(*s_rope).rearrange("p (a t) -> p a t", a=2)
            S.dma('sp', rope[0:64], cd['rope'][:, :, j * TT:(j + 1) * TT].rearrange("a p t -> p a t"), writes=['rope'], semkey='rope')

            def proj(lhs_fn, M, rkeys):
                p, pk = pf()
                for c in range(16):
                    mm(p[0:M, :], lhs_fn(c), uT[:, c, :], rkeys + [('uT', c)], [pk], start=(c == 0), stop=(c == 15))
                return p, pk
            buf, key = wload('cq')
            wv_ = wview(buf, 0, 16, 512)
            for c in range(4):
                p, pk = proj(lambda cc, c=c: wv_[:, cc, c * 128:(c + 1) * 128], 128, [key])
                cp('act', cq[:, c, :], p[:, :], [pk], ['cq'])
            buf, key = wload('ckvkr')
            wv_ = wview(buf, 0, 16, 320)
            for c in range(2):
                p, pk = proj(lambda cc, c=c: wv_[:, cc, c * 128:(c + 1) * 128], 128, [key])
                cp('act', ckv[:, c, :], p[:, :], [pk], ['ckv'])

            def rope_mm(lhs_fn, nkc, rhs_fn, rkeys):
                p1, pk1 = pf()
                for c in range(nkc):
                    mm(p1[0:64, :], lhs_fn(c, 0, 64), rhs_fn(c), rkeys(c), [pk1], start=(c == 0), stop=(c == nkc - 1))
                p2, pk2 = pf()
                for c in range(nkc):
                    mm(p2[0:32, :], lhs_fn(c, 32, 64), rhs_fn(c), rkeys(c), [pk2], start=(c == 0), stop=(c == nkc - 1))
                for c in range(nkc):
                    mm(p2[32:64, :], lhs_fn(c, 0, 32), rhs_fn(c), rkeys(c), [pk2], start=(c == 0), stop=(c == nkc - 1))
                tt('dve', tmp1[0:64, :], p1[0:64, :], rope[0:64, 0, :], ALU.mult, [pk1, 'rope'], ['tmp1'])
                tt('dve', tmp2[0:64, :], p2[0:64, :], rope[0:64, 1, :], ALU.mult, [pk2, 'rope'], ['tmp2'])
                tt('dve', tmp1[0:64, :], tmp1[0:64, :], tmp2[0:64, :], ALU.add, ['tmp1', 'tmp2'], ['tmp1'])

            kb = 256
            rope_mm(lambda c, lo, hi: wv_[:, c, kb + lo:kb + hi], 16, lambda c: uT[:, c, :], lambda c: [key, ('uT', c)])
            cp('act', krst[0:64, :], tmp1[0:64, :], ['tmp1'], ['krst'])
            S.dma('sp', kr_d[:, j * TT:(j + 1) * TT], krst[0:64, :], reads=['krst'], writes=[('kr_d', j)], semkey='krst')

            rms_stats(lambda c: cq[:, c, :], 4, 512.0, EPS, ['cq'], sq_buf=qT, sqkey='qTs')
            g0 = pc('qn_g')
            for c in range(4):
                stt(cqn[:, c, :], cq[:, c, :], par[:, g0 + c:g0 + c + 1], rstd[:], ALU.mult, ALU.mult, ['cq', 'par', 'rstd'], ['cqn'])
            rms_stats(lambda c: ckv[:, c, :], 2, 256.0, EPS, ['ckv'], sq_buf=qT, sqkey='qTs')
            g0 = pc('kvn_g')
            for c in range(2):
                stt(ckvn[:, c, :], ckv[:, c, :], par[:, g0 + c:g0 + c + 1], rstd[:], ALU.mult, ALU.mult, ['ckv', 'par', 'rstd'], ['ckvn'])

            SC = float((128 + 64) ** -0.5)
            buf, key = wload('uq')
            wq_ = wview(buf, 0, 4, 1536)
            sqk = [('qTs', c) for c in range(4)]
            for h in range(8):
                p, pk = pf()
                for c in range(4):
                    mm(p[:, :], wq_[:, c, h * 192:h * 192 + 128], cqn[:, c, :], [key, 'cqn'], [pk], start=(c == 0), stop=(c == 3))
                S.op('act', lambda e, p=p, h=h: e.mul(qT[:, h, :], p[:, :], SC), [pk] + sqk, [('qTh', h)] + (sqk if h < 4 else []))
                rb = h * 192 + 128
                rope_mm(lambda c, lo, hi, rb=rb: wq_[:, c, rb + lo:rb + hi], 4, lambda c: cqn[:, c, :], lambda c: [key, 'cqn'])
                S.op('act', lambda e, h=h: e.mul(qrT[0:64, h, :], tmp1[0:64, :], SC), ['tmp1'], [('qrT', h)])
            buf, key = wload('ukv')
            wkv_ = wview(buf, 0, 2, 2048)
            for h in range(8):
                p, pk = pf()
                for c in range(2):
                    mm(p[:, :], wkv_[:, c, h * 256:h * 256 + 128], ckvn[:, c, :], [key, 'ckvn'], [pk], start=(c == 0), stop=(c == 1))
                cp('act', knst[:, :], p[:, :], [pk], ['knst'])
                S.dma('sp', kn_d[h][:, j * TT:(j + 1) * TT], knst[:, :], reads=['knst'], writes=[('kn_d', h, j)], semkey='knst')
            wkv4 = wkv_.rearrange("p k (h two d) -> p k h two d", h=8, two=2)
            for tb in range(4):
                for hg in range(2):
                    p, pk = pf()
                    for c in range(2):
                        mm(p[:, :].rearrange("p (h d) -> p h d", h=4), ckvn[:, c, tb * 128:(tb + 1) * 128],
                           wkv4[:, c, hg * 4:(hg + 1) * 4, 1, :], [key, 'ckvn'], [pk], start=(c == 0), stop=(c == 1))
                    cp('act', vst[tb % 2][:, hg * 512:(hg + 1) * 512], p[:, :], [pk], [('vst', tb % 2)])
                S.dma('sp', vt_d[j * TT + tb * 128:j * TT + (tb + 1) * 128, :], vst[tb % 2], reads=[('vst', tb % 2)],
                      writes=[('vt_d', j, tb)], semkey='vst%d' % (tb % 2))
            S.barrier()
            S.dma('sp', krl[0:64, 0:NK], kr_d[:, 0:NK], reads=[('kr_d', jj) for jj in range(j + 1)], writes=['krl'], semkey='krl')
            for h in range(8):
                S.dma('sp', Kh[:, 0:NK], kn_d[h][:, 0:NK], reads=[('kn_d', h, jj) for jj in range(j + 1)], writes=['Kh'], semkey='Kh')
                S.dma('sp', Vh[:, 0:NK // 128, :], vt_d[0:NK, h * 128:(h + 1) * 128].rearrange("(b p) d -> p b d", p=128),
                      reads=[('vt_d', jj, tb_) for jj in range(j + 1) for tb_ in range(4)], writes=['Vh'], semkey='Vh')
                for qb in range(4):
                    q0 = j * TT + qb * 128
                    nk = q0 + 128
                    qs = slice(qb * 128, (qb + 1) * 128)
                    ng = (nk + 511) // 512
                    for gi in range(ng):
                        k0 = gi * 512
                        kw = min(512, nk - k0)
                        p, pk = pf()
                        mm(p[:, 0:kw], qT[:, h, qs], Kh[:, k0:k0 + kw], [('qTh', h), 'Kh'], [pk], start=True, stop=False)
                        mm(p[:, 0:kw], qrT[0:64, h, qs], krl[0:64, k0:k0 + kw], [('qrT', h), 'krl'], [pk], start=False, stop=True)
                        if k0 + kw == nk:
                            if kw > 128:
                                cp('act', Ssb[:, k0:k0 + kw - 128], p[:, 0:kw - 128], [pk], ['Ssb'])
                            tt('dve', Ssb[:, nk - 128:nk], p[:, kw - 128:kw], cmask[:], ALU.add, [pk, 'cmask'], ['Ssb'])
                        else:
                            cp('act', Ssb[:, k0:k0 + kw], p[:, 0:kw], [pk], ['Ssb'])
                    S.op('dve', lambda e, nk=nk: e.reduce_max(mx, Ssb[:, 0:nk], mybir.AxisListType.X), ['Ssb'], ['mx'])
                    ts('dve', mx, mx, -1.0, ALU.mult, ['mx'], ['mx'])
                    act(P_[:, 0:nk], Ssb[:, 0:nk], AF.Exp, ['Ssb', 'mx'], ['P_', 'rsum'], bias=mx, accum=rsum[:, 0:1])
                    S.op('dve', lambda e: e.reciprocal(rsum[:, 1:2], rsum[:, 0:1]), ['rsum'], ['rinv'])
                    nb = nk // 128
                    po, pok = pf()
                    for b4 in range(0, nb, 4):
                        nbb = min(4, nb - b4)
                        pt_, ptk = pb()
                        for i in range(nbb):
                            tr(pt_[:, i * 128:(i + 1) * 128], P_[:, (b4 + i) * 128:(b4 + i + 1) * 128], identB[:], ['P_', 'identB'], [ptk], inc=(i == nbb - 1))
                        pti = (b4 // 4) % 2
                        cp('act' if pti else 'dve', PT[pti][:, 0:nbb * 128], pt_[:, 0:nbb * 128], [ptk], [('PT', pti)])
                        for i in range(nbb):
                            bb = b4 + i
                            mm(po[:, 0:128], PT[pti][:, i * 128:(i + 1) * 128], Vh[:, bb, :], [('PT', pti), 'Vh'], [pok],
                               start=(bb == 0), stop=(bb == nb - 1))
                    S.op('act', lambda e, po=po: e.mul(osb[:, :], po[:, 0:128], rsum[:, 1:2]), [pok, 'rinv'], ['osb'])
                    pt_, ptk = pb()
                    tr(pt_[:, 0:128], osb[:, :], identB[:], ['osb', 'identB'], [ptk])
                    cp('dve', yT[:, 8 + h, qs], pt_[:, 0:128], [ptk], [('yT', 8 + h)])

        def phase_ffn(l, j):
            S.barrier()
            z = AF32(0, 16 * TT).rearrange("p (c t) -> p c t", c=16)
            o = 16 * TT
            h1 = [AB16(o, 4 * TT // 2).rearrange("p (c t) -> p c t", c=4), AB16(o + 4 * TT // 2, 4 * TT // 2).rearrange("p (c t) -> p c t", c=4)]
            o += 4 * TT
            qm = AB16(o, 4 * TT // 2).rearrange("p (h t) -> p h t", h=4); o += 4 * TT // 2
            om = AB16(o, 4 * TT // 2).rearrange("p (h t) -> p h t", h=4); o += 4 * TT // 2
            Pm_ = AB16(o, 128); o += 128
            PTm = AB16(o, 128); o += 128
            osb = AB16(o, 64); o += 64
            rt = AF32(o, TT); o += TT
            assert o <= ARENA_N
            yk = [('yT', c) for c in range(16)]
            for nb in range(4):
                buf, key = wload(('wout', nb))
                wv_ = wview(buf, 0, 16, 512)
                for i in range(4):
                    n = nb * 4 + i
                    p, pk = pf()
                    for c in range(16):
                        mm(p[:, :], wv_[:, c, i * 128:(i + 1) * 128], yT[:, c, :], [key, ('yT', c)], [pk], start=(c == 0), stop=(c == 15))
                    cp('act' if n % 2 else 'dve', z[:, n, :], p[:, :], [pk], ['z'])
            post_norm_res(z, 'z', 'mix_post_g')
            pre_norm('mem_pre_g')
            buf, key = wload('wq')
            wv_ = wview(buf, 0, 16, 512)
            SCM = float(128 ** -0.5)
            for h in range(4):
                p, pk = pf()
                for c in range(16):
                    mm(p[:, :], wv_[:, c, h * 128:(h + 1) * 128], uT[:, c, :], [key, ('uT', c)], [pk], start=(c == 0), stop=(c == 15))
                S.op('act', lambda e, p=p, h=h: e.mul(qm[:, h, :], p[:, :], SCM), [pk], [('qm', h)])
            rsum = small[:, 28:30]
            mx = small[:, 30:31]
            for h in range(4):
                for qb in range(4):
                    qs = slice(qb * 128, (qb + 1) * 128)
                    p, pk = pf()
                    mm(p[:, 0:NMEM], qm[:, h, qs], kmT[:, h, :], [('qm', h), 'kmT'], [pk])
                    S.op('dve', lambda e, p=p: e.reduce_max(mx, p[:, 0:NMEM], mybir.AxisListType.X), [pk], ['mxm'])
                    ts('dve', mx, mx, -1.0, ALU.mult, ['mxm'], ['mxm'])
                    act(Pm_[:, :], p[:, 0:NMEM], AF.Exp, [pk, 'mxm'], ['Pm_', 'rsm'], bias=mx, accum=rsum[:, 0:1])
                    S.op('dve', lambda e: e.reciprocal(rsum[:, 1:2], rsum[:, 0:1]), ['rsm'], ['rim'])
                    pt_, ptk = pb()
                    for i in range(2):
                        tr(pt_[:, i * 128:(i + 1) * 128], Pm_[:, i * 128:(i + 1) * 128], identB[:], ['Pm_', 'identB'], [ptk], inc=(i == 1))
                    cp('dve', PTm[:, :], pt_[:, 0:256], [ptk], ['PTm'])
                    po, pok = pf()
                    for i in range(2):
                        mm(po[:, 0:128], PTm[:, i * 128:(i + 1) * 128], vm[:, i, h * 128:(h + 1) * 128], ['PTm', 'vm'], [pok], start=(i == 0), stop=(i == 1))
                    S.op('act', lambda e, po=po: e.mul(osb[:, :], po[:, 0:128], rsum[:, 1:2]), [pok, 'rim'], ['osbm'])
                    pt_, ptk = pb()
                    tr(pt_[:, 0:128], osb[:, :], identB[:], ['osbm', 'identB'], [ptk])
                    cp('dve', om[:, h, qs], pt_[:, 0:128], [ptk], ['om'])
            buf, key = wload('wo')
            wv_ = wview(buf, 0, 4, 2048)
            for n in range(16):
                p, pk = pf()
                for c in range(4):
                    mm(p[:, :], wv_[:, c, n * 128:(n + 1) * 128], om[:, c, :], [key, 'om'], [pk], start=(c == 0), stop=(c == 3))
                cp('act' if n % 2 else 'dve', z[:, n, :], p[:, :], [pk], ['z'])
            post_norm_res(z, 'z', 'mem_post_g')
            pre_norm('ffn_pre_g')
            for blk in range(16):
                buf, key = wload(('ff1', blk))
                wv_ = wview(buf, 0, 16, 512)
                hb = h1[blk % 2]
                hk = ('h1', blk % 2)
                for i in range(4):
                    p, pk = pf()
                    for c in range(16):
                        mm(p[:, :], wv_[:, c, i * 128:(i + 1) * 128], uT[:, c, :], [key, ('uT', c)], [pk], start=(c == 0), stop=(c == 15))
                    act(rt, p[:, :], AF.Relu, [pk], ['rt'])
                    tt('pool', hb[:, i, :], rt, rt, ALU.mult, ['rt'], [hk])
                buf, key = wload(('ff2', blk))
                wv_ = wview(buf, 0, 4, 2048)
                for n in range(16):
                    p, pk = pf()
                    for c in range(4):
                        mm(p[:, :], wv_[:, c, n * 128:(n + 1) * 128], hb[:, c, :], [key, hk], [pk], start=(c == 0), stop=(c == 3))
                    if blk == 0:
                        cp('act' if n % 2 else 'dve', z[:, n, :], p[:, :], [pk], [('zf', n)])
                    else:
                        tt('dve', z[:, n, :], z[:, n, :], p[:, :], ALU.add, [pk, ('zf', n)], [('zf', n)])
            S.op('pool', lambda e: e.memset(small[:, 59:60], 0.0), [('zf', n) for n in range(16)], ['z'])
            post_norm_res(z, 'z', 'ffn_post_g')

        emit_convert(0, convert_list(0))
        prep_mem()
        for l in range(L):
            cur['l'] = l
            layer_setup(l)
            nxt = convert_list(l + 1) if l + 1 < L else []
            per = (len(nxt) + NT - 1) // NT if nxt else 0
            for j in range(NT):
                S.barrier()
                if per:
                    emit_convert(l + 1, nxt[j * per:(j + 1) * per])
                load_tile(l, j)
                phase_rwkv(l, j)
                phase_mla(l, j)
                phase_ffn(l, j)
                store_tile(l, j)
        S.barrier(final=True)
        S.emit()
    return nc, cn


def make_in_maps(inputs, T, L, nb):
    cn = make_consts(T)
    maps = []
    f32 = lambda a: np.ascontiguousarray(np.asarray(a, np.float32))
    params = np.stack([pack_params(inputs, l) for l in range(L)], 0)
    shared = {
        "params": f32(params),
        "memg": f32(_fm(inputs['mem_norm_g'], 16)),
        "wimg": build_wimg(inputs, L),
        "w_up": f32(inputs['w_up'][:L]), "a_up": f32(inputs['a_up'][:L]), "g_up": f32(inputs['g_up'][:L]),
        "v_up": f32(inputs['v_up'][:max(L - 1, 1)]),
    }
    for k, v in cn.items():
        shared["c_" + k] = f32(v)
    for b in range(nb):
        m = dict(shared)
        m["x"] = f32(inputs['x'][b][:T])
        m["mem"] = f32(inputs['mem'][b])
        maps.append(m)
    return maps


def kernel(**inputs):
    B, T, _ = inputs['x'].shape
    L = inputs['w_in'].shape[0]
    nc, _ = build(T, L)
    maps = make_in_maps(inputs, T, L, B)
    res = run_bass_kernel_spmd(nc, maps, core_ids=list(range(B)))
    return np.stack([np.asarray(res.results[b]["out"], np.float32) for b in range(B)], 0)
```
